# Optimizing a Trainium2 kernel written in Bass

```python
import math
import jax, jax.numpy as jnp
from jax import lax
import numpy as np

D_MODEL = 1024
BATCH = 16
SEQ = 4096
DEPTH = 1

CHUNK = 64
Q_BLOCK = 128
MLA_HEADS = 8
Q_LORA = 384
KV_LORA = 256
NOPE_DIM = 64
ROPE_DIM = 32
V_DIM = 64
ROPE_THETA = 10000.0
POOL_WIDTH = 512
POOL_WINDOWS = (2, 4, 8, 16)
N_POOL_GROUPS = len(POOL_WINDOWS)
POOL_GROUP_DIM = POOL_WIDTH // N_POOL_GROUPS
MEM_LEN = 256
MEM_HEADS = 4
MEM_HEAD_DIM = D_MODEL // MEM_HEADS
N_GROUPS = 4
EXPERTS_PER_GROUP = 8
N_EXPERTS = N_GROUPS * EXPERTS_PER_GROUP
EXPERT_FF = 256
TOP_K = 2
EPS = 1e-6
IN_SPLITS = tuple(int(v) for v in np.cumsum([Q_LORA, KV_LORA, ROPE_DIM, POOL_WIDTH, D_MODEL]))
IN_COLS = Q_LORA + KV_LORA + ROPE_DIM + POOL_WIDTH + 2 * D_MODEL

kernel_name = "hybrid_mla_pool_memxattn_hmoe"


def rms_norm(x, g):
    xf = x.astype(jnp.float32)
    y = xf * lax.rsqrt(jnp.mean(xf * xf, axis=-1, keepdims=True) + EPS)
    return (y * g.astype(jnp.float32)).astype(x.dtype)


def rope_tables(positions):
    inv_freq = 1.0 / (ROPE_THETA ** (jnp.arange(0, ROPE_DIM, 2, dtype=jnp.float32) / ROPE_DIM))
    ang = positions.astype(jnp.float32)[..., None] * inv_freq
    return jnp.cos(ang), jnp.sin(ang)


def apply_rope(x, cos, sin):
    x1, x2 = jnp.split(x.astype(jnp.float32), 2, axis=-1)
    return jnp.concatenate([x1 * cos - x2 * sin, x2 * cos + x1 * sin], axis=-1).astype(x.dtype)


def mla_attention(q_nope, q_rope, k_nope, k_rope, v):
    S = q_nope.shape[1]
    scale = 1.0 / math.sqrt(NOPE_DIM + ROPE_DIM)
    outs = []
    for blk in range(S // Q_BLOCK):
        s0, s1 = blk * Q_BLOCK, (blk + 1) * Q_BLOCK
        sc = (jnp.einsum('bqhd,bkhd->bhqk', q_nope[:, s0:s1], k_nope[:, :s1])
              + jnp.einsum('bqhd,bkd->bhqk', q_rope[:, s0:s1], k_rope[:, :s1]))
        sc = sc.astype(jnp.float32) * scale
        q_chunk = (s0 + jnp.arange(Q_BLOCK)) // CHUNK
        k_chunk = jnp.arange(s1) // CHUNK
        mask = k_chunk[None, :] <= q_chunk[:, None]
        p = jax.nn.softmax(jnp.where(mask, sc, -jnp.inf), axis=-1).astype(v.dtype)
        outs.append(jnp.einsum('bhqk,bkhd->bqhd', p, v[:, :s1]))
    return jnp.concatenate(outs, axis=1)


def multiscale_pool(u, group_w, scale):
    B, S, C = u.shape
    uf = u.astype(jnp.float32)
    cs = jnp.concatenate([jnp.zeros((B, 1, C), jnp.float32), jnp.cumsum(uf, axis=1)], axis=1)
    t = jnp.arange(S)
    outs = []
    for g, w in enumerate(POOL_WINDOWS):
        c0, c1 = g * POOL_GROUP_DIM, (g + 1) * POOL_GROUP_DIM
        csg = cs[:, :, c0:c1]
        lower = jnp.concatenate([jnp.zeros((B, w - 1, POOL_GROUP_DIM), jnp.float32), csg[:, :S - w + 1]], axis=1)
        count = jnp.minimum(t + 1, w).astype(jnp.float32)[None, :, None]
        outs.append((csg[:, 1:] - lower) / count - uf[:, :, c0:c1])
    d = jnp.stack(outs, axis=2).astype(u.dtype)
    y = jnp.einsum('bsgc,gce->bsge', d, group_w).reshape(B, S, C)
    return y * scale


def memory_cross_attention(hn, memn, w_q, w_kv, w_o):
    B, S, _ = hn.shape
    q = (hn @ w_q).reshape(B, S, MEM_HEADS, MEM_HEAD_DIM)
    k, v = jnp.split(memn @ w_kv, 2, axis=-1)
    k = k.reshape(B, MEM_LEN, MEM_HEADS, MEM_HEAD_DIM)
    v = v.reshape(B, MEM_LEN, MEM_HEADS, MEM_HEAD_DIM)
    sc = jnp.einsum('bqhd,bmhd->bhqm', q, k).astype(jnp.float32) / math.sqrt(MEM_HEAD_DIM)
    p = jax.nn.softmax(sc, axis=-1).astype(v.dtype)
    o = jnp.einsum('bhqm,bmhd->bqhd', p, v).reshape(B, S, D_MODEL)
    return o @ w_o


def hierarchical_moe(xn, w_rg, b_rg, w_re, b_re, w_gate, w_up, w_down):
    B, S, _ = xn.shape
    group_prob = jax.nn.softmax((xn @ w_rg).astype(jnp.float32) + b_rg.astype(jnp.float32), axis=-1)
    g_w, g_idx = lax.top_k(group_prob, 1)
    e_logits = ((xn @ w_re).astype(jnp.float32) + b_re.astype(jnp.float32)).reshape(B, S, N_GROUPS, EXPERTS_PER_GROUP)
    sel = jnp.einsum('bsge,bsg->bse', e_logits, jax.nn.one_hot(g_idx[..., 0], N_GROUPS, dtype=jnp.float32))
    e_w, e_idx = lax.top_k(jax.nn.softmax(sel, axis=-1), TOP_K)
    e_w = e_w / jnp.sum(e_w, axis=-1, keepdims=True)
    weights = g_w * e_w
    global_idx = g_idx * EXPERTS_PER_GROUP + e_idx
    cw = jnp.einsum('bsk,bskn->bsn', weights, jax.nn.one_hot(global_idx, N_EXPERTS, dtype=jnp.float32)).astype(xn.dtype)
    out = jnp.zeros_like(xn)
    for e in range(N_EXPERTS):
        hid = jax.nn.silu(xn @ w_gate[e]) * (xn @ w_up[e])
        out = out + (hid * cw[..., e:e + 1]) @ w_down[e]
    return out


def setup_inputs(seed: int = 0) -> dict:
    key = jax.random.key(seed)
    ks = iter(jax.random.split(key, 40))
    f32 = jnp.float32

    def w(shape, fan_in):
        return jax.random.normal(next(ks), shape, f32) * (fan_in ** -0.5)

    def gain(shape):
        return 1.0 + 0.02 * jax.random.normal(next(ks), shape, f32)

    def bias(shape):
        return 0.01 * jax.random.normal(next(ks), shape, f32)

    L = DEPTH
    x = jax.random.normal(next(ks), (BATCH, SEQ, D_MODEL), f32)
    mem = jax.random.normal(next(ks), (BATCH, MEM_LEN, D_MODEL), f32)
    offsets = jax.random.randint(next(ks), (BATCH, 1), 0, 4096, dtype=jnp.int32)
    positions = (offsets + jnp.arange(SEQ, dtype=jnp.int32)[None, :]).astype(jnp.int32)
    return {
        "x": x,
        "mem": mem,
        "positions": positions,
        "mix_norm_g": gain((L, D_MODEL)),
        "w_in": w((L, D_MODEL, IN_COLS), D_MODEL),
        "q_norm_g": gain((L, Q_LORA)),
        "w_q_up": w((L, Q_LORA, MLA_HEADS * (NOPE_DIM + ROPE_DIM)), Q_LORA),
        "kv_norm_g": gain((L, KV_LORA)),
        "w_kv_up": w((L, KV_LORA, MLA_HEADS * (NOPE_DIM + V_DIM)), KV_LORA),
        "w_attn_branch": w((L, MLA_HEADS * V_DIM, D_MODEL), MLA_HEADS * V_DIM),
        "pool_w": w((L, N_POOL_GROUPS, POOL_GROUP_DIM, POOL_GROUP_DIM), POOL_GROUP_DIM),
        "pool_scale": 1.0 + 0.1 * jax.random.normal(next(ks), (L, POOL_WIDTH), f32),
        "w_pool_branch": w((L, POOL_WIDTH, D_MODEL), POOL_WIDTH),
        "w_mix_out": w((L, D_MODEL, D_MODEL), D_MODEL),
        "xattn_norm_g": gain((L, D_MODEL)),
        "mem_norm_g": gain((L, D_MODEL)),
        "w_xq": w((L, D_MODEL, D_MODEL), D_MODEL),
        "w_xkv": w((L, D_MODEL, 2 * D_MODEL), D_MODEL),
        "w_xo": w((L, D_MODEL, D_MODEL), D_MODEL),
        "ffn_norm_g": gain((L, D_MODEL)),
        "w_router_group": w((L, D_MODEL, N_GROUPS), D_MODEL),
        "b_router_group": bias((L, N_GROUPS)),
        "w_router_expert": w((L, D_MODEL, N_EXPERTS), D_MODEL),
        "b_router_expert": bias((L, N_EXPERTS)),
        "w_exp_gate": w((L, N_EXPERTS, D_MODEL, EXPERT_FF), D_MODEL),
        "w_exp_up": w((L, N_EXPERTS, D_MODEL, EXPERT_FF), D_MODEL),
        "w_exp_down": w((L, N_EXPERTS, EXPERT_FF, D_MODEL), EXPERT_FF),
        "final_norm_g": gain((D_MODEL,)),
    }


def reference(x, mem, positions, mix_norm_g, w_in, q_norm_g, w_q_up, kv_norm_g, w_kv_up,
              w_attn_branch, pool_w, pool_scale, w_pool_branch, w_mix_out,
              xattn_norm_g, mem_norm_g, w_xq, w_xkv, w_xo,
              ffn_norm_g, w_router_group, b_router_group, w_router_expert, b_router_expert,
              w_exp_gate, w_exp_up, w_exp_down, final_norm_g):
    B, S, _ = x.shape
    cos, sin = rope_tables(positions)
    h = x
    for l in range(DEPTH):
        hn = rms_norm(h, mix_norm_g[l])
        z = hn @ w_in[l]
        q_lat, kv_lat, k_rope, u_pool, gate_a, gate_b = jnp.split(z, IN_SPLITS, axis=-1)
        q = (rms_norm(q_lat, q_norm_g[l]) @ w_q_up[l]).reshape(B, S, MLA_HEADS, NOPE_DIM + ROPE_DIM)
        q_nope, q_rope = q[..., :NOPE_DIM], q[..., NOPE_DIM:]
        q_rope = apply_rope(q_rope, cos[:, :, None, :], sin[:, :, None, :])
        kv = (rms_norm(kv_lat, kv_norm_g[l]) @ w_kv_up[l]).reshape(B, S, MLA_HEADS, NOPE_DIM + V_DIM)
        k_nope, v = kv[..., :NOPE_DIM], kv[..., NOPE_DIM:]
        k_rope = apply_rope(k_rope, cos, sin)
        attn = mla_attention(q_nope, q_rope, k_nope, k_rope, v).reshape(B, S, MLA_HEADS * V_DIM)
        y_a = attn @ w_attn_branch[l]
        y_b = multiscale_pool(u_pool, pool_w[l], pool_scale[l]) @ w_pool_branch[l]
        g_a = jax.nn.sigmoid(gate_a.astype(jnp.float32)).astype(h.dtype)
        g_b = jax.nn.sigmoid(gate_b.astype(jnp.float32)).astype(h.dtype)
        h = h + (g_a * y_a + g_b * y_b) @ w_mix_out[l]
        h = h + memory_cross_attention(rms_norm(h, xattn_norm_g[l]), rms_norm(mem, mem_norm_g[l]),
                                       w_xq[l], w_xkv[l], w_xo[l])
        h = h + hierarchical_moe(rms_norm(h, ffn_norm_g[l]), w_router_group[l], b_router_group[l],
                                 w_router_expert[l], b_router_expert[l],
                                 w_exp_gate[l], w_exp_up[l], w_exp_down[l])
    return rms_norm(h, final_norm_g)
```

```python
import math
from contextlib import ExitStack
import numpy as np
import ml_dtypes
import concourse.bass as bass
import concourse.mybir as mybir
from concourse.bass_utils import run_bass_kernel_spmd

F32 = mybir.dt.float32
BF16 = mybir.dt.bfloat16
I32 = mybir.dt.int32
AF = mybir.ActivationFunctionType
ALU = mybir.AluOpType
AX = mybir.AxisListType

D = 1024
NCH = 8
EPS = 1e-6
Q_LORA, KV_LORA, ROPE, POOLW = 384, 256, 32, 512
NH, NOPE, VD = 8, 64, 64
MEM, MH, MHD = 256, 4, 256
NG, EPG, NE, FF = 4, 8, 32, 256
C_Q, C_KV, C_KR, C_POOL, C_GA, C_GB = 0, 384, 640, 672, 1184, 2208
IN_COLS = 3232


class Buf:
    def __init__(self, name):
        self.name = name
        self.w = {}
        self.r = {}
        self.wsem = None
        self.rsem = None


class Sched:
    def __init__(self, nc, es):
        self.nc = nc
        self.es = es
        self.E = {"pe": nc.tensor, "act": nc.scalar, "dve": nc.vector, "pool": nc.gpsimd, "sp": nc.sync}
        self.sem = {}
        self.cnt = {}
        self.seen = {e: {} for e in self.E}
        for e in self.E:
            self.sem[e] = es.enter_context(nc.semaphore("s_" + e))
            self.cnt[e] = 0
        self.dsems = []
        self.nops = 0

    def _wait(self, eng, deps):
        for key, (sem, val, _e) in deps.items():
            if self.seen[eng].get(key, 0) < val:
                self.E[eng].wait_ge(sem, val)
                self.seen[eng][key] = val

    @staticmethod
    def _merge(dst, src, skip_eng=None):
        for k, (s, v, e) in src.items():
            if skip_eng is not None and e == skip_eng:
                continue
            if k not in dst or dst[k][1] < v:
                dst[k] = (s, v, e)

    def op(self, eng, fn, reads=(), writes=()):
        deps = {}
        for b in reads:
            self._merge(deps, b.w)
        for b in writes:
            self._merge(deps, b.w, skip_eng=eng)
            self._merge(deps, b.r, skip_eng=eng)
        self._wait(eng, deps)
        ins = fn(self.E[eng])
        self.cnt[eng] += 1
        ins.then_inc(self.sem[eng], 1)
        tok = (self.sem[eng], self.cnt[eng], eng)
        for b in reads:
            self._merge(b.r, {eng: tok})
        for b in writes:
            b.w = {eng: tok}
            b.r = {}
        self.nops += 1

    def _newsem(self, name):
        s = self.es.enter_context(self.nc.semaphore("%s_%d" % (name, len(self.dsems))))
        ent = [s, 0, "d%d" % len(self.dsems)]
        self.dsems.append(ent)
        return ent

    def dma(self, q, out, in_, load=None, store=None, **kw):
        deps = {}
        if load is not None:
            b = load
            self._merge(deps, b.w)
            self._merge(deps, b.r)
            if b.name.startswith("con"):
                deps = {k: v for k, v in deps.items() if v[2] != "dma"}
            key = "w_" + q
        else:
            b = store
            self._merge(deps, b.w)
            key = "r_" + q
        sems = b.__dict__.setdefault("qsem", {})
        if key not in sems:
            sems[key] = self._newsem("d%s_%s" % (key, b.name))
        ent = sems[key]
        self._wait(q, deps)
        ins = self.E[q].dma_start(out=out, in_=in_, **kw)
        ent[1] += 16
        ins.then_inc(ent[0], 16)
        tok = (ent[0], ent[1], "dma")
        if load is not None:
            self._merge(b.w, {ent[2]: tok})
        else:
            self._merge(b.r, {ent[2]: tok})

    def idma(self, out, in_, idx_ap, scatter, buf, idxbuf):
        q = "pool"
        deps = {}
        self._merge(deps, idxbuf.w)
        b = buf
        if scatter:
            self._merge(deps, b.w)
            key = "r_pool_i"
        else:
            self._merge(deps, b.w)
            self._merge(deps, b.r)
            key = "w_pool_i"
        sems = b.__dict__.setdefault("qsem", {})
        if key not in sems:
            sems[key] = self._newsem("di_" + b.name)
        ent = sems[key]
        self._wait(q, deps)
        if scatter:
            ins = self.E[q].indirect_dma_start(out=out, out_offset=bass.IndirectOffsetOnAxis(ap=idx_ap, axis=0), in_=in_, in_offset=None)
        else:
            ins = self.E[q].indirect_dma_start(out=out, out_offset=None, in_=in_, in_offset=bass.IndirectOffsetOnAxis(ap=idx_ap, axis=0))
        ent[1] += 16
        ins.then_inc(ent[0], 16)
        tok = (ent[0], ent[1], "dma")
        self._merge(idxbuf.r, {ent[2]: tok})
        if scatter:
            self._merge(b.r, {ent[2]: tok})
        else:
            self._merge(b.w, {ent[2]: tok})

    def barrier(self, engines=None):
        for e in (engines or self.E):
            deps = {}
            for f in self.E:
                if f != e and self.cnt[f] > 0:
                    deps[f] = (self.sem[f], self.cnt[f], f)
            for ent in self.dsems:
                if ent[1] > 0:
                    deps[ent[2]] = (ent[0], ent[1], "dma")
            self._wait(e, deps)


def _bc_last(ap, n):
    sh = list(ap.shape)
    return ap.unsqueeze(len(sh)).broadcast_to(sh + [n])


class Prog:
    def __init__(self, NB, S, phases=("p1", "p2a", "p2b", "p3"), dbg=False):
        self.NB, self.S = NB, S
        self.NTOK = NB * S
        self.phases = phases
        self.dbg = dbg

    def build(self):
        nc = bass.Bass("TRN2", target_bir_lowering=False)
        self.nc = nc
        NTOK = self.NTOK
        dt = lambda n, s, d=F32, kind="ExternalInput": nc.dram_tensor(n, list(s), d, kind=kind).ap()
        self.x = dt("x", [NTOK, D])
        self.mem = dt("mem", [self.NB * MEM, D])
        self.pos = dt("positions", [self.NB, self.S], I32)
        self.w = {}
        for n, s in [("mix_norm_g", [D]), ("w_in", [D, IN_COLS]), ("q_norm_g", [Q_LORA]), ("w_q_up", [Q_LORA, 768]),
                     ("kv_norm_g", [KV_LORA]), ("w_kv_up", [KV_LORA, 1024]), ("w_attn_branch", [512, D]),
                     ("pool_w", [4, 128, 128]), ("pool_scale", [POOLW]), ("w_pool_branch", [POOLW, D]),
                     ("w_mix_out", [D, D]), ("xattn_norm_g", [D]), ("mem_norm_g", [D]), ("w_xq", [D, D]),
                     ("w_xkv", [D, 2 * D]), ("w_xo", [D, D]), ("ffn_norm_g", [D]), ("w_router", [D, 36]),
                     ("b_router", [36]), ("w_exp_gate", [NE, D, FF]), ("w_exp_up", [NE, D, FF]),
                     ("w_exp_down", [NE, FF, D]), ("final_norm_g", [D])]:
            self.w[n] = dt(n, s)
        self.c_ident = dt("c_ident", [128, 128])
        self.c_sel = dt("c_sel", [32, 32 * 128])
        self.c_inv16 = dt("c_inv16", [128, 16])
        self.c_ones = dt("c_ones", [128, 128])
        self.c_sel2 = dt("c_sel2", [64, 128])
        self.c_rope = dt("c_rope", [128, 4])
        self.c_tri = dt("c_tri", [128, 128])
        self.c_kb = dt("c_kb", [128, 96])
        self.c_ep = dt("c_ep", [128, 8])
        NBK = 2 * NTOK // 512 + 32
        self.NBK3 = NBK
        self.wgs = dt("wgs", [NE * 128, NCH * FF], BF16, kind="Internal")
        self.wus = dt("wus", [NE * 128, NCH * FF], BF16, kind="Internal")
        self.wds = dt("wds", [NE * 128, 2 * D], BF16, kind="Internal")
        self.hn3 = dt("hn3", [NTOK, D], BF16, kind="Internal")
        self.lscr = dt("lscr", [NTOK, 36], F32, kind="Internal")
        self.s_win2 = dt("s_win2", [128, NCH * 2560], BF16, kind="Internal")
        self.s_wab = dt("s_wab", [128, 4 * D], BF16, kind="Internal")
        self.s_pw = dt("s_pw", [128, 4 * 128], BF16, kind="Internal")
        self.s_wpb = dt("s_wpb", [128, 4 * D], BF16, kind="Internal")
        self.s_wmo = dt("s_wmo", [128, NCH * D], BF16, kind="Internal")
        self.s_wxq = dt("s_wxq", [128, NCH * D], BF16, kind="Internal")
        self.s_wxo = dt("s_wxo", [128, NCH * D], BF16, kind="Internal")
        self.s_wkv = dt("s_wkv", [4, 128, NCH * 512], BF16, kind="Internal")
        self.sortx = dt("sortx", [NBK * 512, D + 8], BF16, kind="Internal")
        self.sortout = dt("sortout", [NBK * 512, D], F32, kind="Internal")
        self.out = dt("out", [NTOK, D], kind="ExternalOutput")
        if self.dbg:
            self.h2 = dt("h2", [NTOK, D], kind="ExternalOutput" if "p2a" in self.phases else "ExternalInput")
            self.attnT = dt("attnT", [512, NTOK], BF16, kind="ExternalOutput" if "p1" in self.phases else "ExternalInput")
        else:
            self.h2 = dt("h2", [NTOK, D], kind="Internal")
            self.attnT = dt("attnT", [512, NTOK], BF16, kind="Internal")
        with ExitStack() as es:
            self.es = es
            self.s = Sched(nc, es)
            self.psum = es.enter_context(nc.psum_tensor("psum", [128, 8 * 512], F32))
            self.pb = [Buf("pb%d" % i) for i in range(8)]
            if "p1" not in self.phases:
                self.prep_dense_scratch()
                self.prep_expert_scratch()
                self.s.barrier()
            for name, fn in [("p1", self.phase1), ("p2a", self.phase2a), ("p2b", self.phase2b), ("p3", self.phase3)]:
                if name in self.phases:
                    with ExitStack() as ps:
                        fn(ps)
                        self.s.barrier()
        return nc

    def uname(self, n):
        self._uid = getattr(self, "_uid", 0) + 1
        return "%s_%d" % (n, self._uid)

    def bank(self, i, dtype=F32):
        ap = self.psum[:, i * 512:(i + 1) * 512]
        if dtype is not F32:
            ap = ap.bitcast(dtype)
        return ap


    def mk_norm(self, sb, tag, xs=True, junk=True):
        n = {}
        if xs:
            n["XS"] = [sb("XS%s%d" % (tag, i), [128, D], BF16) for i in range(2)]
            n["bXS"] = [Buf("XS%d" % i) for i in range(2)]
        if junk:
            n["JUNK"] = sb("JUNK" + tag, [128, D], BF16)
        n["bJ"] = Buf("JUNK")
        n["ssq"] = sb("ssq" + tag, [128, 16], F32); n["bssq"] = Buf("ssq")
        n["rstd"] = sb("rstd" + tag, [128, 16], F32); n["brstd"] = Buf("rstd")
        n["epsc"] = sb("epsc" + tag, [128, 1], F32)
        self.s.op("dve", lambda e: e.memset(n["epsc"][:], EPS), writes=[n["bssq"]])
        return n

    def rms_stats(self, n, src_fn, bsrc, ntt, width=D, col0=0, junk_fn=None, bjunk=None):
        s = self.s
        ssq, rstd = n["ssq"], n["rstd"]
        for t in range(ntt):
            jout = junk_fn(t) if junk_fn is not None else n["JUNK"][:, :width]
            bj = bjunk if junk_fn is not None else n["bJ"]
            s.op("act", lambda e, t=t, jout=jout: e.activation(out=jout, in_=src_fn(t), func=AF.Square,
                                                              accum_out=ssq[:, col0 + t:col0 + t + 1]),
                 reads=[bsrc], writes=[bj, n["bssq"]])
        s.op("act", lambda e: e.activation(out=rstd[:, col0:col0 + ntt], in_=ssq[:, col0:col0 + ntt], func=AF.Ln,
                                           scale=1.0 / width, bias=n["epsc"][:]),
             reads=[n["bssq"]], writes=[n["brstd"]])
        s.op("act", lambda e: e.activation(out=rstd[:, col0:col0 + ntt], in_=rstd[:, col0:col0 + ntt], func=AF.Exp, scale=-0.5),
             reads=[n["brstd"]], writes=[n["brstd"]])

    def norm_T(self, n, src_fn, bsrc, gb, bgb, ntt, dstT, bdst, ident_b, bcon, nch=NCH):
        s = self.s
        self.rms_stats(n, src_fn, bsrc, ntt)
        XS, bXS, rstd = n["XS"], n["bXS"], n["rstd"]
        for t in range(ntt):
            k = t % 2
            s.op("dve", lambda e, t=t, k=k: e.scalar_tensor_tensor(out=XS[k][:], in0=src_fn(t), scalar=rstd[:, t:t + 1],
                                                                   in1=gb[:], op0=ALU.mult, op1=ALU.mult),
                 reads=[bsrc, n["brstd"], bgb], writes=[bXS[k]])
            def tr(e, k=k):
                ins = None
                for c in range(nch):
                    ins = e.transpose(out=self.bank(0, BF16)[:, c * 128:(c + 1) * 128], in_=XS[k][:, c * 128:(c + 1) * 128],
                                      identity=ident_b[:])
                return ins
            s.op("pe", tr, reads=[bXS[k], bcon], writes=[self.pb[0]])
            s.op("act", lambda e, t=t: e.copy(out=dstT[:, :, t * 128:(t + 1) * 128],
                                              in_=self.bank(0, BF16).rearrange("p (c t) -> p c t", c=nch)),
                 reads=[self.pb[0]], writes=[bdst])

    def norm_scale4(self, n, src_fn, bsrc, gb, bgb, ntt, XS4, bXS4):
        s = self.s
        self.rms_stats(n, src_fn, bsrc, ntt, junk_fn=lambda t: XS4[:, t, :], bjunk=bXS4)
        rstd = n["rstd"]
        for t in range(ntt):
            s.op("dve", lambda e, t=t: e.scalar_tensor_tensor(out=XS4[:, t, :], in0=src_fn(t), scalar=rstd[:, t:t + 1],
                                                              in1=gb[:], op0=ALU.mult, op1=ALU.mult),
                 reads=[bsrc, n["brstd"], bgb], writes=[bXS4])

    def norm_tr4(self, XS4, bXS4, ntt, dstT, bdst, ident_b, bcon, nch=NCH, banks=(0, 0)):
        s = self.s
        for t in range(ntt):
            bk = banks[t % 2]
            def tr(e, t=t, bk=bk):
                ins = None
                for c in range(nch):
                    ins = e.transpose(out=self.bank(bk, BF16)[:, c * 128:(c + 1) * 128], in_=XS4[:, t, c * 128:(c + 1) * 128],
                                      identity=ident_b[:])
                return ins
            s.op("pe", tr, reads=[bXS4, bcon], writes=[self.pb[bk]])
            if t % 2 == 0:
                s.op("act", lambda e, t=t, bk=bk: e.copy(out=dstT[:, :, t * 128:(t + 1) * 128],
                                                        in_=self.bank(bk, BF16).rearrange("p (c t) -> p c t", c=nch)),
                     reads=[self.pb[bk]], writes=[bdst])
            else:
                s.op("dve", lambda e, t=t, bk=bk: e.tensor_copy(out=dstT[:, :, t * 128:(t + 1) * 128],
                                                               in_=self.bank(bk, BF16).rearrange("p (c t) -> p c t", c=nch)),
                     reads=[self.pb[bk]], writes=[bdst])

    def mm_group(self, e, out, pairs):
        ins = None
        n = len(pairs)
        for i, (l, r) in enumerate(pairs):
            ins = e.matmul(out=out, lhsT=l, rhs=r, start=(i == 0), stop=(i == n - 1))
        return ins

    def phase2a(self, ps):
        nc, s, pb = self.nc, self.s, self.pb
        sb = lambda n, sh, d=F32: ps.enter_context(nc.sbuf_tensor(self.uname(n), list(sh), d))
        NBLK = self.NTOK // 512
        BPS = self.S // 512
        W = self.w
        CON = Buf("con2a")
        ident_b = sb("ident_b", [128, 128], BF16)
        g1b = sb("g1b", [128, D], F32)
        inv16 = sb("inv16", [128, 16], F32)
        psc = sb("psc", [128, 4], F32)
        WIN2 = sb("WIN2", [128, NCH, 2560], BF16)
        WAB = sb("WAB", [128, 4, D], BF16)
        PW = sb("PW", [128, 4, 128], BF16)
        WPB = sb("WPB", [128, 4, D], BF16)
        WMO = sb("WMO", [128, NCH, D], BF16)
        s.dma("pool", ident_b[:], self.c_ident, load=CON)
        s.dma("sp", g1b[:], W["mix_norm_g"].partition_broadcast(128), load=CON)
        s.dma("sp", inv16[:], self.c_inv16, load=CON)
        s.dma("sp", psc[:], W["pool_scale"].rearrange("(g p) -> p g", p=128), load=CON, allow_slow_non_contiguous=True)
        fl = lambda t: t[:].rearrange("p c n -> p (c n)")
        CONW = Buf("conw2a")
        win2_v = self.s_win2.rearrange("p (c n) -> p c n", c=NCH)
        s.dma("sp", WIN2[:, :, 0:512], win2_v[:, :, 0:512], load=CON)
        s.dma("sp", fl(PW), self.s_pw, load=CON)
        s.dma("sp", WIN2[:, :, 512:2560], win2_v[:, :, 512:2560], load=CONW)
        s.dma("sp", fl(WAB), self.s_wab, load=CONW)
        s.dma("sp", fl(WPB), self.s_wpb, load=CONW)
        s.dma("sp", fl(WMO), self.s_wmo, load=CONW)
        nrm = self.mk_norm(sb, "a")
        Xs = [sb("X%d" % i, [128, 4, D], F32) for i in range(2)]; bXs = [Buf("X%d" % i) for i in range(2)]
        HTs = [sb("HT%d" % i, [128, NCH, 512], BF16) for i in range(2)]; bHTs = [Buf("HT%d" % i) for i in range(2)]
        ATs = [sb("AT%d" % i, [128, 4, 512], BF16) for i in range(2)]; bATs = [Buf("AT%d" % i) for i in range(2)]
        XS4 = sb("XS4", [128, 4, D], BF16); bXS4 = Buf("XS4")
        U = sb("U", [128, 4, 528], F32); bU = [Buf("U%d" % g) for g in range(4)]
        TA = sb("TA", [128, 528], F32); TB = sb("TB", [128, 528], F32); bTA = Buf("TA"); bTB = Buf("TB")
        FX = sb("FX", [128, 16], F32); bFX = Buf("FX")
        DT = sb("DT", [128, 4, 512], BF16); bDT = [Buf("DT%d" % g) for g in range(4)]
        YB = sb("YB", [128, 4, 512], BF16); bYB = Buf("YB")
        MT = sb("MT", [128, NCH, 512], BF16); bMT = Buf("MT")
        SA = [sb("SA%d" % i, [128, 512], F32) for i in range(2)]; bSA = [Buf("SA%d" % i) for i in range(2)]
        SB_ = [sb("SB%d" % i, [128, 512], F32) for i in range(2)]; bSB = [Buf("SB%d" % i) for i in range(2)]
        def pro_load(blk):
            tok0 = blk * 512
            X, bX, AT, bAT = Xs[blk % 2], bXs[blk % 2], ATs[blk % 2], bATs[blk % 2]
            s.dma("sp", X[:], self.x[tok0:tok0 + 512, :].rearrange("(t p) d -> p t d", p=128), load=bX)
            s.dma("sp", AT[:], self.attnT[:, tok0:tok0 + 512].rearrange("(c p) t -> p c t", p=128), load=bAT)

        def pro_scale(blk):
            X, bX = Xs[blk % 2], bXs[blk % 2]
            self.norm_scale4(nrm, lambda t: X[:, t, :], bX, g1b, CON, 4, XS4, bXS4)

        def pro_T(blk):
            self.norm_tr4(XS4, bXS4, 4, HTs[blk % 2], bHTs[blk % 2], ident_b, CON, banks=(0, 2))

        YBs = [YB, sb("YB2", [128, 4, 512], BF16)]; bYBs = [bYB, Buf("YB2")]

        def pool_group(blk, g):
            first = (blk % BPS == 0)
            HT, bHT = HTs[blk % 2], bHTs[blk % 2]
            YBc, bYBc = YBs[blk % 2], bYBs[blk % 2]
            w = 2 << g
            bk = 1 + (g % 2)
            s.op("pe", lambda e: self.mm_group(e, self.bank(bk), [(WIN2[:, c, g * 128:(g + 1) * 128], HT[:, c, :]) for c in range(NCH)]),
                 reads=[bHT, CON], writes=[pb[bk]])
            if first:
                s.op("dve", lambda e: e.memset(U[:, g, 0:16], 0.0), writes=[bU[g]])
            s.op("act", lambda e: e.copy(out=U[:, g, 16:528], in_=self.bank(bk)), reads=[pb[bk]], writes=[bU[g]])
            cur, bcur = U[:, g, :], bU[g]
            lo = 0
            sh = 1
            bufs = [(TA, bTA), (TB, bTB)]
            bi = 0
            while sh < w:
                lo += sh
                dst, bdst = bufs[bi]
                s.op("dve", lambda e: e.tensor_tensor(out=dst[:, lo:528], in0=cur[:, lo:528], in1=cur[:, lo - sh:528 - sh], op=ALU.add),
                     reads=[bcur], writes=[bdst])
                cur, bcur = dst, bdst
                bi ^= 1
                sh *= 2
            s.op("dve", lambda e: e.scalar_tensor_tensor(out=DT[:, g, :], in0=cur[:, 16:528], scalar=1.0 / w, in1=U[:, g, 16:528], op0=ALU.mult, op1=ALU.subtract),
                 reads=[bcur, bU[g]], writes=[bDT[g]])
            if first:
                s.op("dve", lambda e: e.tensor_tensor(out=FX[:, 0:w - 1], in0=cur[:, 16:16 + w - 1], in1=inv16[:, 0:w - 1], op=ALU.mult),
                     reads=[bcur, CON], writes=[bFX])
                s.op("dve", lambda e: e.tensor_tensor(out=DT[:, g, 0:w - 1], in0=FX[:, 0:w - 1], in1=U[:, g, 16:16 + w - 1], op=ALU.subtract),
                     reads=[bFX, bU[g]], writes=[bDT[g]])
            s.op("dve", lambda e: e.tensor_copy(out=U[:, g, 0:16], in_=U[:, g, 512:528]), reads=[bU[g]], writes=[bU[g]])

        def pool_group2(blk, g):
            YBc, bYBc = YBs[blk % 2], bYBs[blk % 2]
            s.op("pe", lambda e: e.matmul(out=self.bank(3), lhsT=PW[:, g, :], rhs=DT[:, g, :], start=True, stop=True),
                 reads=[bDT[g], CON], writes=[pb[3]])
            s.op("act", lambda e: e.activation(out=YBc[:, g, :], in_=self.bank(3), func=AF.Copy, scale=psc[:, g:g + 1]),
                 reads=[pb[3], CON], writes=[bYBc])

        pro_load(0); pro_scale(0); pro_T(0)
        for g in range(4):
            pool_group(0, g)
        for g in range(4):
            pool_group2(0, g)
        for blk in range(NBLK):
            tok0 = blk * 512
            X, bX, AT, bAT, HT, bHT = Xs[blk % 2], bXs[blk % 2], ATs[blk % 2], bATs[blk % 2], HTs[blk % 2], bHTs[blk % 2]
            YBc, bYBc = YBs[blk % 2], bYBs[blk % 2]
            more = blk + 1 < NBLK
            if more:
                pro_load(blk + 1)
            for dc in range(NCH):
                b0 = 4
                k = dc % 2
                cs = slice(dc * 128, (dc + 1) * 128)
                s.op("pe", lambda e: self.mm_group(e, self.bank(b0), [(WIN2[:, c, 512 + dc * 128:512 + (dc + 1) * 128], HT[:, c, :]) for c in range(NCH)]),
                     reads=[bHT, CON, CONW], writes=[pb[b0]])
                s.op("pe", lambda e: self.mm_group(e, self.bank(b0 + 1), [(WIN2[:, c, 1536 + dc * 128:1536 + (dc + 1) * 128], HT[:, c, :]) for c in range(NCH)]),
                     reads=[bHT, CON, CONW], writes=[pb[b0 + 1]])
                s.op("pe", lambda e: self.mm_group(e, self.bank(b0 + 2), [(WAB[:, c, cs], AT[:, c, :]) for c in range(4)]),
                     reads=[bAT, CON, CONW], writes=[pb[b0 + 2]])
                s.op("pe", lambda e: self.mm_group(e, self.bank(b0 + 3), [(WPB[:, c, cs], YBc[:, c, :]) for c in range(4)]),
                     reads=[bYBc, CON, CONW], writes=[pb[b0 + 3]])
                s.op("act", lambda e: e.activation(out=SA[k][:], in_=self.bank(b0), func=AF.Sigmoid), reads=[pb[b0]], writes=[bSA[k]])
                s.op("act", lambda e: e.activation(out=SB_[k][:], in_=self.bank(b0 + 1), func=AF.Sigmoid), reads=[pb[b0 + 1]], writes=[bSB[k]])
                s.op("dve", lambda e: e.tensor_tensor(out=SA[k][:], in0=SA[k][:], in1=self.bank(b0 + 2), op=ALU.mult),
                     reads=[bSA[k], pb[b0 + 2]], writes=[bSA[k]])
                s.op("dve", lambda e: e.tensor_tensor(out=SB_[k][:], in0=SB_[k][:], in1=self.bank(b0 + 3), op=ALU.mult),
                     reads=[bSB[k], pb[b0 + 3]], writes=[bSB[k]])
                s.op("dve", lambda e: e.tensor_tensor(out=MT[:, dc, :], in0=SA[k][:], in1=SB_[k][:], op=ALU.add),
                     reads=[bSA[k], bSB[k]], writes=[bMT])
                if more:
                    if dc == 2:
                        pro_scale(blk + 1)
                    elif dc == 3:
                        pro_T(blk + 1)
                    if 4 <= dc <= 7:
                        pool_group(blk + 1, dc - 4)
                    if 5 <= dc <= 7:
                        pool_group2(blk + 1, dc - 5)
            if more:
                pool_group2(blk + 1, 3)
            for t in range(4):
                for dh in range(2):
                    bd = (t * 2 + dh) % 2
                    s.op("pe", lambda e: self.mm_group(e, self.bank(bd), [(MT[:, c, t * 128:(t + 1) * 128], WMO[:, c, dh * 512:(dh + 1) * 512]) for c in range(NCH)]),
                         reads=[bMT, CON, CONW], writes=[pb[bd]])
                    s.op("dve", lambda e: e.tensor_tensor(out=X[:, t, dh * 512:(dh + 1) * 512], in0=X[:, t, dh * 512:(dh + 1) * 512], in1=self.bank(bd), op=ALU.add),
                         reads=[bX, pb[bd]], writes=[bX])
            s.dma("sp", self.h2[tok0:tok0 + 512, :].rearrange("(t p) d -> p t d", p=128), X[:], store=bX)

    def phase2b(self, ps):
        nc, s, pb = self.nc, self.s, self.pb
        sb = lambda n, sh, d=F32: ps.enter_context(nc.sbuf_tensor(self.uname(n), list(sh), d))
        NBLK = self.NTOK // 512
        BPS = self.S // 512
        W = self.w
        CON = Buf("con2b")
        ident_b = sb("ident_b", [128, 128], BF16)
        ones_b = sb("ones_b", [128, 128], BF16)
        g2b = sb("g2b", [128, D], F32)
        gmb = sb("gmb", [128, D], F32)
        WXQ = sb("WXQ", [128, NCH, D], BF16)
        WXO = sb("WXO", [128, NCH, D], BF16)
        s.dma("pool", ident_b[:], self.c_ident, load=CON)
        s.dma("pool", ones_b[:], self.c_ones, load=CON)
        s.dma("sp", g2b[:], W["xattn_norm_g"].partition_broadcast(128), load=CON)
        s.dma("sp", gmb[:], W["mem_norm_g"].partition_broadcast(128), load=CON)
        g3b = sb("g3b", [128, D], F32)
        wr = sb("wr", [128, NCH, 36], BF16)
        bb3 = sb("bb3", [128, 36], F32)
        s.dma("sp", g3b[:], W["ffn_norm_g"].partition_broadcast(128), load=CON)
        s.dma("pool", wr[:], W["w_router"].rearrange("(c p) n -> p c n", p=128), load=CON)
        s.dma("sp", bb3[:], W["b_router"].partition_broadcast(128), load=CON)
        CONW = Buf("conw2b")
        s.dma("sp", WXQ[:].rearrange("p c n -> p (c n)"), self.s_wxq, load=CONW)
        s.dma("sp", WXO[:].rearrange("p c n -> p (c n)"), self.s_wxo, load=CONW)
        nrm = self.mk_norm(sb, "b")
        nrm3 = self.mk_norm(sb, "b3", xs=False, junk=False)
        nrm3["JUNK"] = nrm["JUNK"]; nrm3["bJ"] = nrm["bJ"]
        XS3 = sb("XS3", [128, 4, D], BF16); bXS3 = Buf("XS3")
        XT3 = sb("XT3", [128, NCH, 512], BF16); bXT3 = Buf("XT3")
        L4 = sb("L4", [128, 4, 36], F32); bL4 = Buf("L4")
        WT = sb("WT", [128, NCH, 512], BF16); bWT = Buf("WT")
        Xs = [sb("X%d" % i, [128, 4, D], F32) for i in range(2)]; bXs = [Buf("X%d" % i) for i in range(2)]
        HTs = [sb("HT%d" % i, [128, NCH, 512], BF16) for i in range(2)]; bHTs = [Buf("HT%d" % i) for i in range(2)]
        XS4 = sb("XS4", [128, 4, D], BF16); bXS4 = Buf("XS4")
        MX = sb("MX", [128, 2, D], F32); bMX = Buf("MX")
        MXS = sb("MXS", [128, 2, D], BF16); bMXS = Buf("MXS")
        MHT = sb("MHT", [128, NCH, MEM], BF16); bMHT = Buf("MHT")
        KXs = [sb("KX%d" % i, [128, NCH, MEM], BF16) for i in range(2)]; bKXs = [Buf("KX%d" % i) for i in range(2)]
        VXs = [sb("VX%d" % i, [128, 2, D], BF16) for i in range(2)]; bVXs = [Buf("VX%d" % i) for i in range(2)]
        QX = sb("QX", [128, NCH, 512], BF16); bQX = Buf("QX")
        OT = sb("OT", [128, NCH, 512], BF16); bOT = Buf("OT")
        PT = [sb("PT%d" % i, [128, 512], BF16) for i in range(8)]; bPT = [Buf("PT%d" % i) for i in range(8)]
        RL = [sb("RL%d" % i, [128, 512], F32) for i in range(2)]; bRL = [Buf("RL%d" % i) for i in range(2)]
        RLs = sb("RLs", [128, 512], F32); bRLs = Buf("RLs")
        wkv = W["w_xkv"]

        def mem_kv(b):
            KX, bKX, VX, bVX = KXs[b % 2], bKXs[b % 2], VXs[b % 2], bVXs[b % 2]
            s.dma("sp", MX[:], self.mem[b * MEM:(b + 1) * MEM, :].rearrange("(t p) d -> p t d", p=128), load=bMX)
            self.norm_scale4(nrm, lambda t: MX[:, t, :], bMX, gmb, CON, 2, MXS, bMXS)
            self.norm_tr4(MXS, bMXS, 2, MHT, bMHT, ident_b, CON)
            for q4 in range(4):
                s.dma("sp", WT[:].rearrange("p c n -> p (c n)"), self.s_wkv[q4], load=bWT)
                if q4 < 2:
                    for jj in range(4):
                        j = q4 * 4 + jj
                        bk = 1 + (jj % 2)
                        s.op("pe", lambda e, jj=jj, bk=bk: self.mm_group(e, self.bank(bk)[:, 0:MEM], [(WT[:, c, jj * 128:(jj + 1) * 128], MHT[:, c, :]) for c in range(NCH)]),
                             reads=[bWT, bMHT], writes=[pb[bk]])
                        s.op("act", lambda e, j=j, bk=bk: e.copy(out=KX[:, j, :], in_=self.bank(bk)[:, 0:MEM]), reads=[pb[bk]], writes=[bKX])
                else:
                    half = q4 - 2
                    for mc in range(2):
                        bk = 1 + mc
                        s.op("pe", lambda e, mc=mc, bk=bk: self.mm_group(e, self.bank(bk), [(MHT[:, c, mc * 128:(mc + 1) * 128], WT[:, c, :]) for c in range(NCH)]),
                             reads=[bWT, bMHT], writes=[pb[bk]])
                        s.op("act", lambda e, mc=mc, bk=bk, half=half: e.copy(out=VX[:, mc, half * 512:(half + 1) * 512], in_=self.bank(bk)),
                             reads=[pb[bk]], writes=[bVX])

        def pro_load(blk):
            tok0 = blk * 512
            s.dma("sp", Xs[blk % 2][:], self.h2[tok0:tok0 + 512, :].rearrange("(t p) d -> p t d", p=128), load=bXs[blk % 2])

        def pro_scale(blk):
            X = Xs[blk % 2]
            self.norm_scale4(nrm, lambda t: X[:, t, :], bXs[blk % 2], g2b, CON, 4, XS4, bXS4)

        def pro_T(blk):
            self.norm_tr4(XS4, bXS4, 4, HTs[blk % 2], bHTs[blk % 2], ident_b, CON, banks=(0, 3))

        def ffn_scale(blk):
            X = Xs[blk % 2]
            self.norm_scale4(nrm3, lambda t: X[:, t, :], bXs[blk % 2], g3b, CON, 4, XS3, bXS3)
            s.dma("sp", self.hn3[blk * 512:(blk + 1) * 512, :].rearrange("(t p) d -> p t d", p=128), XS3[:], store=bXS3)

        def router_part(blk):
            self.norm_tr4(XS3, bXS3, 4, XT3, bXT3, ident_b, CON, banks=(0, 6))
            def rl(e):
                ins = None
                for t in range(4):
                    for c in range(NCH):
                        ins = e.matmul(out=self.bank(2)[:, t * 36:(t + 1) * 36], lhsT=XT3[:, c, t * 128:(t + 1) * 128], rhs=wr[:, c, :],
                                       start=(c == 0), stop=(c == NCH - 1))
                return ins
            s.op("pe", rl, reads=[bXT3, CON], writes=[pb[2]])
            s.op("dve", lambda e: e.tensor_tensor(out=L4[:], in0=self.bank(2)[:, 0:144].rearrange("p (t n) -> p t n", t=4),
                                                  in1=bb3[:].unsqueeze(1).broadcast_to([128, 4, 36]), op=ALU.add), reads=[pb[2], CON], writes=[bL4])
            s.dma("sp", self.lscr[blk * 512:(blk + 1) * 512, :].rearrange("(t p) n -> p t n", p=128), L4[:], store=bL4)

        mem_kv(0)
        pro_load(0); pro_scale(0); pro_T(0)
        for blk in range(NBLK):
            tok0 = blk * 512
            b = blk // BPS
            X, bX, HT, bHT = Xs[blk % 2], bXs[blk % 2], HTs[blk % 2], bHTs[blk % 2]
            KX, bKX, VX, bVX = KXs[b % 2], bKXs[b % 2], VXs[b % 2], bVXs[b % 2]
            if blk + 1 < NBLK:
                pro_load(blk + 1)
            for j in range(NCH):
                bk = 1 + (j % 2)
                s.op("pe", lambda e, j=j, bk=bk: self.mm_group(e, self.bank(bk), [(WXQ[:, c, j * 128:(j + 1) * 128], HT[:, c, :]) for c in range(NCH)]),
                     reads=[bHT, CON, CONW], writes=[pb[bk]])
                if j % 2 == 0:
                    s.op("act", lambda e, j=j, bk=bk: e.copy(out=QX[:, j, :], in_=self.bank(bk)), reads=[pb[bk]], writes=[bQX])
                else:
                    s.op("dve", lambda e, j=j, bk=bk: e.tensor_copy(out=QX[:, j, :], in_=self.bank(bk)), reads=[pb[bk]], writes=[bQX])
            if blk > 0:
                router_part(blk - 1)
            if blk + 1 < NBLK:
                if (blk + 1) % BPS == 0:
                    mem_kv((blk + 1) // BPS)
                pro_scale(blk + 1)
            for h in range(MH):
                for mc in range(2):
                    bs = 3 + mc
                    pi = h * 2 + mc
                    s.op("pe", lambda e: self.mm_group(e, self.bank(bs), [(KX[:, h * 2 + dh, mc * 128:(mc + 1) * 128], QX[:, h * 2 + dh, :]) for dh in range(2)]),
                         reads=[bKX, bQX], writes=[pb[bs]])
                    s.op("act", lambda e: e.activation(out=PT[pi][:], in_=self.bank(bs), func=AF.Exp, scale=1.0 / math.sqrt(MHD)),
                         reads=[pb[bs]], writes=[bPT[pi]])
            for h in range(MH):
                hp = h % 2
                s.op("pe", lambda e: self.mm_group(e, self.bank(5), [(ones_b[:], PT[h * 2 + mc][:]) for mc in range(2)]),
                     reads=[bPT[h * 2], bPT[h * 2 + 1], CON], writes=[pb[5]])
                s.op("act", lambda e: e.activation(out=RLs[:], in_=self.bank(5), func=AF.Ln), reads=[pb[5]], writes=[bRLs])
                s.op("act", lambda e: e.activation(out=RL[hp][:], in_=RLs[:], func=AF.Exp, scale=-1.0), reads=[bRLs], writes=[bRL[hp]])
                for dvh in range(2):
                    bo = 6 + dvh
                    s.op("pe", lambda e: self.mm_group(e, self.bank(bo), [(VX[:, mc, h * 256 + dvh * 128:h * 256 + (dvh + 1) * 128], PT[h * 2 + mc][:]) for mc in range(2)]),
                         reads=[bVX, bPT[h * 2], bPT[h * 2 + 1]], writes=[pb[bo]])
                    s.op("dve", lambda e: e.tensor_tensor(out=OT[:, h * 2 + dvh, :], in0=RL[hp][:], in1=self.bank(bo), op=ALU.mult),
                         reads=[bRL[hp], pb[bo]], writes=[bOT])
            if blk + 1 < NBLK:
                pro_T(blk + 1)
            for t in range(4):
                for dh in range(2):
                    bd = 1 + (t * 2 + dh) % 2
                    s.op("pe", lambda e, t=t, dh=dh, bd=bd: self.mm_group(e, self.bank(bd), [(OT[:, c, t * 128:(t + 1) * 128], WXO[:, c, dh * 512:(dh + 1) * 512]) for c in range(NCH)]),
                         reads=[bOT, CON, CONW], writes=[pb[bd]])
                    s.op("dve", lambda e, t=t, dh=dh, bd=bd: e.tensor_tensor(out=X[:, t, dh * 512:(dh + 1) * 512], in0=X[:, t, dh * 512:(dh + 1) * 512],
                                                                            in1=self.bank(bd), op=ALU.add),
                         reads=[bX, pb[bd]], writes=[bX])
            s.dma("sp", self.h2[tok0:tok0 + 512, :].rearrange("(t p) d -> p t d", p=128), X[:], store=bX)
            ffn_scale(blk)
        router_part(NBLK - 1)

    def prep_dense_scratch(self):
        s, W = self.s, self.w
        b = Buf("DSCR")
        v = lambda ap, c: ap.rearrange("p (c n) -> p c n", c=c)
        win_v = W["w_in"].rearrange("(c p) n -> p c n", p=128)
        for half in range(2):
            s.dma("pool", v(self.s_win2, NCH)[:, :, half * 1280:(half + 1) * 1280], win_v[:, :, C_POOL + half * 1280:C_POOL + (half + 1) * 1280], load=b)
        s.dma("pool", v(self.s_wab, 4), W["w_attn_branch"].rearrange("(c p) n -> p c n", p=128), load=b)
        s.dma("pool", v(self.s_pw, 4), W["pool_w"].rearrange("g c e -> c g e"), load=b)
        s.dma("pool", v(self.s_wpb, 4), W["w_pool_branch"].rearrange("(c p) n -> p c n", p=128), load=b)
        s.dma("pool", v(self.s_wmo, NCH), W["w_mix_out"].rearrange("(c p) n -> p c n", p=128), load=b)
        s.dma("pool", v(self.s_wxq, NCH), W["w_xq"].rearrange("(c p) n -> p c n", p=128), load=b)
        s.dma("pool", v(self.s_wxo, NCH), W["w_xo"].rearrange("(c p) n -> p c n", p=128), load=b)
        kv_v = W["w_xkv"].rearrange("(c p) n -> p c n", p=128)
        for q4 in range(4):
            s.dma("pool", v(self.s_wkv[q4], NCH), kv_v[:, :, q4 * 512:(q4 + 1) * 512], load=b)

    def prep_expert_scratch(self):
        s = self.s
        self.bWSCR = Buf("WSCR")
        for e in range(NE):
            rows = slice(e * 128, (e + 1) * 128)
            s.dma("pool", self.wgs[rows, :].rearrange("p (c f) -> p c f", c=NCH), self.w["w_exp_gate"][e].rearrange("(c p) f -> p c f", p=128), load=self.bWSCR)
            s.dma("pool", self.wus[rows, :].rearrange("p (c f) -> p c f", c=NCH), self.w["w_exp_up"][e].rearrange("(c p) f -> p c f", p=128), load=self.bWSCR)
            s.dma("pool", self.wds[rows, :].rearrange("p (c d) -> p c d", c=2), self.w["w_exp_down"][e].rearrange("(c p) d -> p c d", p=128), load=self.bWSCR)

    def phase3(self, ps):
        nc, s, pb = self.nc, self.s, self.pb
        sb = lambda n, sh, d=F32: ps.enter_context(nc.sbuf_tensor(self.uname(n), list(sh), d))
        NT = self.NTOK // 128
        NBK = self.NBK3
        TM = min(1024, self.NTOK)
        NMT = self.NTOK // TM
        NTT = TM // 128
        MAGIC = 12582912.0
        CON = Buf("con3")
        ident_b = sb("ident_b", [128, 128], BF16)
        ones_b = sb("ones_b", [128, 128], BF16)
        tri_b = sb("tri_b", [128, 128], BF16)
        kb = sb("kb", [128, NBK], F32)
        ep = sb("ep", [128, 8], F32)
        ones64 = sb("ones64", [128, max(NT, 32)], F32)
        g3b = sb("g3b", [128, D], F32)
        gfb = sb("gfb", [128, D], F32)
        wr = sb("wr", [128, NCH, 36], BF16)
        bb = sb("bb", [128, 36], F32)
        s.dma("pool", ident_b[:], self.c_ident, load=CON)
        s.dma("pool", ones_b[:], self.c_ones, load=CON)
        s.dma("pool", tri_b[:], self.c_tri, load=CON)
        s.dma("sp", kb[:], self.c_kb[:, 0:NBK], load=CON)
        s.dma("sp", ep[:], self.c_ep, load=CON)
        s.dma("sp", g3b[:], self.w["ffn_norm_g"].partition_broadcast(128), load=CON)
        s.dma("sp", gfb[:], self.w["final_norm_g"].partition_broadcast(128), load=CON)
        s.dma("pool", wr[:], self.w["w_router"].rearrange("(c p) n -> p c n", p=128), load=CON)
        s.dma("sp", bb[:], self.w["b_router"].partition_broadcast(128), load=CON)
        s.op("dve", lambda e: e.memset(ones64[:], 1.0), writes=[CON])
        OHb = [sb("OHb%d" % k, [128, NT, 32], BF16) for k in range(2)]; bOHb = [Buf("OHb%d" % k) for k in range(2)]
        W12 = sb("W12", [128, 2, NT], F32); bW12 = Buf("W12")
        DEST = sb("DEST", [128, 2, NT], I32); bDEST = Buf("DEST")
        IDXW = sb("IDXW", [128, NBK], I32); bIDXW = Buf("IDXW")
        nrm = self.mk_norm(sb, "3", xs=False)

        L = sb("L", [128, NT, 36], F32); bL = Buf("L")
        s.dma("sp", L[:], self.lscr.rearrange("(t p) n -> p t n", p=128), load=bL)
        with ExitStack() as p1b:
            sb1 = lambda n, sh, d=F32: p1b.enter_context(nc.sbuf_tensor(self.uname(n), list(sh), d))
            rr = [sb1("r%d" % i, [128, NT], F32) for i in range(6)]
            br = [Buf("r%d" % i) for i in range(6)]
            r1, r2, r3, r4, r5 = rr[1], rr[2], rr[3], rr[4], rr[5]
            dg = sb1("dg", [128, NT, 4], F32); bdg = Buf("dg")
            eg = sb1("eg", [128, NT, 4], F32); beg = Buf("eg")
            oh = sb1("oh", [128, NT, 4], F32); boh = Buf("oh")
            tmp = sb1("tmp", [128, NT, 4, 8], F32); btmp = Buf("tmp")
            sl = sb1("sl", [128, NT, 8], F32); bsl = Buf("sl")
            sl2 = sb1("sl2", [128, NT, 8], F32); bsl2 = Buf("sl2")
            eq1 = sb1("eq1", [128, NT, 8], F32); beq1 = Buf("eq1")
            eq2 = sb1("eq2", [128, NT, 8], F32); beq2 = Buf("eq2")
            Lg = L[:, :, 0:4]
            Le = L[:, :, 4:36].rearrange("p t (g e) -> p t g e", g=4)
            s.op("dve", lambda e: e.tensor_reduce(out=r1[:], in_=Lg, axis=AX.X, op=ALU.max), reads=[bL], writes=[br[1]])
            s.op("dve", lambda e: e.tensor_tensor(out=dg[:], in0=Lg, in1=_bc_last(r1[:], 4), op=ALU.subtract), reads=[bL, br[1]], writes=[bdg])
            s.op("act", lambda e: e.activation(out=eg[:], in_=dg[:], func=AF.Exp), reads=[bdg], writes=[beg])
            s.op("dve", lambda e: e.tensor_reduce(out=r2[:], in_=eg[:], axis=AX.X, op=ALU.add), reads=[beg], writes=[br[2]])
            s.op("dve", lambda e: e.reciprocal(out=r2[:], in_=r2[:]), reads=[br[2]], writes=[br[2]])
            s.op("dve", lambda e: e.tensor_single_scalar(out=oh[:], in_=dg[:], scalar=0.0, op=ALU.is_ge), reads=[bdg], writes=[boh])
            s.op("dve", lambda e: e.tensor_tensor(out=tmp[:], in0=Le, in1=_bc_last(oh[:], 8), op=ALU.mult), reads=[bL, boh], writes=[btmp])
            s.op("dve", lambda e: e.tensor_reduce(out=sl[:], in_=tmp[:].rearrange("p t g e -> p t e g"), axis=AX.X, op=ALU.add), reads=[btmp], writes=[bsl])
            s.op("dve", lambda e: e.tensor_reduce(out=r3[:], in_=sl[:], axis=AX.X, op=ALU.max), reads=[bsl], writes=[br[3]])
            s.op("dve", lambda e: e.tensor_tensor(out=eq1[:], in0=sl[:], in1=_bc_last(r3[:], 8), op=ALU.is_equal), reads=[bsl, br[3]], writes=[beq1])
            s.op("dve", lambda e: e.scalar_tensor_tensor(out=sl2[:], in0=eq1[:], scalar=-1e30, in1=sl[:], op0=ALU.mult, op1=ALU.add), reads=[beq1, bsl], writes=[bsl2])
            s.op("dve", lambda e: e.tensor_reduce(out=r4[:], in_=sl2[:], axis=AX.X, op=ALU.max), reads=[bsl2], writes=[br[4]])
            s.op("dve", lambda e: e.tensor_tensor(out=eq2[:], in0=sl2[:], in1=_bc_last(r4[:], 8), op=ALU.is_equal), reads=[bsl2, br[4]], writes=[beq2])
            for k, (eq, beq) in enumerate([(eq1, beq1), (eq2, beq2)]):
                s.op("dve", lambda e: e.tensor_tensor(out=OHb[k][:].rearrange("p t (g e) -> p t g e", g=4), in0=_bc_last(oh[:], 8),
                                                      in1=eq[:].unsqueeze(2).broadcast_to([128, NT, 4, 8]), op=ALU.mult), reads=[boh, beq], writes=[bOHb[k]])
            s.op("dve", lambda e: e.tensor_tensor(out=r5[:], in0=r4[:], in1=r3[:], op=ALU.subtract), reads=[br[3], br[4]], writes=[br[5]])
            s.op("act", lambda e: e.activation(out=r5[:], in_=r5[:], func=AF.Exp), reads=[br[5]], writes=[br[5]])
            s.op("dve", lambda e: e.tensor_scalar(out=r3[:], in0=r5[:], scalar1=1.0, scalar2=None, op0=ALU.add), reads=[br[5]], writes=[br[3]])
            s.op("dve", lambda e: e.reciprocal(out=r3[:], in_=r3[:]), reads=[br[3]], writes=[br[3]])
            s.op("dve", lambda e: e.tensor_tensor(out=W12[:, 0, :], in0=r3[:], in1=r2[:], op=ALU.mult), reads=[br[3], br[2]], writes=[bW12])
            s.op("dve", lambda e: e.tensor_tensor(out=W12[:, 1, :], in0=W12[:, 0, :], in1=r5[:], op=ALU.mult), reads=[bW12, br[5]], writes=[bW12])
            NC32 = NT * 32
            PW = [sb1("PW%d" % k, [128, NT, 32], F32) for k in range(2)]; bPW = [Buf("PW%d" % k) for k in range(2)]
            TOT = [sb1("TOT%d" % k, [128, NT, 32], F32) for k in range(2)]; bTOT = [Buf("TOT%d" % k) for k in range(2)]
            INC = [sb1("INC%d" % k, [128, 32, NT], F32) for k in range(2)]; bINC = [Buf("INC%d" % k) for k in range(2)]
            CN = sb1("CN", [128, 32], F32); bCN = Buf("CN")
            PC = sb1("PC", [128, 32], F32); bPC = Buf("PC")
            BINC = sb1("BINC", [128, 32], F32); bBINC = Buf("BINC")
            BASE = sb1("BASE", [128, 2, 32], F32); bBASE = Buf("BASE")
            DF = sb1("DF", [128, 2, NT], F32); bDF = Buf("DF")
            CMP = sb1("CMP", [128, NBK, 31], F32); bCMP = Buf("CMP")
            EK = sb1("EK", [128, NBK], F32); bEK = Buf("EK")
            for k in range(2):
                flat = OHb[k][:].rearrange("p t e -> p (t e)")
                for q in range(0, NC32, 512):
                    w_ = min(512, NC32 - q)
                    bk = 4 + (q // 512) % 2
                    s.op("pe", lambda e: e.matmul(out=self.bank(bk)[:, 0:w_], lhsT=tri_b[:], rhs=flat[:, q:q + w_], start=True, stop=True), reads=[bOHb[k], CON], writes=[pb[bk]])
                    s.op("act", lambda e: e.copy(out=PW[k][:].rearrange("p t e -> p (t e)")[:, q:q + w_], in_=self.bank(bk)[:, 0:w_]), reads=[pb[bk]], writes=[bPW[k]])
                    bk2 = 6 + (q // 512) % 2
                    s.op("pe", lambda e: e.matmul(out=self.bank(bk2)[:, 0:w_], lhsT=ones_b[:], rhs=flat[:, q:q + w_], start=True, stop=True), reads=[bOHb[k], CON], writes=[pb[bk2]])
                    s.op("dve", lambda e: e.tensor_copy(out=TOT[k][:].rearrange("p t e -> p (t e)")[:, q:q + w_], in_=self.bank(bk2)[:, 0:w_]), reads=[pb[bk2]], writes=[bTOT[k]])
                for ex in range(32):
                    s.op("dve", lambda e: e.tensor_tensor_scan(out=INC[k][:, ex, :], data0=ones64[:, 0:NT], data1=TOT[k][:, :, ex], initial=0.0, op0=ALU.mult, op1=ALU.add),
                         reads=[bTOT[k], CON], writes=[bINC[k]])
            s.op("dve", lambda e: e.tensor_tensor(out=CN[:], in0=INC[0][:, :, NT - 1], in1=INC[1][:, :, NT - 1], op=ALU.add), reads=[bINC[0], bINC[1]], writes=[bCN])
            s.op("dve", lambda e: e.tensor_scalar(out=PC[:], in0=CN[:], scalar1=511.0, scalar2=1.0 / 512, op0=ALU.add, op1=ALU.mult), reads=[bCN], writes=[bPC])
            s.op("dve", lambda e: e.tensor_scalar(out=PC[:], in0=PC[:], scalar1=-0.5 + 1.0 / 2048, scalar2=None, op0=ALU.add), reads=[bPC], writes=[bPC])
            s.op("dve", lambda e: e.tensor_scalar(out=PC[:], in0=PC[:], scalar1=MAGIC, scalar2=None, op0=ALU.add), reads=[bPC], writes=[bPC])
            s.op("dve", lambda e: e.tensor_scalar(out=PC[:], in0=PC[:], scalar1=-MAGIC, scalar2=512.0, op0=ALU.add, op1=ALU.mult), reads=[bPC], writes=[bPC])
            s.op("dve", lambda e: e.tensor_tensor_scan(out=BINC[:], data0=ones64[:, 0:32], data1=PC[:], initial=0.0, op0=ALU.mult, op1=ALU.add), reads=[bPC, CON], writes=[bBINC])
            s.op("dve", lambda e: e.tensor_tensor(out=BASE[:, 0, :], in0=BINC[:], in1=PC[:], op=ALU.subtract), reads=[bBINC, bPC], writes=[bBASE])
            s.op("dve", lambda e: e.tensor_tensor(out=BASE[:, 1, :], in0=BASE[:, 0, :], in1=INC[0][:, :, NT - 1], op=ALU.add), reads=[bBASE, bINC[0]], writes=[bBASE])
            for k in range(2):
                s.op("dve", lambda e: e.tensor_tensor(out=PW[k][:], in0=PW[k][:], in1=INC[k][:].rearrange("p e t -> p t e"), op=ALU.add), reads=[bPW[k], bINC[k]], writes=[bPW[k]])
                s.op("dve", lambda e: e.tensor_tensor(out=PW[k][:], in0=PW[k][:], in1=TOT[k][:], op=ALU.subtract), reads=[bPW[k], bTOT[k]], writes=[bPW[k]])
                s.op("dve", lambda e: e.tensor_tensor(out=PW[k][:], in0=PW[k][:], in1=BASE[:, k, :].unsqueeze(1).broadcast_to([128, NT, 32]), op=ALU.add), reads=[bPW[k], bBASE], writes=[bPW[k]])
                s.op("dve", lambda e: e.tensor_tensor(out=PW[k][:], in0=PW[k][:], in1=OHb[k][:], op=ALU.mult), reads=[bPW[k], bOHb[k]], writes=[bPW[k]])
                s.op("dve", lambda e: e.tensor_reduce(out=DF[:, k, :], in_=PW[k][:], axis=AX.X, op=ALU.add), reads=[bPW[k]], writes=[bDF])
            s.op("dve", lambda e: e.tensor_copy(out=DEST[:], in_=DF[:]), reads=[bDF], writes=[bDEST])
            s.op("dve", lambda e: e.tensor_tensor(out=CMP[:], in0=_bc_last(kb[:], 31), in1=BINC[:, 0:31].unsqueeze(1).broadcast_to([128, NBK, 31]), op=ALU.is_ge),
                 reads=[bBINC, CON], writes=[bCMP])
            s.op("dve", lambda e: e.tensor_reduce(out=EK[:], in_=CMP[:], axis=AX.X, op=ALU.add), reads=[bCMP], writes=[bEK])
            s.op("dve", lambda e: e.tensor_scalar(out=EK[:], in0=EK[:], scalar1=128.0, scalar2=ep[:, 0:1], op0=ALU.mult, op1=ALU.add), reads=[bEK, CON], writes=[bEK])
            s.op("dve", lambda e: e.tensor_copy(out=IDXW[:], in_=EK[:]), reads=[bEK], writes=[bIDXW])
            NXR = 8
            XR = [sb1("XR%d" % i, [128, D + 8], BF16) for i in range(NXR)]; bXR = [Buf("XR%d" % i) for i in range(NXR)]
            s.barrier(["sp", "pool"])
            for t in range(NT):
                ia, ib = (2 * t) % NXR, (2 * t + 1) % NXR
                s.dma("sp", XR[ia][:, 0:D], self.hn3[t * 128:(t + 1) * 128, :], load=bXR[ia])
                s.op("dve", lambda e: e.tensor_copy(out=XR[ib][:, 0:D], in_=XR[ia][:, 0:D]), reads=[bXR[ia]], writes=[bXR[ib]])
                for k, i in ((0, ia), (1, ib)):
                    s.op("dve", lambda e: e.tensor_copy(out=XR[i][:, D:D + 1], in_=W12[:, k, t:t + 1]), reads=[bW12], writes=[bXR[i]])
                    s.idma(self.sortx, XR[i][:], DEST[:, k, t:t + 1], scatter=True, buf=bXR[i], idxbuf=bDEST)
            s.barrier()
        with ExitStack() as p2:
            sb2 = lambda n, sh, d=F32: p2.enter_context(nc.sbuf_tensor(self.uname(n), list(sh), d))
            XSS = [sb2("XSS%d" % i, [128, 4, D + 8], BF16) for i in range(3)]; bXSS = [Buf("XSS%d" % i) for i in range(3)]
            XT = [sb2("XT%d" % i, [128, NCH, 512], BF16) for i in range(2)]; bXT = [Buf("XT%d" % i) for i in range(2)]
            CWT = [sb2("CWT%d" % i, [1, 512], BF16) for i in range(2)]; bCWT = [Buf("CWT%d" % i) for i in range(2)]
            WG = [sb2("WG%d" % i, [128, NCH * FF], BF16) for i in range(3)]; bWG = [Buf("WG%d" % i) for i in range(3)]
            WU = [sb2("WU%d" % i, [128, NCH * FF], BF16) for i in range(3)]; bWU = [Buf("WU%d" % i) for i in range(3)]
            WD = [sb2("WD%d" % i, [128, 2, D], BF16) for i in range(3)]; bWD = [Buf("WD%d" % i) for i in range(3)]
            HID = [sb2("HID%d" % i, [128, 2, 512], BF16) for i in range(2)]; bHID = [Buf("HID%d" % i) for i in range(2)]
            SG = [sb2("SG%d" % i, [128, 512], BF16) for i in range(2)]; bSG = [Buf("SG%d" % i) for i in range(2)]
            T1 = [sb2("T1%d" % i, [128, 512], BF16) for i in range(2)]; bT1 = [Buf("T1%d" % i) for i in range(2)]
            MO = [sb2("MO%d" % i, [128, 4, D], F32) for i in range(2)]; bMO = [Buf("MO%d" % i) for i in range(2)]

            def load_blk(k):
                i = k % 3
                s.dma("sp", XSS[i][:], self.sortx[k * 512:(k + 1) * 512, :].rearrange("(t p) c -> p t c", p=128), load=bXSS[i])
                s.idma(WG[i][:], self.wgs, IDXW[:, k:k + 1], scatter=False, buf=bWG[i], idxbuf=bIDXW)
                s.idma(WU[i][:], self.wus, IDXW[:, k:k + 1], scatter=False, buf=bWU[i], idxbuf=bIDXW)
                s.idma(WD[i][:].rearrange("p c d -> p (c d)"), self.wds, IDXW[:, k:k + 1], scatter=False, buf=bWD[i], idxbuf=bIDXW)

            def front(k):
                i = k % 2
                w3 = k % 3
                def trcw(e):
                    ins = None
                    for t in range(4):
                        ins = e.transpose(out=self.bank(1, BF16)[0:1, t * 128:(t + 1) * 128], in_=XSS[w3][:, t, D:D + 1], identity=ident_b[:])
                    return ins
                s.op("pe", trcw, reads=[bXSS[w3], CON], writes=[pb[1]])
                s.op("act", lambda e: e.copy(out=CWT[i][:], in_=self.bank(1, BF16)[0:1, 0:512]), reads=[pb[1]], writes=[bCWT[i]])
                for t in range(4):
                    tb = t % 2
                    def tr(e):
                        ins = None
                        for c in range(NCH):
                            ins = e.transpose(out=self.bank(tb, BF16)[:, c * 128:(c + 1) * 128], in_=XSS[w3][:, t, c * 128:(c + 1) * 128], identity=ident_b[:])
                        return ins
                    s.op("pe", tr, reads=[bXSS[w3], CON], writes=[pb[tb]])
                    if t % 2 == 0:
                        s.op("act", lambda e: e.copy(out=XT[i][:, :, t * 128:(t + 1) * 128], in_=self.bank(tb, BF16).rearrange("p (c t) -> p c t", c=NCH)), reads=[pb[tb]], writes=[bXT[i]])
                    else:
                        s.op("dve", lambda e: e.tensor_copy(out=XT[i][:, :, t * 128:(t + 1) * 128], in_=self.bank(tb, BF16).rearrange("p (c t) -> p c t", c=NCH)), reads=[pb[tb]], writes=[bXT[i]])
                for fc in range(2):
                    bg, bu = 4 + fc, 6 + fc
                    s.op("pe", lambda e: self.mm_group(e, self.bank(bg), [(WG[w3][:, c * FF + fc * 128:c * FF + (fc + 1) * 128], XT[i][:, c, :]) for c in range(NCH)]),
                         reads=[bWG[w3], bXT[i]], writes=[pb[bg]])
                    s.op("pe", lambda e: self.mm_group(e, self.bank(bu), [(WU[w3][:, c * FF + fc * 128:c * FF + (fc + 1) * 128], XT[i][:, c, :]) for c in range(NCH)]),
                         reads=[bWU[w3], bXT[i]], writes=[pb[bu]])
                    if fc == 0:
                        s.op("pe", lambda e: e.matmul(out=self.bank(1), lhsT=ones_b[0:1, :], rhs=CWT[i][:], start=True, stop=True), reads=[bCWT[i], CON], writes=[pb[1]])
                    s.op("act", lambda e: e.activation(out=SG[fc][:], in_=self.bank(bg), func=AF.Silu), reads=[pb[bg]], writes=[bSG[fc]])
                    s.op("dve", lambda e: e.tensor_tensor(out=T1[fc][:], in0=SG[fc][:], in1=self.bank(bu), op=ALU.mult), reads=[bSG[fc], pb[bu]], writes=[bT1[fc]])
                    s.op("dve", lambda e: e.tensor_tensor(out=HID[i][:, fc, :], in0=T1[fc][:], in1=self.bank(1), op=ALU.mult), reads=[bT1[fc], pb[1]], writes=[bHID[i]])

            def back(k):
                i = k % 2
                w3 = k % 3
                n = 0
                for t in range(4):
                    for dh in range(2):
                        bd = 2 + (n % 2)
                        s.op("pe", lambda e: self.mm_group(e, self.bank(bd), [(HID[i][:, fc, t * 128:(t + 1) * 128], WD[w3][:, fc, dh * 512:(dh + 1) * 512]) for fc in range(2)]),
                             reads=[bHID[i], bWD[w3]], writes=[pb[bd]])
                        dst = MO[i][:, t, dh * 512:(dh + 1) * 512]
                        if n % 2 == 0:
                            s.op("act", lambda e: e.copy(out=dst, in_=self.bank(bd)), reads=[pb[bd]], writes=[bMO[i]])
                        else:
                            s.op("dve", lambda e: e.tensor_copy(out=dst, in_=self.bank(bd)), reads=[pb[bd]], writes=[bMO[i]])
                        n += 1
                s.dma("sp", self.sortout[k * 512:(k + 1) * 512, :].rearrange("(t p) d -> p t d", p=128), MO[i][:], store=bMO[i])

            for k0 in range(min(3, NBK)):
                load_blk(k0)
            front(0)
            for k in range(NBK):
                if k + 1 < NBK:
                    front(k + 1)
                back(k)
                if k + 3 < NBK:
                    load_blk(k + 3)
            s.barrier()
        with ExitStack() as p3:
            sb3 = lambda n, sh, d=F32: p3.enter_context(nc.sbuf_tensor(self.uname(n), list(sh), d))
            NB4 = 6
            G1 = [sb3("G1%d" % i, [128, D], F32) for i in range(NB4)]; bG1 = [Buf("G1%d" % i) for i in range(NB4)]
            G2 = [sb3("G2%d" % i, [128, D], F32) for i in range(NB4)]; bG2 = [Buf("G2%d" % i) for i in range(NB4)]
            Hh = [sb3("Hh%d" % i, [128, D], F32) for i in range(NB4)]; bHh = [Buf("Hh%d" % i) for i in range(NB4)]
            sq = [sb3("sq%d" % i, [128, 1], F32) for i in range(NB4)]; bsq = [Buf("sq%d" % i) for i in range(NB4)]

            def fetch(t):
                k = t % NB4
                s.idma(G1[k][:], self.sortout, DEST[:, 0, t:t + 1], scatter=False, buf=bG1[k], idxbuf=bDEST)
                s.idma(G2[k][:], self.sortout, DEST[:, 1, t:t + 1], scatter=False, buf=bG2[k], idxbuf=bDEST)
                s.dma("sp", Hh[k][:], self.h2[t * 128:(t + 1) * 128, :], load=bHh[k])

            for t in range(min(NB4 - 1, NT)):
                fetch(t)
            for t in range(NT):
                k = t % NB4
                if t + NB4 - 1 < NT:
                    fetch(t + NB4 - 1)
                s.op("dve", lambda e: e.tensor_tensor(out=G1[k][:], in0=G1[k][:], in1=G2[k][:], op=ALU.add), reads=[bG1[k], bG2[k]], writes=[bG1[k]])
                s.op("dve", lambda e: e.tensor_tensor(out=Hh[k][:], in0=Hh[k][:], in1=G1[k][:], op=ALU.add), reads=[bHh[k], bG1[k]], writes=[bHh[k]])
                s.op("act", lambda e: e.activation(out=G2[k][:], in_=Hh[k][:], func=AF.Square, accum_out=sq[k][:]), reads=[bHh[k]], writes=[bG2[k], bsq[k]])
                s.op("act", lambda e: e.activation(out=sq[k][:], in_=sq[k][:], func=AF.Ln, scale=1.0 / D, bias=nrm["epsc"][:]), reads=[bsq[k]], writes=[bsq[k]])
                s.op("act", lambda e: e.activation(out=sq[k][:], in_=sq[k][:], func=AF.Exp, scale=-0.5), reads=[bsq[k]], writes=[bsq[k]])
                s.op("dve", lambda e: e.scalar_tensor_tensor(out=G1[k][:], in0=Hh[k][:], scalar=sq[k][:], in1=gfb[:], op0=ALU.mult, op1=ALU.mult),
                     reads=[bHh[k], bsq[k], CON], writes=[bG1[k]])
                s.dma("sp", self.out[t * 128:(t + 1) * 128, :], G1[k][:], store=bG1[k])

    def phase3_group(self, ps):
        nc, s, pb = self.nc, self.s, self.pb
        sb = lambda n, sh, d=F32: ps.enter_context(nc.sbuf_tensor(self.uname(n), list(sh), d))
        NT = self.NTOK // 128
        NBK = self.NTOK // 512 + 4
        TM = min(1024, self.NTOK)
        NMT = self.NTOK // TM
        NTT = TM // 128
        MAGIC = 12582912.0
        CON = Buf("con3")
        ident_b = sb("ident_b", [128, 128], BF16)
        ones_b = sb("ones_b", [128, 128], BF16)
        tri_b = sb("tri_b", [128, 128], BF16)
        sel = sb("sel", [8, 8 * 128], BF16)
        kb = sb("kb", [128, NBK], F32)
        ep = sb("ep", [128, 8], F32)
        ones64 = sb("ones64", [128, NT], F32)
        g3b = sb("g3b", [128, D], F32)
        gfb = sb("gfb", [128, D], F32)
        wr = sb("wr", [128, NCH, 36], BF16)
        bb = sb("bb", [128, NTT, 36], F32)
        s.dma("pool", ident_b[:], self.c_ident, load=CON)
        s.dma("pool", ones_b[:], self.c_ones, load=CON)
        s.dma("pool", tri_b[:], self.c_tri, load=CON)
        s.dma("pool", sel[:], self.c_sel[0:8, 0:1024], load=CON)
        s.dma("sp", kb[:], self.c_kb[:, 0:NBK], load=CON)
        s.dma("sp", ep[:], self.c_ep, load=CON)
        s.dma("sp", g3b[:], self.w["ffn_norm_g"].partition_broadcast(128), load=CON)
        s.dma("sp", gfb[:], self.w["final_norm_g"].partition_broadcast(128), load=CON)
        s.dma("pool", wr[:], self.w["w_router"].rearrange("(c p) n -> p c n", p=128), load=CON)
        for t in range(NTT):
            s.dma("sp", bb[:, t, :], self.w["b_router"].partition_broadcast(128), load=CON)
        s.op("dve", lambda e: e.memset(ones64[:], 1.0), writes=[CON])
        OHall = sb("OHall", [128, NT, 4], F32); bOH = Buf("OHall")
        CW8all = sb("CW8all", [128, NT, 8], BF16); bCW8 = Buf("CW8all")
        DEST = sb("DEST", [128, NT], I32); bDEST = Buf("DEST")
        IDXW = sb("IDXW", [128, NBK * 8], I32); bIDXW = Buf("IDXW")
        nrm = self.mk_norm(sb, "3")

        with ExitStack() as p1:
            sb1 = lambda n, sh, d=F32: p1.enter_context(nc.sbuf_tensor(self.uname(n), list(sh), d))
            H = sb1("H", [128, NTT, D], F32); bH = Buf("H")
            XT = sb1("XT", [128, NCH, TM], BF16); bXT = Buf("XT")
            L = sb1("L", [128, NTT, 36], F32); bL = Buf("L")
            rr = [sb1("r%d" % i, [128, NTT], F32) for i in range(6)]
            br = [Buf("r%d" % i) for i in range(6)]
            r1, r2, r3, r4, r5 = rr[1], rr[2], rr[3], rr[4], rr[5]
            dg = sb1("dg", [128, NTT, 4], F32); bdg = Buf("dg")
            eg = sb1("eg", [128, NTT, 4], F32); beg = Buf("eg")
            tmp = sb1("tmp", [128, NTT, 4, 8], F32); btmp = Buf("tmp")
            sl = sb1("sl", [128, NTT, 8], F32); bsl = Buf("sl")
            sl2 = sb1("sl2", [128, NTT, 8], F32); bsl2 = Buf("sl2")
            eq1 = sb1("eq1", [128, NTT, 8], F32); beq1 = Buf("eq1")
            eq2 = sb1("eq2", [128, NTT, 8], F32); beq2 = Buf("eq2")
            XS, bXS, rstd = nrm["XS"], nrm["bXS"], nrm["rstd"]
            for mt in range(NMT):
                tok0 = mt * TM
                oh = OHall[:, mt * NTT:(mt + 1) * NTT, :]
                s.dma("sp", H[:], self.h2[tok0:tok0 + TM, :].rearrange("(t p) d -> p t d", p=128), load=bH)
                self.rms_stats(nrm, lambda t: H[:, t, :], bH, NTT)
                for t in range(NTT):
                    k = t % 2
                    s.op("dve", lambda e, t=t, k=k: e.scalar_tensor_tensor(out=XS[k][:], in0=H[:, t, :], scalar=rstd[:, t:t + 1],
                                                                           in1=g3b[:], op0=ALU.mult, op1=ALU.mult),
                         reads=[bH, nrm["brstd"], CON], writes=[bXS[k]])
                    s.dma("sp", self.hn3[tok0 + t * 128:tok0 + (t + 1) * 128, :], XS[k][:], store=bXS[k])
                    def tr(e, k=k):
                        ins = None
                        for c in range(NCH):
                            ins = e.transpose(out=self.bank(0, BF16)[:, c * 128:(c + 1) * 128], in_=XS[k][:, c * 128:(c + 1) * 128], identity=ident_b[:])
                        return ins
                    s.op("pe", tr, reads=[bXS[k], CON], writes=[pb[0]])
                    s.op("act", lambda e, t=t: e.copy(out=XT[:, :, t * 128:(t + 1) * 128], in_=self.bank(0, BF16).rearrange("p (c t) -> p c t", c=NCH)),
                         reads=[pb[0]], writes=[bXT])
                def rl(e):
                    ins = None
                    for t in range(NTT):
                        for c in range(NCH):
                            ins = e.matmul(out=self.bank(1)[:, t * 36:(t + 1) * 36], lhsT=XT[:, c, t * 128:(t + 1) * 128], rhs=wr[:, c, :],
                                           start=(c == 0), stop=(c == NCH - 1))
                    return ins
                s.op("pe", rl, reads=[bXT, CON], writes=[pb[1]])
                s.op("dve", lambda e: e.tensor_tensor(out=L[:], in0=self.bank(1)[:, :NTT * 36].rearrange("p (t n) -> p t n", t=NTT), in1=bb[:], op=ALU.add),
                     reads=[pb[1], CON], writes=[bL])
                Lg = L[:, :, 0:4]
                Le = L[:, :, 4:36].rearrange("p t (g e) -> p t g e", g=4)
                s.op("dve", lambda e: e.tensor_reduce(out=r1[:], in_=Lg, axis=AX.X, op=ALU.max), reads=[bL], writes=[br[1]])
                s.op("dve", lambda e: e.tensor_tensor(out=dg[:], in0=Lg, in1=_bc_last(r1[:], 4), op=ALU.subtract), reads=[bL, br[1]], writes=[bdg])
                s.op("act", lambda e: e.activation(out=eg[:], in_=dg[:], func=AF.Exp), reads=[bdg], writes=[beg])
                s.op("dve", lambda e: e.tensor_reduce(out=r2[:], in_=eg[:], axis=AX.X, op=ALU.add), reads=[beg], writes=[br[2]])
                s.op("dve", lambda e: e.reciprocal(out=r2[:], in_=r2[:]), reads=[br[2]], writes=[br[2]])
                s.op("dve", lambda e, oh=oh: e.tensor_single_scalar(out=oh, in_=dg[:], scalar=0.0, op=ALU.is_ge), reads=[bdg], writes=[bOH])
                s.op("dve", lambda e, oh=oh: e.tensor_tensor(out=tmp[:], in0=Le, in1=_bc_last(oh, 8), op=ALU.mult), reads=[bL, bOH], writes=[btmp])
                s.op("dve", lambda e: e.tensor_reduce(out=sl[:], in_=tmp[:].rearrange("p t g e -> p t e g"), axis=AX.X, op=ALU.add), reads=[btmp], writes=[bsl])
                s.op("dve", lambda e: e.tensor_reduce(out=r3[:], in_=sl[:], axis=AX.X, op=ALU.max), reads=[bsl], writes=[br[3]])
                s.op("dve", lambda e: e.tensor_tensor(out=eq1[:], in0=sl[:], in1=_bc_last(r3[:], 8), op=ALU.is_equal), reads=[bsl, br[3]], writes=[beq1])
                s.op("dve", lambda e: e.scalar_tensor_tensor(out=sl2[:], in0=eq1[:], scalar=-1e30, in1=sl[:], op0=ALU.mult, op1=ALU.add),
                     reads=[beq1, bsl], writes=[bsl2])
                s.op("dve", lambda e: e.tensor_reduce(out=r4[:], in_=sl2[:], axis=AX.X, op=ALU.max), reads=[bsl2], writes=[br[4]])
                s.op("dve", lambda e: e.tensor_tensor(out=eq2[:], in0=sl2[:], in1=_bc_last(r4[:], 8), op=ALU.is_equal), reads=[bsl2, br[4]], writes=[beq2])
                s.op("dve", lambda e: e.tensor_tensor(out=r5[:], in0=r4[:], in1=r3[:], op=ALU.subtract), reads=[br[3], br[4]], writes=[br[5]])
                s.op("act", lambda e: e.activation(out=r5[:], in_=r5[:], func=AF.Exp), reads=[br[5]], writes=[br[5]])
                s.op("dve", lambda e: e.tensor_scalar(out=r3[:], in0=r5[:], scalar1=1.0, scalar2=None, op0=ALU.add), reads=[br[5]], writes=[br[3]])
                s.op("dve", lambda e: e.reciprocal(out=r3[:], in_=r3[:]), reads=[br[3]], writes=[br[3]])
                s.op("dve", lambda e: e.tensor_tensor(out=r3[:], in0=r3[:], in1=r2[:], op=ALU.mult), reads=[br[3], br[2]], writes=[br[3]])
                s.op("dve", lambda e: e.tensor_tensor(out=r4[:], in0=r3[:], in1=r5[:], op=ALU.mult), reads=[br[3], br[5]], writes=[br[4]])
                s.op("dve", lambda e: e.tensor_tensor(out=eq1[:], in0=eq1[:], in1=_bc_last(r3[:], 8), op=ALU.mult), reads=[beq1, br[3]], writes=[beq1])
                s.op("dve", lambda e: e.tensor_tensor(out=eq2[:], in0=eq2[:], in1=_bc_last(r4[:], 8), op=ALU.mult), reads=[beq2, br[4]], writes=[beq2])
                s.op("dve", lambda e, mt=mt: e.tensor_tensor(out=CW8all[:, mt * NTT:(mt + 1) * NTT, :], in0=eq1[:], in1=eq2[:], op=ALU.add),
                     reads=[beq1, beq2], writes=[bCW8])
            OHb = sb1("OHb", [128, NT * 4], BF16); bOHb = Buf("OHb")
            TOTs = sb1("TOTs", [128, NT, 4], F32); bTOT = Buf("TOTs")
            INC = sb1("INC", [128, 4, NT], F32); bINC = Buf("INC")
            EXC = sb1("EXC", [128, 4, NT], F32); bEXC = Buf("EXC")
            PC = sb1("PC", [128, 4], F32); bPC = Buf("PC")
            BASE = sb1("BASE", [128, 4], F32); bBASE = Buf("BASE")
            END = sb1("END", [128, 4], F32); bEND = Buf("END")
            V = sb1("V", [128, NT, 4], F32); bV = Buf("V")
            DF = sb1("DF", [128, NT], F32); bDF = Buf("DF")
            GK = sb1("GK", [128, NBK], F32); bGK = Buf("GK")
            GT = sb1("GT", [128, NBK], F32); bGT = Buf("GT")
            IXF = sb1("IXF", [128, NBK, 8], F32); bIXF = Buf("IXF")
            NC4 = NT * 4
            s.op("dve", lambda e: e.tensor_copy(out=OHb[:], in_=OHall[:].rearrange("p t g -> p (t g)")), reads=[bOH], writes=[bOHb])
            s.op("pe", lambda e: e.matmul(out=self.bank(2)[:, 0:NC4], lhsT=tri_b[:], rhs=OHb[:], start=True, stop=True), reads=[bOHb, CON], writes=[pb[2]])
            s.op("pe", lambda e: e.matmul(out=self.bank(3)[:, 0:NC4], lhsT=ones_b[:], rhs=OHb[:], start=True, stop=True), reads=[bOHb, CON], writes=[pb[3]])
            s.op("act", lambda e: e.copy(out=TOTs[:].rearrange("p t g -> p (t g)"), in_=self.bank(3)[:, 0:NC4]), reads=[pb[3]], writes=[bTOT])
            for g in range(4):
                s.op("dve", lambda e, g=g: e.tensor_tensor_scan(out=INC[:, g, :], data0=ones64[:], data1=TOTs[:, :, g], initial=0.0, op0=ALU.mult, op1=ALU.add),
                     reads=[bTOT, CON], writes=[bINC])
            s.op("dve", lambda e: e.tensor_tensor(out=EXC[:], in0=INC[:], in1=TOTs[:].rearrange("p t g -> p g t"), op=ALU.subtract), reads=[bINC, bTOT], writes=[bEXC])
            s.op("dve", lambda e: e.tensor_scalar(out=PC[:], in0=INC[:, :, NT - 1], scalar1=511.0, scalar2=1.0 / 512, op0=ALU.add, op1=ALU.mult), reads=[bINC], writes=[bPC])
            s.op("dve", lambda e: e.tensor_scalar(out=PC[:], in0=PC[:], scalar1=-0.5 + 1.0 / 2048, scalar2=None, op0=ALU.add), reads=[bPC], writes=[bPC])
            s.op("dve", lambda e: e.tensor_scalar(out=PC[:], in0=PC[:], scalar1=MAGIC, scalar2=None, op0=ALU.add), reads=[bPC], writes=[bPC])
            s.op("dve", lambda e: e.tensor_scalar(out=PC[:], in0=PC[:], scalar1=-MAGIC, scalar2=512.0, op0=ALU.add, op1=ALU.mult), reads=[bPC], writes=[bPC])
            s.op("dve", lambda e: e.memset(BASE[:], 0.0), writes=[bBASE])
            for g in range(1, 4):
                s.op("dve", lambda e, g=g: e.tensor_tensor(out=BASE[:, g:g + 1], in0=BASE[:, g - 1:g], in1=PC[:, g - 1:g], op=ALU.add), reads=[bBASE, bPC], writes=[bBASE])
            s.op("dve", lambda e: e.tensor_tensor(out=END[:], in0=BASE[:], in1=PC[:], op=ALU.add), reads=[bBASE, bPC], writes=[bEND])
            s.op("dve", lambda e: e.tensor_tensor(out=V[:], in0=self.bank(2)[:, 0:NC4].rearrange("p (t g) -> p t g", g=4), in1=EXC[:].rearrange("p g t -> p t g"), op=ALU.add),
                 reads=[pb[2], bEXC], writes=[bV])
            s.op("dve", lambda e: e.tensor_tensor(out=V[:], in0=V[:], in1=BASE[:].unsqueeze(1).broadcast_to([128, NT, 4]), op=ALU.add), reads=[bV, bBASE], writes=[bV])
            s.op("dve", lambda e: e.tensor_tensor(out=V[:], in0=V[:], in1=OHall[:], op=ALU.mult), reads=[bV, bOH], writes=[bV])
            s.op("dve", lambda e: e.tensor_reduce(out=DF[:], in_=V[:], axis=AX.X, op=ALU.add), reads=[bV], writes=[bDF])
            s.op("dve", lambda e: e.tensor_copy(out=DEST[:], in_=DF[:]), reads=[bDF], writes=[bDEST])
            s.op("dve", lambda e: e.tensor_scalar(out=GK[:], in0=kb[:], scalar1=END[:, 0:1], scalar2=None, op0=ALU.is_ge), reads=[bEND, CON], writes=[bGK])
            for g in range(1, 3):
                s.op("dve", lambda e, g=g: e.tensor_scalar(out=GT[:], in0=kb[:], scalar1=END[:, g:g + 1], scalar2=None, op0=ALU.is_ge), reads=[bEND, CON], writes=[bGT])
                s.op("dve", lambda e: e.tensor_tensor(out=GK[:], in0=GK[:], in1=GT[:], op=ALU.add), reads=[bGK, bGT], writes=[bGK])
            s.op("dve", lambda e: e.scalar_tensor_tensor(out=IXF[:], in0=_bc_last(GK[:], 8), scalar=1024.0, in1=ep[:].unsqueeze(1).broadcast_to([128, NBK, 8]),
                                                         op0=ALU.mult, op1=ALU.add), reads=[bGK, CON], writes=[bIXF])
            s.op("dve", lambda e: e.tensor_copy(out=IDXW[:], in_=IXF[:].rearrange("p k e -> p (k e)")), reads=[bIXF], writes=[bIDXW])
            XR = [sb1("XR%d" % i, [128, D + 8], BF16) for i in range(3)]; bXR = [Buf("XR%d" % i) for i in range(3)]
            s.barrier(["sp", "pool"])
            for t in range(NT):
                k = t % 3
                s.dma("sp", XR[k][:, 0:D], self.hn3[t * 128:(t + 1) * 128, :], load=bXR[k])
                s.op("dve", lambda e, t=t, k=k: e.tensor_copy(out=XR[k][:, D:D + 8], in_=CW8all[:, t, :]), reads=[bCW8], writes=[bXR[k]])
                s.idma(self.sortx, XR[k][:], DEST[:, t:t + 1], scatter=True, buf=bXR[k], idxbuf=bDEST)
            s.barrier()
        with ExitStack() as p2:
            sb2 = lambda n, sh, d=F32: p2.enter_context(nc.sbuf_tensor(self.uname(n), list(sh), d))
            XSS = sb2("XSS", [128, 4, D + 8], BF16); bXSS = Buf("XSS")
            XT = sb2("XT", [128, NCH, 512], BF16); bXT = Buf("XT")
            CWT = sb2("CWT", [8, 512], BF16); bCWT = Buf("CWT")
            WG = [sb2("WG%d" % i, [128, NCH * FF], BF16) for i in range(2)]; bWG = [Buf("WG%d" % i) for i in range(2)]
            WU = [sb2("WU%d" % i, [128, NCH * FF], BF16) for i in range(2)]; bWU = [Buf("WU%d" % i) for i in range(2)]
            WD = [sb2("WD%d" % i, [128, 8, D], BF16) for i in range(2)]; bWD = [Buf("WD%d" % i) for i in range(2)]
            HID = sb2("HID", [128, 8, 512], BF16); bHID = [Buf("HID%d" % i) for i in range(8)]
            SG = [sb2("SG%d" % i, [128, 512], BF16) for i in range(2)]; bSG = [Buf("SG%d" % i) for i in range(2)]
            T1 = [sb2("T1%d" % i, [128, 512], BF16) for i in range(2)]; bT1 = [Buf("T1%d" % i) for i in range(2)]
            MO = [sb2("MO%d" % i, [128, 4, D], F32) for i in range(2)]; bMO = [Buf("MO%d" % i) for i in range(2)]

            def load_expert(j):
                sl_ = j % 2
                s.idma(WG[sl_][:], self.wgs, IDXW[:, j:j + 1], scatter=False, buf=bWG[sl_], idxbuf=bIDXW)
                s.idma(WU[sl_][:], self.wus, IDXW[:, j:j + 1], scatter=False, buf=bWU[sl_], idxbuf=bIDXW)

            def load_down(q):
                sl_ = q % 2
                for el in range(4):
                    j = q * 4 + el
                    s.idma(WD[sl_][:, el * 2:el * 2 + 2, :].rearrange("p c d -> p (c d)"), self.wds, IDXW[:, j:j + 1], scatter=False, buf=bWD[sl_], idxbuf=bIDXW)

            NJ = NBK * 8
            load_expert(0); load_expert(1); load_down(0)
            it = 0
            for k in range(NBK):
                s.dma("sp", XSS[:], self.sortx[k * 512:(k + 1) * 512, :].rearrange("(t p) c -> p t c", p=128), load=bXSS)
                for t in range(4):
                    def tr(e, t=t):
                        ins = None
                        for c in range(NCH):
                            ins = e.transpose(out=self.bank(0, BF16)[:, c * 128:(c + 1) * 128], in_=XSS[:, t, c * 128:(c + 1) * 128], identity=ident_b[:])
                        return ins
                    s.op("pe", tr, reads=[bXSS, CON], writes=[pb[0]])
                    s.op("act", lambda e, t=t: e.copy(out=XT[:, :, t * 128:(t + 1) * 128], in_=self.bank(0, BF16).rearrange("p (c t) -> p c t", c=NCH)),
                         reads=[pb[0]], writes=[bXT])
                def trcw(e):
                    ins = None
                    for t in range(4):
                        ins = e.transpose(out=self.bank(1, BF16)[0:8, t * 128:(t + 1) * 128], in_=XSS[:, t, D:D + 8], identity=ident_b[:])
                    return ins
                s.op("pe", trcw, reads=[bXSS, CON], writes=[pb[1]])
                s.op("act", lambda e: e.copy(out=CWT[:], in_=self.bank(1, BF16)[0:8, 0:512]), reads=[pb[1]], writes=[bCWT])
                mo = k % 2
                for sg in range(2):
                    for el4 in range(4):
                        el = sg * 4 + el4
                        j = k * 8 + el
                        kk = j % 2
                        bc = 2 + (j % 2)
                        s.op("pe", lambda e, bc=bc, el=el: e.matmul(out=self.bank(bc), lhsT=sel[:, el * 128:(el + 1) * 128], rhs=CWT[:], start=True, stop=True),
                             reads=[bCWT, CON], writes=[pb[bc]])
                        for fc in range(2):
                            jj = it % 2
                            bg, bu = 4 + jj, 6 + jj
                            s.op("pe", lambda e, kk=kk, fc=fc, bg=bg: self.mm_group(e, self.bank(bg), [(WG[kk][:, c * FF + fc * 128:c * FF + (fc + 1) * 128], XT[:, c, :]) for c in range(NCH)]),
                                 reads=[bWG[kk], bXT], writes=[pb[bg]])
                            s.op("pe", lambda e, kk=kk, fc=fc, bu=bu: self.mm_group(e, self.bank(bu), [(WU[kk][:, c * FF + fc * 128:c * FF + (fc + 1) * 128], XT[:, c, :]) for c in range(NCH)]),
                                 reads=[bWU[kk], bXT], writes=[pb[bu]])
                            s.op("act", lambda e, jj=jj, bg=bg: e.activation(out=SG[jj][:], in_=self.bank(bg), func=AF.Silu), reads=[pb[bg]], writes=[bSG[jj]])
                            s.op("dve", lambda e, jj=jj, bu=bu: e.tensor_tensor(out=T1[jj][:], in0=SG[jj][:], in1=self.bank(bu), op=ALU.mult),
                                 reads=[bSG[jj], pb[bu]], writes=[bT1[jj]])
                            hj = el4 * 2 + fc
                            s.op("dve", lambda e, jj=jj, bc=bc, hj=hj: e.tensor_tensor(out=HID[:, hj, :], in0=T1[jj][:], in1=self.bank(bc), op=ALU.mult),
                                 reads=[bT1[jj], pb[bc]], writes=[bHID[hj]])
                            it += 1
                        if j + 2 < NJ:
                            load_expert(j + 2)
                    q = k * 2 + sg
                    ws = q % 2
                    for t in range(4):
                        for dh in range(2):
                            bd = (t * 2 + dh) % 2
                            s.op("pe", lambda e, t=t, dh=dh, bd=bd, ws=ws: self.mm_group(e, self.bank(bd), [(HID[:, jx, t * 128:(t + 1) * 128], WD[ws][:, jx, dh * 512:(dh + 1) * 512]) for jx in range(8)]),
                                 reads=bHID + [bWD[ws]], writes=[pb[bd]])
                            dst = MO[mo][:, t, dh * 512:(dh + 1) * 512]
                            if sg == 0:
                                s.op("act", lambda e, dst=dst, bd=bd: e.copy(out=dst, in_=self.bank(bd)), reads=[pb[bd]], writes=[bMO[mo]])
                            else:
                                s.op("dve", lambda e, dst=dst, bd=bd: e.tensor_tensor(out=dst, in0=dst, in1=self.bank(bd), op=ALU.add), reads=[bMO[mo], pb[bd]], writes=[bMO[mo]])
                    if q + 1 < NBK * 2:
                        load_down(q + 1)
                s.dma("sp", self.sortout[k * 512:(k + 1) * 512, :].rearrange("(t p) d -> p t d", p=128), MO[mo][:], store=bMO[mo])
            s.barrier()
        with ExitStack() as p3:
            sb3 = lambda n, sh, d=F32: p3.enter_context(nc.sbuf_tensor(self.uname(n), list(sh), d))
            G = [sb3("G%d" % i, [128, D], F32) for i in range(4)]; bG = [Buf("G%d" % i) for i in range(4)]
            Hh = [sb3("Hh%d" % i, [128, D], F32) for i in range(4)]; bHh = [Buf("Hh%d" % i) for i in range(4)]
            ssq, rstd = nrm["ssq"], nrm["rstd"]
            sq = [sb3("sq%d" % i, [128, 1], F32) for i in range(4)]; bsq = [Buf("sq%d" % i) for i in range(4)]
            for t in range(NT):
                k = t % 4
                s.idma(G[k][:], self.sortout, DEST[:, t:t + 1], scatter=False, buf=bG[k], idxbuf=bDEST)
                s.dma("sp", Hh[k][:], self.h2[t * 128:(t + 1) * 128, :], load=bHh[k])
                s.op("dve", lambda e, k=k: e.tensor_tensor(out=Hh[k][:], in0=Hh[k][:], in1=G[k][:], op=ALU.add), reads=[bHh[k], bG[k]], writes=[bHh[k]])
                s.op("act", lambda e, k=k: e.activation(out=nrm["JUNK"][:], in_=Hh[k][:], func=AF.Square, accum_out=sq[k][:]), reads=[bHh[k]], writes=[nrm["bJ"], bsq[k]])
                s.op("act", lambda e, k=k: e.activation(out=sq[k][:], in_=sq[k][:], func=AF.Ln, scale=1.0 / D, bias=nrm["epsc"][:]), reads=[bsq[k]], writes=[bsq[k]])
                s.op("act", lambda e, k=k: e.activation(out=sq[k][:], in_=sq[k][:], func=AF.Exp, scale=-0.5), reads=[bsq[k]], writes=[bsq[k]])
                s.op("dve", lambda e, k=k: e.scalar_tensor_tensor(out=G[k][:], in0=Hh[k][:], scalar=sq[k][:], in1=gfb[:], op0=ALU.mult, op1=ALU.mult),
                     reads=[bHh[k], bsq[k], CON], writes=[bG[k]])
                s.dma("sp", self.out[t * 128:(t + 1) * 128, :], G[k][:], store=bG[k])

    def phase1(self, ps):
        nc, s, pb = self.nc, self.s, self.pb
        sb = lambda n, sh, d=F32: ps.enter_context(nc.sbuf_tensor(self.uname(n), list(sh), d))
        S = self.S
        BPS = S // 512
        NKC = S // 128
        W = self.w
        SCALE = 1.0 / math.sqrt(NOPE + ROPE)
        TWO_PI = 2.0 * math.pi
        MAGIC = 12582912.0
        PI_LO = 3.1415925
        CON = Buf("con1")
        ident_b = sb("ident_b", [128, 128], BF16)
        g1b = sb("g1b", [128, D], F32)
        gqkvb = sb("gqkvb", [128, 640], F32)
        sel2 = sb("sel2", [64, 128], BF16)
        ropec = sb("ropec", [128, 4], F32)
        WIN1 = sb("WIN1", [128, NCH, 704], BF16)
        WQ = sb("WQ", [128, 3, NH, 128], BF16)
        WKN = sb("WKN", [128, 2, NH, 128], BF16)
        WV = sb("WV", [128, 2, 512], BF16)
        ident_f = sb("ident_f", [65, 65], F32)
        s.dma("sp", ident_f[:], self.c_ident[0:65, 0:65], load=CON)
        s.dma("pool", ident_b[:], self.c_ident, load=CON)
        s.dma("pool", sel2[:], self.c_sel2, load=CON)
        s.dma("sp", ropec[:], self.c_rope, load=CON)
        s.dma("sp", g1b[:], W["mix_norm_g"].partition_broadcast(128), load=CON)
        s.dma("sp", gqkvb[:, 0:384], W["q_norm_g"].partition_broadcast(128), load=CON)
        s.dma("sp", gqkvb[:, 384:640], W["kv_norm_g"].partition_broadcast(128), load=CON)
        s.op("dve", lambda e: e.memset(WKN[:], 0.0), writes=[CON])
        win_v = W["w_in"].rearrange("(c p) n -> p c n", p=128)
        s.dma("pool", WIN1[:, :, 0:672], win_v[:, :, 0:672], load=CON)
        s.dma("pool", WIN1[:, :, 672:688], win_v[:, :, 656:672], load=CON)
        s.dma("pool", WIN1[:, :, 688:704], win_v[:, :, 640:656], load=CON)
        for c in range(3):
            src = W["w_q_up"][c * 128:(c + 1) * 128, :].rearrange("p (h n) -> p h n", h=NH)
            s.dma("pool", WQ[:, c, :, 0:96], src, load=CON)
            s.dma("pool", WQ[:, c, :, 96:112], src[:, :, 80:96], load=CON)
            s.dma("pool", WQ[:, c, :, 112:128], src[:, :, 64:80], load=CON)
        for c in range(2):
            src = W["w_kv_up"][c * 128:(c + 1) * 128, :].rearrange("p (h n) -> p h n", h=NH)
            s.dma("pool", WKN[:, c, :, 0:64], src[:, :, 0:64], load=CON)
            s.dma("pool", WV[:, c, :].rearrange("p (h n) -> p h n", h=NH), src[:, :, 64:128], load=CON)
        self.prep_dense_scratch()
        self.prep_expert_scratch()
        nrm = self.mk_norm(sb, "1", xs=False, junk=False)
        X = sb("X", [128, 4, D], F32); bX = Buf("X")
        HT = sb("HT", [128, NCH, 512], BF16); bHT = Buf("HT")
        LAT = [sb("LAT%d" % i, [128, 640], F32) for i in range(2)]; bLAT = [Buf("LAT%d" % i) for i in range(2)]
        LS = [sb("LS%d" % i, [128, 640], BF16) for i in range(2)]; bLS = [Buf("LS%d" % i) for i in range(2)]
        LT = sb("LT", [128, 5, 512], BF16); bLT = Buf("LT")
        lsq_ = [sb("lsq%d" % i, [128, 2], F32) for i in range(2)]; blsq_ = [Buf("lsq%d" % i) for i in range(2)]
        lrs_ = [sb("lrs%d" % i, [128, 2], F32) for i in range(2)]; blrs_ = [Buf("lrs%d" % i) for i in range(2)]
        bJ2 = [Buf("J0"), Buf("J1")]
        PKS = sb("PKS", [64, 512], BF16); bPKS = Buf("PKS")
        QT = sb("QT", [128, NH, 512], BF16); bQT = Buf("QT")
        PT = [sb("PT%d" % i, [128, 512], BF16) for i in range(4)]; bPT = [Buf("PT%d" % i) for i in range(4)]
        ATM = sb("ATM", [128, 4, 512], BF16); bATM = Buf("ATM")
        ATT = sb("ATT", [128, 4, 512], BF16); bATT = Buf("ATT")
        rec = [sb("rec%d" % i, [128, 4], F32) for i in range(2)]; brec = [Buf("rec%d" % i) for i in range(2)]
        KT = sb("KT", [128, NH, S], BF16); bKT = [Buf("KT%d" % i) for i in range(BPS)]
        VA = sb("VA", [128, NKC, NH, 65], BF16); bVA = [Buf("VA%d" % i) for i in range(BPS)]
        TQ = sb("TQ", [128, 512], BF16); bTQ = Buf("TQ")
        TK = sb("TK", [64, 512], BF16); bTK = Buf("TK")
        posI = sb("posI", [128, 512], I32); bposI = Buf("posI")
        posF = sb("posF", [128, 512], F32); bposF = Buf("posF")
        ARG = sb("ARG", [128, 512], F32); bARG = Buf("ARG")
        TT_ = sb("TT", [128, 512], F32); bTT = Buf("TT")
        nrm["JUNK"] = TT_[:].bitcast(BF16)
        nrm["bJ"] = bTT
        s.op("dve", lambda e: e.memset(VA[:, :, :, 64:65], 1.0), writes=bVA)
        QTs = [QT, sb("QT2", [128, NH, 512], BF16)]; bQTs = [bQT, Buf("QT2")]
        XS4 = sb("XS4", [128, 2, D], BF16); bXS4 = Buf("XS4"); bXS4b = Buf("XS4b")

        def stageA(b, i):
            tok0 = b * S + i * 512
            cols = slice(i * 512, (i + 1) * 512)
            QTc, bQTc = QTs[i % 2], bQTs[i % 2]
            s.dma("sp", X[:], self.x[tok0:tok0 + 512, :].rearrange("(t p) d -> p t d", p=128), load=bX); yield
            s.dma("sp", posI[:], self.pos[b, i * 512:(i + 1) * 512].partition_broadcast(128), load=bposI); yield
            s.op("dve", lambda e: e.tensor_copy(out=posF[:], in_=posI[:]), reads=[bposI], writes=[bposF]); yield
            for tbl, btbl, ic, nr in [(TQ, bTQ, 0, 128), (TK, bTK, 2, 64)]:
                s.op("dve", lambda e: e.tensor_scalar(out=ARG[:nr], in0=posF[:nr], scalar1=ropec[:nr, ic:ic + 1],
                                                      scalar2=ropec[:nr, ic + 1:ic + 2], op0=ALU.mult, op1=ALU.add),
                     reads=[bposF, CON], writes=[bARG]); yield
                s.op("dve", lambda e: e.tensor_scalar(out=TT_[:nr], in0=ARG[:nr], scalar1=1.0 / TWO_PI, scalar2=MAGIC, op0=ALU.mult, op1=ALU.add),
                     reads=[bARG], writes=[bTT]); yield
                s.op("dve", lambda e: e.tensor_scalar(out=TT_[:nr], in0=TT_[:nr], scalar1=-MAGIC, scalar2=None, op0=ALU.add),
                     reads=[bTT], writes=[bTT]); yield
                s.op("dve", lambda e: e.scalar_tensor_tensor(out=ARG[:nr], in0=TT_[:nr], scalar=-TWO_PI, in1=ARG[:nr], op0=ALU.mult, op1=ALU.add),
                     reads=[bTT, bARG], writes=[bARG]); yield
                s.op("dve", lambda e: e.tensor_scalar(out=ARG[:nr], in0=ARG[:nr], scalar1=-PI_LO, scalar2=PI_LO, op0=ALU.max, op1=ALU.min),
                     reads=[bARG], writes=[bARG]); yield
                s.op("act", lambda e: e.activation(out=tbl[:nr], in_=ARG[:nr], func=AF.Sin), reads=[bARG], writes=[btbl]); yield
            for t in range(4):
                s.op("act", lambda e: e.activation(out=HT[:].rearrange("p c t -> p (c t)")[:, t * D:(t + 1) * D], in_=X[:, t, :], func=AF.Square,
                                                   accum_out=nrm["ssq"][:, t:t + 1]),
                     reads=[bX], writes=[bHT, nrm["bssq"]]); yield
            s.op("act", lambda e: e.activation(out=nrm["rstd"][:, 0:4], in_=nrm["ssq"][:, 0:4], func=AF.Ln, scale=1.0 / D, bias=nrm["epsc"][:]),
                 reads=[nrm["bssq"]], writes=[nrm["brstd"]]); yield
            s.op("act", lambda e: e.activation(out=nrm["rstd"][:, 0:4], in_=nrm["rstd"][:, 0:4], func=AF.Exp, scale=-0.5), reads=[nrm["brstd"]], writes=[nrm["brstd"]]); yield
            bXS2 = [bXS4, bXS4b]
            for t in range(4):
                s.op("dve", lambda e: e.scalar_tensor_tensor(out=XS4[:, t % 2, :], in0=X[:, t, :], scalar=nrm["rstd"][:, t:t + 1], in1=g1b[:], op0=ALU.mult, op1=ALU.mult),
                     reads=[bX, nrm["brstd"], CON], writes=[bXS2[t % 2]]); yield
                def tr0(e):
                    ins = None
                    for c in range(NCH):
                        ins = e.transpose(out=self.bank(0, BF16)[:, c * 128:(c + 1) * 128], in_=XS4[:, t % 2, c * 128:(c + 1) * 128], identity=ident_b[:])
                    return ins
                s.op("pe", tr0, reads=[bXS2[t % 2], CON], writes=[pb[0]]); yield
                s.op("dve", lambda e: e.tensor_copy(out=HT[:, :, t * 128:(t + 1) * 128], in_=self.bank(0, BF16).rearrange("p (c t) -> p c t", c=NCH)),
                     reads=[pb[0]], writes=[bHT]); yield
            for t in range(4):
                k = t % 2
                lsq, blsq, lrs, blrs = lsq_[k], blsq_[k], lrs_[k], blrs_[k]
                ts_ = slice(t * 128, (t + 1) * 128)
                s.op("pe", lambda e: self.mm_group(e, self.bank(1), [(HT[:, c, ts_], WIN1[:, c, 0:512]) for c in range(NCH)]),
                     reads=[bHT, CON], writes=[pb[1]]); yield
                s.op("pe", lambda e: self.mm_group(e, self.bank(2)[:, 0:128], [(HT[:, c, ts_], WIN1[:, c, 512:640]) for c in range(NCH)]),
                     reads=[bHT, CON], writes=[pb[2]]); yield
                s.op("dve", lambda e: e.tensor_copy(out=LAT[k][:, 0:512], in_=self.bank(1)), reads=[pb[1]], writes=[bLAT[k]]); yield
                s.op("dve", lambda e: e.tensor_copy(out=LAT[k][:, 512:640], in_=self.bank(2)[:, 0:128]), reads=[pb[2]], writes=[bLAT[k]]); yield
                s.op("act", lambda e: e.activation(out=LS[k][:, 0:384], in_=LAT[k][:, 0:384], func=AF.Square, accum_out=lsq[:, 0:1]),
                     reads=[bLAT[k]], writes=[bLS[k], blsq]); yield
                s.op("act", lambda e: e.activation(out=LS[k][:, 384:640], in_=LAT[k][:, 384:640], func=AF.Square, accum_out=lsq[:, 1:2]),
                     reads=[bLAT[k]], writes=[bLS[k], blsq]); yield
                s.op("act", lambda e: e.activation(out=lrs[:, 0:1], in_=lsq[:, 0:1], func=AF.Ln, scale=1.0 / Q_LORA, bias=nrm["epsc"][:]), reads=[blsq], writes=[blrs]); yield
                s.op("act", lambda e: e.activation(out=lrs[:, 1:2], in_=lsq[:, 1:2], func=AF.Ln, scale=1.0 / KV_LORA, bias=nrm["epsc"][:]), reads=[blsq], writes=[blrs]); yield
                s.op("act", lambda e: e.activation(out=lrs[:], in_=lrs[:], func=AF.Exp, scale=-0.5), reads=[blrs], writes=[blrs]); yield
                s.op("dve", lambda e: e.scalar_tensor_tensor(out=LS[k][:, 0:384], in0=LAT[k][:, 0:384], scalar=lrs[:, 0:1], in1=gqkvb[:, 0:384],
                                                             op0=ALU.mult, op1=ALU.mult), reads=[bLAT[k], blrs, CON], writes=[bLS[k]]); yield
                s.op("dve", lambda e: e.scalar_tensor_tensor(out=LS[k][:, 384:640], in0=LAT[k][:, 384:640], scalar=lrs[:, 1:2], in1=gqkvb[:, 384:640],
                                                             op0=ALU.mult, op1=ALU.mult), reads=[bLAT[k], blrs, CON], writes=[bLS[k]]); yield
                if t >= 1:
                    kp = (t - 1) % 2
                    tsp = slice((t - 1) * 128, t * 128)
                    def trp(e):
                        ins = None
                        for c in range(5):
                            ins = e.transpose(out=self.bank(0, BF16)[:, c * 128:(c + 1) * 128], in_=LS[kp][:, c * 128:(c + 1) * 128], identity=ident_b[:])
                        return ins
                    s.op("pe", trp, reads=[bLS[kp], CON], writes=[pb[0]]); yield
                    s.op("dve", lambda e: e.tensor_copy(out=LT[:, :, tsp], in_=self.bank(0, BF16)[:, 0:640].rearrange("p (c t) -> p c t", c=5)),
                         reads=[pb[0]], writes=[bLT]); yield
            def tr3(e):
                ins = None
                for c in range(5):
                    ins = e.transpose(out=self.bank(0, BF16)[:, c * 128:(c + 1) * 128], in_=LS[1][:, c * 128:(c + 1) * 128], identity=ident_b[:])
                return ins
            s.op("pe", tr3, reads=[bLS[1], CON], writes=[pb[0]]); yield
            s.op("dve", lambda e: e.tensor_copy(out=LT[:, :, 384:512], in_=self.bank(0, BF16)[:, 0:640].rearrange("p (c t) -> p c t", c=5)),
                 reads=[pb[0]], writes=[bLT]); yield
            for h in range(NH):
                bk = 2 - (h % 2)
                s.op("pe", lambda e: self.mm_group(e, self.bank(bk), [(WQ[:, c, h, :], LT[:, c, :]) for c in range(3)]),
                     reads=[bLT, CON], writes=[pb[bk]]); yield
                s.op("dve", lambda e: e.tensor_tensor(out=QTc[:, h, :], in0=self.bank(bk), in1=TQ[:], op=ALU.mult),
                     reads=[pb[bk], bTQ], writes=[bQTc]); yield

            yield "kv"
            s.op("pe", lambda e: self.mm_group(e, self.bank(1)[0:64, :], [(WIN1[:, c, 640:704], HT[:, c, :]) for c in range(NCH)]),
                 reads=[bHT, CON], writes=[pb[1]]); yield
            s.op("dve", lambda e: e.tensor_tensor(out=PKS[:], in0=self.bank(1)[0:64, :], in1=TK[:], op=ALU.mult), reads=[pb[1], bTK], writes=[bPKS]); yield
            for h in range(NH):
                bk = 2 - (h % 2)
                s.op("pe", lambda e: self.mm_group(e, self.bank(bk), [(sel2[:], PKS[:])] + [(WKN[:, c, h, :], LT[:, 3 + c, :]) for c in range(2)]),
                     reads=[bPKS, bLT, CON], writes=[pb[bk]]); yield
                s.op("dve", lambda e: e.tensor_copy(out=KT[:, h, cols], in_=self.bank(bk)), reads=[pb[bk]], writes=[bKT[i]]); yield
            for t in range(4):
                bk = 2 - (t % 2)
                ts_ = slice(t * 128, (t + 1) * 128)
                s.op("pe", lambda e: self.mm_group(e, self.bank(bk), [(LT[:, 3 + c, ts_], WV[:, c, :]) for c in range(2)]),
                     reads=[bLT, CON], writes=[pb[bk]]); yield
                s.op("dve", lambda e: e.tensor_copy(out=VA[:, i * 4 + t, :, 0:64], in_=self.bank(bk).rearrange("p (h n) -> p h n", h=NH)),
                     reads=[pb[bk]], writes=[bVA[i]]); yield
        hold_state = {"hold": False}

        def drain(g, n=None):
            if g is None:
                return
            if hold_state["hold"] and hold_state.get("at_kv") is g:
                return
            k = 0
            for v in g:
                if v == "kv" and hold_state["hold"]:
                    hold_state["at_kv"] = g
                    return
                k += 1
                if n is not None and k >= n:
                    return

        ATMs = [ATM, ATM]; bATMs = [bATM, bATM]

        def stageB(b, i, nxt, prevC):
            QTc, bQTc = QTs[i % 2], bQTs[i % 2]
            ATMc, bATMc = ATMs[i % 2], bATMs[i % 2]
            nkc = 4 * i + 4
            per = max(1, -(-150 // (NH * nkc)))
            tb3 = self.bank(3)[:, 0:260].rearrange("p (q n) -> p q n", n=65)

            def epilogue(h):
                accb = 6 + (h % 2)
                hp = h % 2
                s.op("dve", lambda e: e.tensor_copy(out=TT_[0:65, :], in_=self.bank(accb)[0:65, :]), reads=[pb[accb]], writes=[bTT])
                def trb(e):
                    ins = None
                    for qt in range(4):
                        ins = e.transpose(out=self.bank(3)[:, qt * 65:(qt + 1) * 65], in_=TT_[0:65, qt * 128:(qt + 1) * 128], identity=ident_f[:])
                    return ins
                s.op("pe", trb, reads=[bTT, CON], writes=[pb[3]])
                s.op("dve", lambda e: e.reciprocal(out=rec[hp][:], in_=tb3[:, :, 64]), reads=[pb[3]], writes=[brec[hp]])
                s.op("dve", lambda e: e.tensor_tensor(out=ATMc[:, :, h * 64:(h + 1) * 64], in0=tb3[:, :, 0:64],
                                                      in1=_bc_last(rec[hp][:], 64), op=ALU.mult),
                     reads=[pb[3], brec[hp]], writes=[bATMc])

            def emit_pv(h, kc, slot, j0):
                accb = 6 + (h % 2)
                qs = j0 * 128
                s.op("pe", lambda e: e.matmul(out=self.bank(accb)[0:65, qs:512], lhsT=VA[:, kc, h, :], rhs=PT[slot][:, qs:512],
                                              start=(kc == 0), stop=(kc == nkc - 1)),
                     reads=[bPT[slot], bVA[kc // 4]], writes=[pb[accb]])

            steps = [(h, kc) for h in range(NH) for kc in range(nkc)]
            pend = []
            epi_q = []
            for n, (h, kc) in enumerate(steps):
                j = kc - 4 * i
                j0 = max(0, j)
                qs = j0 * 128
                sbk = 4 + (n % 2)
                slot = n % 4
                s.op("pe", lambda e: e.matmul(out=self.bank(sbk)[:, qs:512], lhsT=KT[:, h, kc * 128:(kc + 1) * 128],
                                              rhs=QTc[:, h, qs:512], start=True, stop=True),
                     reads=[bKT[kc // 4], bQTc], writes=[pb[sbk]])
                s.op("act", lambda e: e.activation(out=PT[slot][:, qs:512], in_=self.bank(sbk)[:, qs:512], func=AF.Exp, scale=SCALE),
                     reads=[pb[sbk]], writes=[bPT[slot]])
                if j >= 0:
                    s.op("dve", lambda e: e.memset(PT[slot][64:128, qs:qs + 64], 0.0), writes=[bPT[slot]])
                pend.append((h, kc, slot, j0))
                if len(pend) > 2:
                    p_ = pend.pop(0)
                    emit_pv(*p_)
                    if p_[1] == nkc - 1:
                        epi_q.append((p_[0], n + 1))
                while epi_q and epi_q[0][1] <= n:
                    epilogue(epi_q.pop(0)[0])
                if n == 2 and prevC is not None:
                    prevC()
                    prevC = None
                drain(nxt, per)
            for p_ in pend:
                emit_pv(*p_)
                if p_[1] == nkc - 1:
                    epi_q.append((p_[0], 0))
            for h_, _ in epi_q:
                epilogue(h_)
            if prevC is not None:
                prevC()

        def stageC(b, i):
            tok0 = b * S + i * 512
            ATMc, bATMc = ATMs[i % 2], bATMs[i % 2]
            for t in range(4):
                def tr2(e):
                    ins = None
                    for c in range(4):
                        ins = e.transpose(out=self.bank(0, BF16)[:, c * 128:(c + 1) * 128], in_=ATMc[:, t, c * 128:(c + 1) * 128], identity=ident_b[:])
                    return ins
                s.op("pe", tr2, reads=[bATMc, CON], writes=[pb[0]])
                s.op("dve", lambda e: e.tensor_copy(out=ATT[:, :, t * 128:(t + 1) * 128], in_=self.bank(0, BF16)[:, 0:512].rearrange("p (c t) -> p c t", c=4)),
                     reads=[pb[0]], writes=[bATT])
            s.dma("sp", self.attnT[:, tok0:tok0 + 512].rearrange("(c p) t -> p c t", p=128), ATT[:], store=bATT)

        carry = None
        for b in range(self.NB):
            if carry is None:
                drain(stageA(b, 0))
            else:
                hold_state["hold"] = False
                hold_state["at_kv"] = None
                drain(carry)
                carry = None
            prevC = None
            for i in range(BPS):
                if i + 1 < BPS:
                    nxt = stageA(b, i + 1)
                elif b + 1 < self.NB:
                    nxt = stageA(b + 1, 0)
                    hold_state["hold"] = True
                    carry = nxt
                else:
                    nxt = None
                stageB(b, i, nxt, prevC)
                drain(nxt)
                prevC = (lambda b=b, i=i: stageC(b, i))
            prevC()
    def phase3_dense(self, ps):
        nc, s = self.nc, self.s
        sb = lambda n, sh, d=F32: ps.enter_context(nc.sbuf_tensor(self.uname(n), list(sh), d))
        TM = 1024
        NMT = self.NTOK // TM
        NTT = TM // 128
        CON = Buf("con3")
        ident_f = sb("ident_f", [128, 128], F32)
        ident_b = sb("ident_b", [128, 128], BF16)
        sel = sb("sel", [32, 32 * 128], BF16)
        g3b = sb("g3b", [128, D], F32)
        gfb = sb("gfb", [128, D], F32)
        wr = sb("wr", [128, NCH, 36], BF16)
        bb = sb("bb", [128, NTT, 36], F32)
        s.dma("sp", ident_f[:], self.c_ident, load=CON)
        s.dma("pool", ident_b[:], self.c_ident, load=CON)
        s.dma("pool", sel[:], self.c_sel, load=CON)
        s.dma("sp", g3b[:], self.w["ffn_norm_g"].partition_broadcast(128), load=CON)
        s.dma("sp", gfb[:], self.w["final_norm_g"].partition_broadcast(128), load=CON)
        s.dma("pool", wr[:], self.w["w_router"].rearrange("(c p) n -> p c n", p=128), load=CON)
        for t in range(NTT):
            s.dma("sp", bb[:, t, :], self.w["b_router"].partition_broadcast(128), load=CON)
        H = sb("H", [128, NTT, D], F32); bH = Buf("H")
        XS = [sb("XS%d" % i, [128, D], BF16) for i in range(2)]; bXS = [Buf("XS%d" % i) for i in range(2)]
        XT = sb("XT", [128, NCH, TM], BF16); bXT = Buf("XT")
        JUNK = sb("JUNK", [128, D], BF16); bJ = Buf("JUNK")
        ssq = sb("ssq", [128, NTT], F32); bssq = Buf("ssq")
        rstd = sb("rstd", [128, NTT], F32); brstd = Buf("rstd")
        L = sb("L", [128, NTT, 36], F32); bL = Buf("L")
        r1 = sb("r1", [128, NTT], F32); r2 = sb("r2", [128, NTT], F32); r3 = sb("r3", [128, NTT], F32)
        r4 = sb("r4", [128, NTT], F32); r5 = sb("r5", [128, NTT], F32)
        br = [Buf("r%d" % i) for i in range(6)]
        dg = sb("dg", [128, NTT, 4], F32); bdg = Buf("dg")
        eg = sb("eg", [128, NTT, 4], F32); beg = Buf("eg")
        oh = sb("oh", [128, NTT, 4], F32); boh = Buf("oh")
        tmp = sb("tmp", [128, NTT, 4, 8], F32); btmp = Buf("tmp")
        sl = sb("sl", [128, NTT, 8], F32); bsl = Buf("sl")
        sl2 = sb("sl2", [128, NTT, 8], F32); bsl2 = Buf("sl2")
        eq1 = sb("eq1", [128, NTT, 8], F32); beq1 = Buf("eq1")
        eq2 = sb("eq2", [128, NTT, 8], F32); beq2 = Buf("eq2")
        cw8 = sb("cw8", [128, NTT, 8], F32); bcw8 = Buf("cw8")
        CW = sb("CW", [128, NTT, 32], BF16); bCW = Buf("CW")
        CWT = sb("CWT", [32, TM], BF16); bCWT = Buf("CWT")
        WG = [sb("WG%d" % i, [128, NCH, FF], BF16) for i in range(2)]; bWG = [Buf("WG%d" % i) for i in range(2)]
        WU = [sb("WU%d" % i, [128, NCH, FF], BF16) for i in range(2)]; bWU = [Buf("WU%d" % i) for i in range(2)]
        WD = [sb("WD%d" % i, [128, 8, D], BF16) for i in range(2)]; bWD = [Buf("WD%d" % i) for i in range(2)]
        HID = sb("HID", [128, 8, TM], BF16); bHID = [Buf("HID%d" % i) for i in range(8)]
        SG = [sb("SG%d" % i, [128, 512], BF16) for i in range(2)]; bSG = [Buf("SG%d" % i) for i in range(2)]
        T1 = [sb("T1%d" % i, [128, 512], BF16) for i in range(2)]; bT1 = [Buf("T1%d" % i) for i in range(2)]
        OUT = [sb("OUT%d" % i, [128, D], F32) for i in range(2)]; bOUT = [Buf("OUT%d" % i) for i in range(2)]
        pb = self.pb
        wg, wu, wd = self.w["w_exp_gate"], self.w["w_exp_up"], self.w["w_exp_down"]

        def load_expert(e):
            sl_ = e % 2
            s.dma("pool", WG[sl_][:], wg[e].rearrange("(c p) f -> p c f", p=128), load=bWG[sl_])
            s.dma("pool", WU[sl_][:], wu[e].rearrange("(c p) f -> p c f", p=128), load=bWU[sl_])

        def load_down(sg):
            sl_ = sg % 2
            for el in range(4):
                e = sg * 4 + el
                s.dma("pool", WD[sl_][:, el * 2:el * 2 + 2, :], wd[e].rearrange("(c p) d -> p c d", p=128),
                      load=bWD[sl_])

        def rms_stats(src_fn, n):
            for t in range(n):
                s.op("act", lambda e, t=t: e.activation(out=JUNK[:], in_=src_fn(t), func=AF.Square,
                                                        accum_out=ssq[:, t:t + 1]),
                     reads=[bH], writes=[bJ, bssq])
            s.op("act", lambda e: e.activation(out=rstd[:, :n], in_=ssq[:, :n], func=AF.Sqrt, scale=1.0 / D, bias=EPS),
                 reads=[bssq], writes=[brstd])
            s.op("dve", lambda e: e.reciprocal(out=rstd[:, :n], in_=rstd[:, :n]), reads=[brstd], writes=[brstd])

        for mt in range(NMT):
            tok0 = mt * TM
            load_expert(0)
            load_expert(1)
            load_down(0)
            s.dma("sp", H[:], self.h2[tok0:tok0 + TM, :].rearrange("(t p) d -> p t d", p=128), load=bH)
            rms_stats(lambda t: H[:, t, :], NTT)
            for t in range(NTT):
                k = t % 2
                s.op("dve", lambda e, t=t, k=k: e.scalar_tensor_tensor(out=XS[k][:], in0=H[:, t, :], scalar=rstd[:, t:t + 1],
                                                                       in1=g3b[:], op0=ALU.mult, op1=ALU.mult),
                     reads=[bH, brstd, CON], writes=[bXS[k]])
                def tr(e, k=k):
                    ins = None
                    for c in range(NCH):
                        ins = e.transpose(out=self.bank(0, BF16)[:, c * 128:(c + 1) * 128], in_=XS[k][:, c * 128:(c + 1) * 128],
                                          identity=ident_b[:])
                    return ins
                s.op("pe", tr, reads=[bXS[k], CON], writes=[pb[0]])
                s.op("act", lambda e, t=t: e.copy(out=XT[:, :, t * 128:(t + 1) * 128],
                                                  in_=self.bank(0, BF16).rearrange("p (c t) -> p c t", c=NCH)),
                     reads=[pb[0]], writes=[bXT])
            def rl(e):
                ins = None
                for t in range(NTT):
                    for c in range(NCH):
                        ins = e.matmul(out=self.bank(1)[:, t * 36:(t + 1) * 36], lhsT=XT[:, c, t * 128:(t + 1) * 128],
                                       rhs=wr[:, c, :], start=(c == 0), stop=(c == NCH - 1))
                return ins
            s.op("pe", rl, reads=[bXT, CON], writes=[pb[1]])
            s.op("dve", lambda e: e.tensor_tensor(out=L[:], in0=self.bank(1)[:, :NTT * 36].rearrange("p (t n) -> p t n", t=NTT),
                                                  in1=bb[:], op=ALU.add), reads=[pb[1], CON], writes=[bL])
            Lg = L[:, :, 0:4]
            Le = L[:, :, 4:36].rearrange("p t (g e) -> p t g e", g=4)
            s.op("dve", lambda e: e.tensor_reduce(out=r1[:], in_=Lg, axis=AX.X, op=ALU.max), reads=[bL], writes=[br[1]])
            s.op("dve", lambda e: e.tensor_tensor(out=dg[:], in0=Lg, in1=_bc_last(r1[:], 4), op=ALU.subtract),
                 reads=[bL, br[1]], writes=[bdg])
            s.op("act", lambda e: e.activation(out=eg[:], in_=dg[:], func=AF.Exp), reads=[bdg], writes=[beg])
            s.op("dve", lambda e: e.tensor_reduce(out=r2[:], in_=eg[:], axis=AX.X, op=ALU.add), reads=[beg], writes=[br[2]])
            s.op("dve", lambda e: e.reciprocal(out=r2[:], in_=r2[:]), reads=[br[2]], writes=[br[2]])
            s.op("dve", lambda e: e.tensor_single_scalar(out=oh[:], in_=dg[:], scalar=0.0, op=ALU.is_ge), reads=[bdg], writes=[boh])
            s.op("dve", lambda e: e.tensor_tensor(out=tmp[:], in0=Le, in1=_bc_last(oh[:], 8), op=ALU.mult),
                 reads=[bL, boh], writes=[btmp])
            s.op("dve", lambda e: e.tensor_reduce(out=sl[:], in_=tmp[:].rearrange("p t g e -> p t e g"), axis=AX.X, op=ALU.add),
                 reads=[btmp], writes=[bsl])
            s.op("dve", lambda e: e.tensor_reduce(out=r3[:], in_=sl[:], axis=AX.X, op=ALU.max), reads=[bsl], writes=[br[3]])
            s.op("dve", lambda e: e.tensor_tensor(out=eq1[:], in0=sl[:], in1=_bc_last(r3[:], 8), op=ALU.is_equal),
                 reads=[bsl, br[3]], writes=[beq1])
            s.op("dve", lambda e: e.scalar_tensor_tensor(out=sl2[:], in0=eq1[:], scalar=-1e30, in1=sl[:], op0=ALU.mult, op1=ALU.add),
                 reads=[beq1, bsl], writes=[bsl2])
            s.op("dve", lambda e: e.tensor_reduce(out=r4[:], in_=sl2[:], axis=AX.X, op=ALU.max), reads=[bsl2], writes=[br[4]])
            s.op("dve", lambda e: e.tensor_tensor(out=eq2[:], in0=sl2[:], in1=_bc_last(r4[:], 8), op=ALU.is_equal),
                 reads=[bsl2, br[4]], writes=[beq2])
            s.op("dve", lambda e: e.tensor_tensor(out=r5[:], in0=r4[:], in1=r3[:], op=ALU.subtract), reads=[br[3], br[4]], writes=[br[5]])
            s.op("act", lambda e: e.activation(out=r5[:], in_=r5[:], func=AF.Exp), reads=[br[5]], writes=[br[5]])
            s.op("dve", lambda e: e.tensor_scalar(out=r3[:], in0=r5[:], scalar1=1.0, scalar2=None, op0=ALU.add), reads=[br[5]], writes=[br[3]])
            s.op("dve", lambda e: e.reciprocal(out=r3[:], in_=r3[:]), reads=[br[3]], writes=[br[3]])
            s.op("dve", lambda e: e.tensor_tensor(out=r3[:], in0=r3[:], in1=r2[:], op=ALU.mult), reads=[br[3], br[2]], writes=[br[3]])
            s.op("dve", lambda e: e.tensor_tensor(out=r4[:], in0=r3[:], in1=r5[:], op=ALU.mult), reads=[br[3], br[5]], writes=[br[4]])
            s.op("dve", lambda e: e.tensor_tensor(out=eq1[:], in0=eq1[:], in1=_bc_last(r3[:], 8), op=ALU.mult),
                 reads=[beq1, br[3]], writes=[beq1])
            s.op("dve", lambda e: e.tensor_tensor(out=eq2[:], in0=eq2[:], in1=_bc_last(r4[:], 8), op=ALU.mult),
                 reads=[beq2, br[4]], writes=[beq2])
            s.op("dve", lambda e: e.tensor_tensor(out=cw8[:], in0=eq1[:], in1=eq2[:], op=ALU.add), reads=[beq1, beq2], writes=[bcw8])
            s.op("dve", lambda e: e.tensor_tensor(out=CW[:].rearrange("p t (g e) -> p t g e", g=4), in0=_bc_last(oh[:], 8),
                                                  in1=cw8[:].unsqueeze(2).broadcast_to([128, NTT, 4, 8]), op=ALU.mult),
                 reads=[boh, bcw8], writes=[bCW])
            def trcw(e):
                ins = None
                for t in range(NTT):
                    ins = e.transpose(out=self.bank(1, BF16)[0:32, t * 128:(t + 1) * 128], in_=CW[:, t, :], identity=ident_b[:])
                return ins
            s.op("pe", trcw, reads=[bCW, CON], writes=[pb[1]])
            s.op("act", lambda e: e.copy(out=CWT[:], in_=self.bank(1, BF16)[0:32, :]), reads=[pb[1]], writes=[bCWT])
            it = 0
            for sg in range(8):
                for el in range(4):
                    e_ = sg * 4 + el
                    k = e_ % 2
                    for half in range(2):
                        tsl = slice(half * 512, (half + 1) * 512)
                        bc = 2 + (it % 2)
                        s.op("pe", lambda e, bc=bc, e_=e_, tsl=tsl: e.matmul(out=self.bank(bc), lhsT=sel[:, e_ * 128:(e_ + 1) * 128],
                                                                          rhs=CWT[:, tsl], start=True, stop=True),
                             reads=[bCWT, CON], writes=[pb[bc]])
                        for fc in range(2):
                            j = it % 2
                            bg, bu = 4 + j, 6 + j
                            def mm(e, W, bnk, fc=fc, tsl=tsl):
                                ins = None
                                for c in range(NCH):
                                    ins = e.matmul(out=self.bank(bnk), lhsT=W[:, c, fc * 128:(fc + 1) * 128], rhs=XT[:, c, tsl],
                                                   start=(c == 0), stop=(c == NCH - 1))
                                return ins
                            s.op("pe", lambda e, mm=mm, k=k, bg=bg: mm(e, WG[k], bg), reads=[bWG[k], bXT], writes=[pb[bg]])
                            s.op("pe", lambda e, mm=mm, k=k, bu=bu: mm(e, WU[k], bu), reads=[bWU[k], bXT], writes=[pb[bu]])
                            s.op("act", lambda e, j=j, bg=bg: e.activation(out=SG[j][:], in_=self.bank(bg), func=AF.Silu),
                                 reads=[pb[bg]], writes=[bSG[j]])
                            s.op("dve", lambda e, j=j, bu=bu: e.tensor_tensor(out=T1[j][:], in0=SG[j][:], in1=self.bank(bu), op=ALU.mult),
                                 reads=[bSG[j], pb[bu]], writes=[bT1[j]])
                            hj = el * 2 + fc
                            s.op("dve", lambda e, j=j, bc=bc, hj=hj, tsl=tsl: e.tensor_tensor(out=HID[:, hj, tsl], in0=T1[j][:],
                                                                                            in1=self.bank(bc), op=ALU.mult),
                                 reads=[bT1[j], pb[bc]], writes=[bHID[hj]])
                            it += 1
                    if e_ + 2 < NE:
                        load_expert(e_ + 2)
                ws = sg % 2
                for t in range(NTT):
                    for dh in range(2):
                        bd = (t * 2 + dh) % 2
                        def dmm(e, t=t, dh=dh, bd=bd, ws=ws):
                            ins = None
                            for j in range(8):
                                ins = e.matmul(out=self.bank(bd), lhsT=HID[:, j, t * 128:(t + 1) * 128],
                                               rhs=WD[ws][:, j, dh * 512:(dh + 1) * 512], start=(j == 0), stop=(j == 7))
                            return ins
                        s.op("pe", dmm, reads=bHID + [bWD[ws]], writes=[pb[bd]])
                        s.op("dve", lambda e, t=t, dh=dh, bd=bd: e.tensor_tensor(out=H[:, t, dh * 512:(dh + 1) * 512],
                                                                                in0=H[:, t, dh * 512:(dh + 1) * 512],
                                                                                in1=self.bank(bd), op=ALU.add),
                             reads=[bH, pb[bd]], writes=[bH])
                if sg + 1 < 8:
                    load_down(sg + 1)
            rms_stats(lambda t: H[:, t, :], NTT)
            for t in range(NTT):
                k = t % 2
                s.op("dve", lambda e, t=t, k=k: e.scalar_tensor_tensor(out=OUT[k][:], in0=H[:, t, :], scalar=rstd[:, t:t + 1],
                                                                       in1=gfb[:], op0=ALU.mult, op1=ALU.mult),
                     reads=[bH, brstd, CON], writes=[bOUT[k]])
                s.dma("sp", self.out[tok0 + t * 128: tok0 + (t + 1) * 128, :], OUT[k][:], store=bOUT[k])


def _consts():
    ident = np.eye(128, dtype=np.float32)
    sel = np.zeros((32, 32, 128), np.float32)
    for e in range(32):
        sel[e, e, :] = 1.0
    inv16 = np.tile((1.0 / np.arange(1, 17, dtype=np.float32))[None, :], (128, 1)).astype(np.float32)
    ones = np.ones((128, 128), np.float32)
    sel2 = np.zeros((64, 128), np.float32)
    for i in range(64):
        sel2[i, 64 + (i % 32)] = 1.0
        sel2[i, 96 + (i % 32)] = 1.0
    invf = (1.0 / (np.float32(10000.0) ** (np.arange(0, 32, 2, dtype=np.float32) / np.float32(32)))).astype(np.float32)
    rope = np.zeros((128, 4), np.float32)
    hp, pi = np.float32(np.pi / 2), np.float32(np.pi)
    for p in range(128):
        if p < 64:
            rope[p, 0], rope[p, 1] = 0.0, hp
        elif p < 96:
            rope[p, 0], rope[p, 1] = invf[p % 16], hp
        elif p < 112:
            rope[p, 0], rope[p, 1] = invf[p % 16], pi
        else:
            rope[p, 0], rope[p, 1] = invf[p % 16], 0.0
        if p < 32:
            rope[p, 2], rope[p, 3] = invf[p % 16], hp
        elif p < 48:
            rope[p, 2], rope[p, 3] = invf[p % 16], pi
        elif p < 64:
            rope[p, 2], rope[p, 3] = invf[p % 16], 0.0
    tri = np.triu(np.ones((128, 128), np.float32), 1)
    kbv = np.tile((512.0 * np.arange(96, dtype=np.float32))[None, :], (128, 1)).astype(np.float32)
    epv = (np.arange(8, dtype=np.float32)[None, :] * 128 + np.arange(128, dtype=np.float32)[:, None]).astype(np.float32)
    return {"c_tri": tri, "c_kb": kbv, "c_ep": epv, "c_ident": ident, "c_sel": sel.reshape(32, 32 * 128), "c_inv16": inv16, "c_ones": ones, "c_sel2": sel2, "c_rope": rope}


def _weights(inp):
    w = {}
    for n in ["mix_norm_g", "w_in", "q_norm_g", "w_q_up", "kv_norm_g", "w_kv_up", "w_attn_branch", "pool_w", "pool_scale",
              "w_pool_branch", "w_mix_out", "xattn_norm_g", "mem_norm_g", "w_xq", "w_xkv", "w_xo", "ffn_norm_g",
              "w_exp_gate", "w_exp_up", "w_exp_down"]:
        w[n] = np.ascontiguousarray(np.asarray(inp[n], np.float32)[0])
    w["final_norm_g"] = np.ascontiguousarray(np.asarray(inp["final_norm_g"], np.float32))
    w["w_router"] = np.ascontiguousarray(np.concatenate([np.asarray(inp["w_router_group"])[0], np.asarray(inp["w_router_expert"])[0]], axis=1))
    w["b_router"] = np.ascontiguousarray(np.concatenate([np.asarray(inp["b_router_group"])[0], np.asarray(inp["b_router_expert"])[0]], axis=0))
    return w


def kernel(**inputs):
    ncores = 8
    x = np.asarray(inputs["x"], np.float32)
    B, S, _ = x.shape
    NB = B // ncores
    prog = Prog(NB, S)
    nc = prog.build()
    w = _weights(inputs)
    w.update(_consts())
    mem = np.asarray(inputs["mem"], np.float32)
    pos = np.asarray(inputs["positions"], np.int32)
    in_maps = []
    for c in range(ncores):
        m = dict(w)
        m["x"] = np.ascontiguousarray(x[c * NB:(c + 1) * NB].reshape(NB * S, D))
        m["mem"] = np.ascontiguousarray(mem[c * NB:(c + 1) * NB].reshape(NB * MEM, D))
        m["positions"] = np.ascontiguousarray(pos[c * NB:(c + 1) * NB])
        in_maps.append(m)
    res = run_bass_kernel_spmd(nc, in_maps, core_ids=list(range(ncores)))
    outs = [np.asarray(r["out"]).reshape(NB, S, D) for r in res.results]
    return np.concatenate(outs, axis=0).astype(np.float32)
```

```python
import math
from contextlib import ExitStack
import numpy as np
import ml_dtypes
import concourse.bass as bass
import concourse.mybir as mybir
from concourse.bass_utils import run_bass_kernel_spmd

F32 = mybir.dt.float32
BF16 = mybir.dt.bfloat16
I32 = mybir.dt.int32
AF = mybir.ActivationFunctionType
ALU = mybir.AluOpType
AX = mybir.AxisListType

D = 1024
NCH = 8
EPS = 1e-6
Q_LORA, KV_LORA, ROPE, POOLW = 384, 256, 32, 512
NH, NOPE, VD = 8, 64, 64
MEM, MH, MHD = 256, 4, 256
NG, EPG, NE, FF = 4, 8, 32, 256
C_Q, C_KV, C_KR, C_POOL, C_GA, C_GB = 0, 384, 640, 672, 1184, 2208
IN_COLS = 3232


class Buf:
    def __init__(self, name):
        self.name = name
        self.w = {}
        self.r = {}
        self.wsem = None
        self.rsem = None


class Sched:
    def __init__(self, nc, es):
        self.nc = nc
        self.es = es
        self.E = {"pe": nc.tensor, "act": nc.scalar, "dve": nc.vector, "pool": nc.gpsimd, "sp": nc.sync}
        self.sem = {}
        self.cnt = {}
        self.seen = {e: {} for e in self.E}
        for e in self.E:
            self.sem[e] = es.enter_context(nc.semaphore("s_" + e))
            self.cnt[e] = 0
        self.dsems = []
        self.nops = 0

    def _wait(self, eng, deps):
        for key, (sem, val, _e) in deps.items():
            if self.seen[eng].get(key, 0) < val:
                self.E[eng].wait_ge(sem, val)
                self.seen[eng][key] = val

    @staticmethod
    def _merge(dst, src, skip_eng=None):
        for k, (s, v, e) in src.items():
            if skip_eng is not None and e == skip_eng:
                continue
            if k not in dst or dst[k][1] < v:
                dst[k] = (s, v, e)

    def op(self, eng, fn, reads=(), writes=()):
        deps = {}
        for b in reads:
            self._merge(deps, b.w)
        for b in writes:
            self._merge(deps, b.w, skip_eng=eng)
            self._merge(deps, b.r, skip_eng=eng)
        self._wait(eng, deps)
        ins = fn(self.E[eng])
        self.cnt[eng] += 1
        ins.then_inc(self.sem[eng], 1)
        tok = (self.sem[eng], self.cnt[eng], eng)
        for b in reads:
            self._merge(b.r, {eng: tok})
        for b in writes:
            b.w = {eng: tok}
            b.r = {}
        self.nops += 1

    def _newsem(self, name):
        s = self.es.enter_context(self.nc.semaphore("%s_%d" % (name, len(self.dsems))))
        ent = [s, 0, "d%d" % len(self.dsems)]
        self.dsems.append(ent)
        return ent

    def dma(self, q, out, in_, load=None, store=None, **kw):
        deps = {}
        if load is not None:
            b = load
            self._merge(deps, b.w)
            self._merge(deps, b.r)
            if b.name.startswith("con"):
                deps = {k: v for k, v in deps.items() if v[2] != "dma"}
            key = "w_" + q
        else:
            b = store
            self._merge(deps, b.w)
            key = "r_" + q
        sems = b.__dict__.setdefault("qsem", {})
        if key not in sems:
            sems[key] = self._newsem("d%s_%s" % (key, b.name))
        ent = sems[key]
        self._wait(q, deps)
        ins = self.E[q].dma_start(out=out, in_=in_, **kw)
        ent[1] += 16
        ins.then_inc(ent[0], 16)
        tok = (ent[0], ent[1], "dma")
        if load is not None:
            self._merge(b.w, {ent[2]: tok})
        else:
            self._merge(b.r, {ent[2]: tok})

    def idma(self, out, in_, idx_ap, scatter, buf, idxbuf):
        q = "pool"
        deps = {}
        self._merge(deps, idxbuf.w)
        b = buf
        if scatter:
            self._merge(deps, b.w)
            key = "r_pool_i"
        else:
            self._merge(deps, b.w)
            self._merge(deps, b.r)
            key = "w_pool_i"
        sems = b.__dict__.setdefault("qsem", {})
        if key not in sems:
            sems[key] = self._newsem("di_" + b.name)
        ent = sems[key]
        self._wait(q, deps)
        if scatter:
            ins = self.E[q].indirect_dma_start(out=out, out_offset=bass.IndirectOffsetOnAxis(ap=idx_ap, axis=0), in_=in_, in_offset=None)
        else:
            ins = self.E[q].indirect_dma_start(out=out, out_offset=None, in_=in_, in_offset=bass.IndirectOffsetOnAxis(ap=idx_ap, axis=0))
        ent[1] += 16
        ins.then_inc(ent[0], 16)
        tok = (ent[0], ent[1], "dma")
        self._merge(idxbuf.r, {ent[2]: tok})
        if scatter:
            self._merge(b.r, {ent[2]: tok})
        else:
            self._merge(b.w, {ent[2]: tok})

    def barrier(self, engines=None):
        for e in (engines or self.E):
            deps = {}
            for f in self.E:
                if f != e and self.cnt[f] > 0:
                    deps[f] = (self.sem[f], self.cnt[f], f)
            for ent in self.dsems:
                if ent[1] > 0:
                    deps[ent[2]] = (ent[0], ent[1], "dma")
            self._wait(e, deps)


def _bc_last(ap, n):
    sh = list(ap.shape)
    return ap.unsqueeze(len(sh)).broadcast_to(sh + [n])


class Prog:
    def __init__(self, NB, S, phases=("p1", "p2a", "p2b", "p3"), dbg=False):
        self.NB, self.S = NB, S
        self.NTOK = NB * S
        self.phases = phases
        self.dbg = dbg

    def build(self):
        nc = bass.Bass("TRN2", target_bir_lowering=False)
        self.nc = nc
        NTOK = self.NTOK
        dt = lambda n, s, d=F32, kind="ExternalInput": nc.dram_tensor(n, list(s), d, kind=kind).ap()
        self.x = dt("x", [NTOK, D])
        self.mem = dt("mem", [self.NB * MEM, D])
        self.pos = dt("positions", [self.NB, self.S], I32)
        self.w = {}
        for n, s in [("mix_norm_g", [D]), ("w_in", [D, IN_COLS]), ("q_norm_g", [Q_LORA]), ("w_q_up", [Q_LORA, 768]),
                     ("kv_norm_g", [KV_LORA]), ("w_kv_up", [KV_LORA, 1024]), ("w_attn_branch", [512, D]),
                     ("pool_w", [4, 128, 128]), ("pool_scale", [POOLW]), ("w_pool_branch", [POOLW, D]),
                     ("w_mix_out", [D, D]), ("xattn_norm_g", [D]), ("mem_norm_g", [D]), ("w_xq", [D, D]),
                     ("w_xkv", [D, 2 * D]), ("w_xo", [D, D]), ("ffn_norm_g", [D]), ("w_router", [D, 36]),
                     ("b_router", [36]), ("w_exp_gate", [NE, D, FF]), ("w_exp_up", [NE, D, FF]),
                     ("w_exp_down", [NE, FF, D]), ("final_norm_g", [D])]:
            self.w[n] = dt(n, s)
        self.c_ident = dt("c_ident", [128, 128])
        self.c_sel = dt("c_sel", [32, 32 * 128])
        self.c_inv16 = dt("c_inv16", [128, 16])
        self.c_ones = dt("c_ones", [128, 128])
        self.c_sel2 = dt("c_sel2", [64, 128])
        self.c_rope = dt("c_rope", [128, 4])
        self.c_tri = dt("c_tri", [128, 128])
        self.c_kb = dt("c_kb", [128, 96])
        self.c_ep = dt("c_ep", [128, 8])
        NBK = 2 * NTOK // 512 + 32
        self.NBK3 = NBK
        self.wgs = dt("wgs", [NE * 128, NCH * FF], BF16, kind="Internal")
        self.wus = dt("wus", [NE * 128, NCH * FF], BF16, kind="Internal")
        self.wds = dt("wds", [NE * 128, 2 * D], BF16, kind="Internal")
        self.hn3 = dt("hn3", [NTOK, D], BF16, kind="Internal")
        self.lscr = dt("lscr", [NTOK, 36], F32, kind="Internal")
        self.s_win2 = dt("s_win2", [128, NCH * 2560], BF16, kind="Internal")
        self.s_wab = dt("s_wab", [128, 4 * D], BF16, kind="Internal")
        self.s_pw = dt("s_pw", [128, 4 * 128], BF16, kind="Internal")
        self.s_wpb = dt("s_wpb", [128, 4 * D], BF16, kind="Internal")
        self.s_wmo = dt("s_wmo", [128, NCH * D], BF16, kind="Internal")
        self.s_wxq = dt("s_wxq", [128, NCH * D], BF16, kind="Internal")
        self.s_wxo = dt("s_wxo", [128, NCH * D], BF16, kind="Internal")
        self.s_wkv = dt("s_wkv", [4, 128, NCH * 512], BF16, kind="Internal")
        self.sortx = dt("sortx", [NBK * 512, D + 8], BF16, kind="Internal")
        self.sortout = dt("sortout", [NBK * 512, D], F32, kind="Internal")
        self.out = dt("out", [NTOK, D], kind="ExternalOutput")
        if self.dbg:
            self.h2 = dt("h2", [NTOK, D], kind="ExternalOutput" if "p2a" in self.phases else "ExternalInput")
            self.attnT = dt("attnT", [512, NTOK], BF16, kind="ExternalOutput" if "p1" in self.phases else "ExternalInput")
        else:
            self.h2 = dt("h2", [NTOK, D], kind="Internal")
            self.attnT = dt("attnT", [512, NTOK], BF16, kind="Internal")
        with ExitStack() as es:
            self.es = es
            self.s = Sched(nc, es)
            self.psum = es.enter_context(nc.psum_tensor("psum", [128, 8 * 512], F32))
            self.pb = [Buf("pb%d" % i) for i in range(8)]
            if "p1" not in self.phases:
                self.prep_dense_scratch()
                self.prep_expert_scratch()
                self.s.barrier()
            for name, fn in [("p1", self.phase1), ("p2a", self.phase2a), ("p2b", self.phase2b), ("p3", self.phase3)]:
                if name in self.phases:
                    with ExitStack() as ps:
                        fn(ps)
                        self.s.barrier()
        return nc

    def uname(self, n):
        self._uid = getattr(self, "_uid", 0) + 1
        return "%s_%d" % (n, self._uid)

    def bank(self, i, dtype=F32):
        ap = self.psum[:, i * 512:(i + 1) * 512]
        if dtype is not F32:
            ap = ap.bitcast(dtype)
        return ap


    def mk_norm(self, sb, tag, xs=True, junk=True):
        n = {}
        if xs:
            n["XS"] = [sb("XS%s%d" % (tag, i), [128, D], BF16) for i in range(2)]
            n["bXS"] = [Buf("XS%d" % i) for i in range(2)]
        if junk:
            n["JUNK"] = sb("JUNK" + tag, [128, D], BF16)
        n["bJ"] = Buf("JUNK")
        n["ssq"] = sb("ssq" + tag, [128, 16], F32); n["bssq"] = Buf("ssq")
        n["rstd"] = sb("rstd" + tag, [128, 16], F32); n["brstd"] = Buf("rstd")
        n["epsc"] = sb("epsc" + tag, [128, 1], F32)
        self.s.op("dve", lambda e: e.memset(n["epsc"][:], EPS), writes=[n["bssq"]])
        return n

    def rms_stats(self, n, src_fn, bsrc, ntt, width=D, col0=0, junk_fn=None, bjunk=None):
        s = self.s
        ssq, rstd = n["ssq"], n["rstd"]
        for t in range(ntt):
            jout = junk_fn(t) if junk_fn is not None else n["JUNK"][:, :width]
            bj = bjunk if junk_fn is not None else n["bJ"]
            s.op("act", lambda e, t=t, jout=jout: e.activation(out=jout, in_=src_fn(t), func=AF.Square,
                                                              accum_out=ssq[:, col0 + t:col0 + t + 1]),
                 reads=[bsrc], writes=[bj, n["bssq"]])
        s.op("act", lambda e: e.activation(out=rstd[:, col0:col0 + ntt], in_=ssq[:, col0:col0 + ntt], func=AF.Ln,
                                           scale=1.0 / width, bias=n["epsc"][:]),
             reads=[n["bssq"]], writes=[n["brstd"]])
        s.op("act", lambda e: e.activation(out=rstd[:, col0:col0 + ntt], in_=rstd[:, col0:col0 + ntt], func=AF.Exp, scale=-0.5),
             reads=[n["brstd"]], writes=[n["brstd"]])

    def norm_T(self, n, src_fn, bsrc, gb, bgb, ntt, dstT, bdst, ident_b, bcon, nch=NCH):
        s = self.s
        self.rms_stats(n, src_fn, bsrc, ntt)
        XS, bXS, rstd = n["XS"], n["bXS"], n["rstd"]
        for t in range(ntt):
            k = t % 2
            s.op("dve", lambda e, t=t, k=k: e.scalar_tensor_tensor(out=XS[k][:], in0=src_fn(t), scalar=rstd[:, t:t + 1],
                                                                   in1=gb[:], op0=ALU.mult, op1=ALU.mult),
                 reads=[bsrc, n["brstd"], bgb], writes=[bXS[k]])
            def tr(e, k=k):
                ins = None
                for c in range(nch):
                    ins = e.transpose(out=self.bank(0, BF16)[:, c * 128:(c + 1) * 128], in_=XS[k][:, c * 128:(c + 1) * 128],
                                      identity=ident_b[:])
                return ins
            s.op("pe", tr, reads=[bXS[k], bcon], writes=[self.pb[0]])
            s.op("act", lambda e, t=t: e.copy(out=dstT[:, :, t * 128:(t + 1) * 128],
                                              in_=self.bank(0, BF16).rearrange("p (c t) -> p c t", c=nch)),
                 reads=[self.pb[0]], writes=[bdst])

    def norm_scale4(self, n, src_fn, bsrc, gb, bgb, ntt, XS4, bXS4):
        s = self.s
        self.rms_stats(n, src_fn, bsrc, ntt, junk_fn=lambda t: XS4[:, t, :], bjunk=bXS4)
        rstd = n["rstd"]
        for t in range(ntt):
            s.op("dve", lambda e, t=t: e.scalar_tensor_tensor(out=XS4[:, t, :], in0=src_fn(t), scalar=rstd[:, t:t + 1],
                                                              in1=gb[:], op0=ALU.mult, op1=ALU.mult),
                 reads=[bsrc, n["brstd"], bgb], writes=[bXS4])

    def norm_tr4(self, XS4, bXS4, ntt, dstT, bdst, ident_b, bcon, nch=NCH, banks=(0, 0)):
        s = self.s
        for t in range(ntt):
            bk = banks[t % 2]
            def tr(e, t=t, bk=bk):
                ins = None
                for c in range(nch):
                    ins = e.transpose(out=self.bank(bk, BF16)[:, c * 128:(c + 1) * 128], in_=XS4[:, t, c * 128:(c + 1) * 128],
                                      identity=ident_b[:])
                return ins
            s.op("pe", tr, reads=[bXS4, bcon], writes=[self.pb[bk]])
            if t % 2 == 0:
                s.op("act", lambda e, t=t, bk=bk: e.copy(out=dstT[:, :, t * 128:(t + 1) * 128],
                                                        in_=self.bank(bk, BF16).rearrange("p (c t) -> p c t", c=nch)),
                     reads=[self.pb[bk]], writes=[bdst])
            else:
                s.op("dve", lambda e, t=t, bk=bk: e.tensor_copy(out=dstT[:, :, t * 128:(t + 1) * 128],
                                                               in_=self.bank(bk, BF16).rearrange("p (c t) -> p c t", c=nch)),
                     reads=[self.pb[bk]], writes=[bdst])

    def mm_group(self, e, out, pairs):
        ins = None
        n = len(pairs)
        for i, (l, r) in enumerate(pairs):
            ins = e.matmul(out=out, lhsT=l, rhs=r, start=(i == 0), stop=(i == n - 1))
        return ins

    def phase2a(self, ps):
        nc, s, pb = self.nc, self.s, self.pb
        sb = lambda n, sh, d=F32: ps.enter_context(nc.sbuf_tensor(self.uname(n), list(sh), d))
        NBLK = self.NTOK // 512
        BPS = self.S // 512
        W = self.w
        CON = Buf("con2a")
        ident_b = sb("ident_b", [128, 128], BF16)
        g1b = sb("g1b", [128, D], F32)
        inv16 = sb("inv16", [128, 16], F32)
        psc = sb("psc", [128, 4], F32)
        WIN2 = sb("WIN2", [128, NCH, 2560], BF16)
        WAB = sb("WAB", [128, 4, D], BF16)
        PW = sb("PW", [128, 4, 128], BF16)
        WPB = sb("WPB", [128, 4, D], BF16)
        WMO = sb("WMO", [128, NCH, D], BF16)
        s.dma("pool", ident_b[:], self.c_ident, load=CON)
        s.dma("sp", g1b[:], W["mix_norm_g"].partition_broadcast(128), load=CON)
        s.dma("sp", inv16[:], self.c_inv16, load=CON)
        s.dma("sp", psc[:], W["pool_scale"].rearrange("(g p) -> p g", p=128), load=CON, allow_slow_non_contiguous=True)
        fl = lambda t: t[:].rearrange("p c n -> p (c n)")
        CONW = Buf("conw2a")
        win2_v = self.s_win2.rearrange("p (c n) -> p c n", c=NCH)
        s.dma("sp", WIN2[:, :, 0:512], win2_v[:, :, 0:512], load=CON)
        s.dma("sp", fl(PW), self.s_pw, load=CON)
        s.dma("sp", WIN2[:, :, 512:2560], win2_v[:, :, 512:2560], load=CONW)
        s.dma("sp", fl(WAB), self.s_wab, load=CONW)
        s.dma("sp", fl(WPB), self.s_wpb, load=CONW)
        s.dma("sp", fl(WMO), self.s_wmo, load=CONW)
        nrm = self.mk_norm(sb, "a")
        Xs = [sb("X%d" % i, [128, 4, D], F32) for i in range(2)]; bXs = [Buf("X%d" % i) for i in range(2)]
        HTs = [sb("HT%d" % i, [128, NCH, 512], BF16) for i in range(2)]; bHTs = [Buf("HT%d" % i) for i in range(2)]
        ATs = [sb("AT%d" % i, [128, 4, 512], BF16) for i in range(2)]; bATs = [Buf("AT%d" % i) for i in range(2)]
        XS4 = sb("XS4", [128, 4, D], BF16); bXS4 = Buf("XS4")
        U = sb("U", [128, 4, 528], F32); bU = [Buf("U%d" % g) for g in range(4)]
        TA = sb("TA", [128, 528], F32); TB = sb("TB", [128, 528], F32); bTA = Buf("TA"); bTB = Buf("TB")
        FX = sb("FX", [128, 16], F32); bFX = Buf("FX")
        DT = sb("DT", [128, 4, 512], BF16); bDT = [Buf("DT%d" % g) for g in range(4)]
        YB = sb("YB", [128, 4, 512], BF16); bYB = Buf("YB")
        MT = sb("MT", [128, NCH, 512], BF16); bMT = Buf("MT")
        SA = [sb("SA%d" % i, [128, 512], F32) for i in range(2)]; bSA = [Buf("SA%d" % i) for i in range(2)]
        SB_ = [sb("SB%d" % i, [128, 512], F32) for i in range(2)]; bSB = [Buf("SB%d" % i) for i in range(2)]
        def pro_load(blk):
            tok0 = blk * 512
            X, bX, AT, bAT = Xs[blk % 2], bXs[blk % 2], ATs[blk % 2], bATs[blk % 2]
            s.dma("sp", X[:], self.x[tok0:tok0 + 512, :].rearrange("(t p) d -> p t d", p=128), load=bX)
            s.dma("sp", AT[:], self.attnT[:, tok0:tok0 + 512].rearrange("(c p) t -> p c t", p=128), load=bAT)

        def pro_scale(blk):
            X, bX = Xs[blk % 2], bXs[blk % 2]
            self.norm_scale4(nrm, lambda t: X[:, t, :], bX, g1b, CON, 4, XS4, bXS4)

        def pro_T(blk):
            self.norm_tr4(XS4, bXS4, 4, HTs[blk % 2], bHTs[blk % 2], ident_b, CON, banks=(0, 2))

        YBs = [YB, sb("YB2", [128, 4, 512], BF16)]; bYBs = [bYB, Buf("YB2")]

        def pool_group(blk, g):
            first = (blk % BPS == 0)
            HT, bHT = HTs[blk % 2], bHTs[blk % 2]
            YBc, bYBc = YBs[blk % 2], bYBs[blk % 2]
            w = 2 << g
            bk = 1 + (g % 2)
            s.op("pe", lambda e: self.mm_group(e, self.bank(bk), [(WIN2[:, c, g * 128:(g + 1) * 128], HT[:, c, :]) for c in range(NCH)]),
                 reads=[bHT, CON], writes=[pb[bk]])
            if first:
                s.op("dve", lambda e: e.memset(U[:, g, 0:16], 0.0), writes=[bU[g]])
            s.op("act", lambda e: e.copy(out=U[:, g, 16:528], in_=self.bank(bk)), reads=[pb[bk]], writes=[bU[g]])
            cur, bcur = U[:, g, :], bU[g]
            lo = 0
            sh = 1
            bufs = [(TA, bTA), (TB, bTB)]
            bi = 0
            while sh < w:
                lo += sh
                dst, bdst = bufs[bi]
                s.op("dve", lambda e: e.tensor_tensor(out=dst[:, lo:528], in0=cur[:, lo:528], in1=cur[:, lo - sh:528 - sh], op=ALU.add),
                     reads=[bcur], writes=[bdst])
                cur, bcur = dst, bdst
                bi ^= 1
                sh *= 2
            s.op("dve", lambda e: e.scalar_tensor_tensor(out=DT[:, g, :], in0=cur[:, 16:528], scalar=1.0 / w, in1=U[:, g, 16:528], op0=ALU.mult, op1=ALU.subtract),
                 reads=[bcur, bU[g]], writes=[bDT[g]])
            if first:
                s.op("dve", lambda e: e.tensor_tensor(out=FX[:, 0:w - 1], in0=cur[:, 16:16 + w - 1], in1=inv16[:, 0:w - 1], op=ALU.mult),
                     reads=[bcur, CON], writes=[bFX])
                s.op("dve", lambda e: e.tensor_tensor(out=DT[:, g, 0:w - 1], in0=FX[:, 0:w - 1], in1=U[:, g, 16:16 + w - 1], op=ALU.subtract),
                     reads=[bFX, bU[g]], writes=[bDT[g]])
            s.op("dve", lambda e: e.tensor_copy(out=U[:, g, 0:16], in_=U[:, g, 512:528]), reads=[bU[g]], writes=[bU[g]])

        def pool_group2(blk, g):
            YBc, bYBc = YBs[blk % 2], bYBs[blk % 2]
            s.op("pe", lambda e: e.matmul(out=self.bank(3), lhsT=PW[:, g, :], rhs=DT[:, g, :], start=True, stop=True),
                 reads=[bDT[g], CON], writes=[pb[3]])
            s.op("act", lambda e: e.activation(out=YBc[:, g, :], in_=self.bank(3), func=AF.Copy, scale=psc[:, g:g + 1]),
                 reads=[pb[3], CON], writes=[bYBc])

        pro_load(0); pro_scale(0); pro_T(0)
        for g in range(4):
            pool_group(0, g)
        for g in range(4):
            pool_group2(0, g)
        for blk in range(NBLK):
            tok0 = blk * 512
            X, bX, AT, bAT, HT, bHT = Xs[blk % 2], bXs[blk % 2], ATs[blk % 2], bATs[blk % 2], HTs[blk % 2], bHTs[blk % 2]
            YBc, bYBc = YBs[blk % 2], bYBs[blk % 2]
            more = blk + 1 < NBLK
            if more:
                pro_load(blk + 1)
            for dc in range(NCH):
                b0 = 4
                k = dc % 2
                cs = slice(dc * 128, (dc + 1) * 128)
                s.op("pe", lambda e: self.mm_group(e, self.bank(b0), [(WIN2[:, c, 512 + dc * 128:512 + (dc + 1) * 128], HT[:, c, :]) for c in range(NCH)]),
                     reads=[bHT, CON, CONW], writes=[pb[b0]])
                s.op("pe", lambda e: self.mm_group(e, self.bank(b0 + 1), [(WIN2[:, c, 1536 + dc * 128:1536 + (dc + 1) * 128], HT[:, c, :]) for c in range(NCH)]),
                     reads=[bHT, CON, CONW], writes=[pb[b0 + 1]])
                s.op("pe", lambda e: self.mm_group(e, self.bank(b0 + 2), [(WAB[:, c, cs], AT[:, c, :]) for c in range(4)]),
                     reads=[bAT, CON, CONW], writes=[pb[b0 + 2]])
                s.op("pe", lambda e: self.mm_group(e, self.bank(b0 + 3), [(WPB[:, c, cs], YBc[:, c, :]) for c in range(4)]),
                     reads=[bYBc, CON, CONW], writes=[pb[b0 + 3]])
                s.op("act", lambda e: e.activation(out=SA[k][:], in_=self.bank(b0), func=AF.Sigmoid), reads=[pb[b0]], writes=[bSA[k]])
                s.op("act", lambda e: e.activation(out=SB_[k][:], in_=self.bank(b0 + 1), func=AF.Sigmoid), reads=[pb[b0 + 1]], writes=[bSB[k]])
                s.op("dve", lambda e: e.tensor_tensor(out=SA[k][:], in0=SA[k][:], in1=self.bank(b0 + 2), op=ALU.mult),
                     reads=[bSA[k], pb[b0 + 2]], writes=[bSA[k]])
                s.op("dve", lambda e: e.tensor_tensor(out=SB_[k][:], in0=SB_[k][:], in1=self.bank(b0 + 3), op=ALU.mult),
                     reads=[bSB[k], pb[b0 + 3]], writes=[bSB[k]])
                s.op("dve", lambda e: e.tensor_tensor(out=MT[:, dc, :], in0=SA[k][:], in1=SB_[k][:], op=ALU.add),
                     reads=[bSA[k], bSB[k]], writes=[bMT])
                if more:
                    if dc == 2:
                        pro_scale(blk + 1)
                    elif dc == 3:
                        pro_T(blk + 1)
                    if 4 <= dc <= 7:
                        pool_group(blk + 1, dc - 4)
                    if 5 <= dc <= 7:
                        pool_group2(blk + 1, dc - 5)
            if more:
                pool_group2(blk + 1, 3)
            for t in range(4):
                for dh in range(2):
                    bd = (t * 2 + dh) % 2
                    s.op("pe", lambda e: self.mm_group(e, self.bank(bd), [(MT[:, c, t * 128:(t + 1) * 128], WMO[:, c, dh * 512:(dh + 1) * 512]) for c in range(NCH)]),
                         reads=[bMT, CON, CONW], writes=[pb[bd]])
                    s.op("dve", lambda e: e.tensor_tensor(out=X[:, t, dh * 512:(dh + 1) * 512], in0=X[:, t, dh * 512:(dh + 1) * 512], in1=self.bank(bd), op=ALU.add),
                         reads=[bX, pb[bd]], writes=[bX])
            s.dma("sp", self.h2[tok0:tok0 + 512, :].rearrange("(t p) d -> p t d", p=128), X[:], store=bX)

    def phase2b(self, ps):
        nc, s, pb = self.nc, self.s, self.pb
        sb = lambda n, sh, d=F32: ps.enter_context(nc.sbuf_tensor(self.uname(n), list(sh), d))
        NBLK = self.NTOK // 512
        BPS = self.S // 512
        W = self.w
        CON = Buf("con2b")
        ident_b = sb("ident_b", [128, 128], BF16)
        ones_b = sb("ones_b", [128, 128], BF16)
        g2b = sb("g2b", [128, D], F32)
        gmb = sb("gmb", [128, D], F32)
        WXQ = sb("WXQ", [128, NCH, D], BF16)
        WXO = sb("WXO", [128, NCH, D], BF16)
        s.dma("pool", ident_b[:], self.c_ident, load=CON)
        s.dma("pool", ones_b[:], self.c_ones, load=CON)
        s.dma("sp", g2b[:], W["xattn_norm_g"].partition_broadcast(128), load=CON)
        s.dma("sp", gmb[:], W["mem_norm_g"].partition_broadcast(128), load=CON)
        g3b = sb("g3b", [128, D], F32)
        wr = sb("wr", [128, NCH, 36], BF16)
        bb3 = sb("bb3", [128, 36], F32)
        s.dma("sp", g3b[:], W["ffn_norm_g"].partition_broadcast(128), load=CON)
        s.dma("pool", wr[:], W["w_router"].rearrange("(c p) n -> p c n", p=128), load=CON)
        s.dma("sp", bb3[:], W["b_router"].partition_broadcast(128), load=CON)
        CONW = Buf("conw2b")
        s.dma("sp", WXQ[:].rearrange("p c n -> p (c n)"), self.s_wxq, load=CONW)
        s.dma("sp", WXO[:].rearrange("p c n -> p (c n)"), self.s_wxo, load=CONW)
        nrm = self.mk_norm(sb, "b")
        nrm3 = self.mk_norm(sb, "b3", xs=False, junk=False)
        nrm3["JUNK"] = nrm["JUNK"]; nrm3["bJ"] = nrm["bJ"]
        XS3 = sb("XS3", [128, 4, D], BF16); bXS3 = Buf("XS3")
        XT3 = sb("XT3", [128, NCH, 512], BF16); bXT3 = Buf("XT3")
        L4 = sb("L4", [128, 4, 36], F32); bL4 = Buf("L4")
        WT = sb("WT", [128, NCH, 512], BF16); bWT = Buf("WT")
        Xs = [sb("X%d" % i, [128, 4, D], F32) for i in range(2)]; bXs = [Buf("X%d" % i) for i in range(2)]
        HTs = [sb("HT%d" % i, [128, NCH, 512], BF16) for i in range(2)]; bHTs = [Buf("HT%d" % i) for i in range(2)]
        XS4 = sb("XS4", [128, 4, D], BF16); bXS4 = Buf("XS4")
        MX = sb("MX", [128, 2, D], F32); bMX = Buf("MX")
        MXS = sb("MXS", [128, 2, D], BF16); bMXS = Buf("MXS")
        MHT = sb("MHT", [128, NCH, MEM], BF16); bMHT = Buf("MHT")
        KXs = [sb("KX%d" % i, [128, NCH, MEM], BF16) for i in range(2)]; bKXs = [Buf("KX%d" % i) for i in range(2)]
        VXs = [sb("VX%d" % i, [128, 2, D], BF16) for i in range(2)]; bVXs = [Buf("VX%d" % i) for i in range(2)]
        QX = sb("QX", [128, NCH, 512], BF16); bQX = Buf("QX")
        OT = sb("OT", [128, NCH, 512], BF16); bOT = Buf("OT")
        PT = [sb("PT%d" % i, [128, 512], BF16) for i in range(8)]; bPT = [Buf("PT%d" % i) for i in range(8)]
        RL = [sb("RL%d" % i, [128, 512], F32) for i in range(2)]; bRL = [Buf("RL%d" % i) for i in range(2)]
        RLs = sb("RLs", [128, 512], F32); bRLs = Buf("RLs")
        wkv = W["w_xkv"]

        def mem_kv(b):
            KX, bKX, VX, bVX = KXs[b % 2], bKXs[b % 2], VXs[b % 2], bVXs[b % 2]
            s.dma("sp", MX[:], self.mem[b * MEM:(b + 1) * MEM, :].rearrange("(t p) d -> p t d", p=128), load=bMX)
            self.norm_scale4(nrm, lambda t: MX[:, t, :], bMX, gmb, CON, 2, MXS, bMXS)
            self.norm_tr4(MXS, bMXS, 2, MHT, bMHT, ident_b, CON)
            for q4 in range(4):
                s.dma("sp", WT[:].rearrange("p c n -> p (c n)"), self.s_wkv[q4], load=bWT)
                if q4 < 2:
                    for jj in range(4):
                        j = q4 * 4 + jj
                        bk = 1 + (jj % 2)
                        s.op("pe", lambda e, jj=jj, bk=bk: self.mm_group(e, self.bank(bk)[:, 0:MEM], [(WT[:, c, jj * 128:(jj + 1) * 128], MHT[:, c, :]) for c in range(NCH)]),
                             reads=[bWT, bMHT], writes=[pb[bk]])
                        s.op("act", lambda e, j=j, bk=bk: e.copy(out=KX[:, j, :], in_=self.bank(bk)[:, 0:MEM]), reads=[pb[bk]], writes=[bKX])
                else:
                    half = q4 - 2
                    for mc in range(2):
                        bk = 1 + mc
                        s.op("pe", lambda e, mc=mc, bk=bk: self.mm_group(e, self.bank(bk), [(MHT[:, c, mc * 128:(mc + 1) * 128], WT[:, c, :]) for c in range(NCH)]),
                             reads=[bWT, bMHT], writes=[pb[bk]])
                        s.op("act", lambda e, mc=mc, bk=bk, half=half: e.copy(out=VX[:, mc, half * 512:(half + 1) * 512], in_=self.bank(bk)),
                             reads=[pb[bk]], writes=[bVX])

        def pro_load(blk):
            tok0 = blk * 512
            s.dma("sp", Xs[blk % 2][:], self.h2[tok0:tok0 + 512, :].rearrange("(t p) d -> p t d", p=128), load=bXs[blk % 2])

        def pro_scale(blk):
            X = Xs[blk % 2]
            self.norm_scale4(nrm, lambda t: X[:, t, :], bXs[blk % 2], g2b, CON, 4, XS4, bXS4)

        def pro_T(blk):
            self.norm_tr4(XS4, bXS4, 4, HTs[blk % 2], bHTs[blk % 2], ident_b, CON, banks=(0, 3))

        def ffn_scale(blk):
            X = Xs[blk % 2]
            self.norm_scale4(nrm3, lambda t: X[:, t, :], bXs[blk % 2], g3b, CON, 4, XS3, bXS3)
            s.dma("sp", self.hn3[blk * 512:(blk + 1) * 512, :].rearrange("(t p) d -> p t d", p=128), XS3[:], store=bXS3)

        def router_part(blk):
            self.norm_tr4(XS3, bXS3, 4, XT3, bXT3, ident_b, CON, banks=(0, 6))
            def rl(e):
                ins = None
                for t in range(4):
                    for c in range(NCH):
                        ins = e.matmul(out=self.bank(2)[:, t * 36:(t + 1) * 36], lhsT=XT3[:, c, t * 128:(t + 1) * 128], rhs=wr[:, c, :],
                                       start=(c == 0), stop=(c == NCH - 1))
                return ins
            s.op("pe", rl, reads=[bXT3, CON], writes=[pb[2]])
            s.op("dve", lambda e: e.tensor_tensor(out=L4[:], in0=self.bank(2)[:, 0:144].rearrange("p (t n) -> p t n", t=4),
                                                  in1=bb3[:].unsqueeze(1).broadcast_to([128, 4, 36]), op=ALU.add), reads=[pb[2], CON], writes=[bL4])
            s.dma("sp", self.lscr[blk * 512:(blk + 1) * 512, :].rearrange("(t p) n -> p t n", p=128), L4[:], store=bL4)

        mem_kv(0)
        pro_load(0); pro_scale(0); pro_T(0)
        for blk in range(NBLK):
            tok0 = blk * 512
            b = blk // BPS
            X, bX, HT, bHT = Xs[blk % 2], bXs[blk % 2], HTs[blk % 2], bHTs[blk % 2]
            KX, bKX, VX, bVX = KXs[b % 2], bKXs[b % 2], VXs[b % 2], bVXs[b % 2]
            if blk + 1 < NBLK:
                pro_load(blk + 1)
            for j in range(NCH):
                bk = 1 + (j % 2)
                s.op("pe", lambda e, j=j, bk=bk: self.mm_group(e, self.bank(bk), [(WXQ[:, c, j * 128:(j + 1) * 128], HT[:, c, :]) for c in range(NCH)]),
                     reads=[bHT, CON, CONW], writes=[pb[bk]])
                if j % 2 == 0:
                    s.op("act", lambda e, j=j, bk=bk: e.copy(out=QX[:, j, :], in_=self.bank(bk)), reads=[pb[bk]], writes=[bQX])
                else:
                    s.op("dve", lambda e, j=j, bk=bk: e.tensor_copy(out=QX[:, j, :], in_=self.bank(bk)), reads=[pb[bk]], writes=[bQX])
            if blk > 0:
                router_part(blk - 1)
            if blk + 1 < NBLK:
                if (blk + 1) % BPS == 0:
                    mem_kv((blk + 1) // BPS)
                pro_scale(blk + 1)
            for h in range(MH):
                for mc in range(2):
                    bs = 3 + mc
                    pi = h * 2 + mc
                    s.op("pe", lambda e: self.mm_group(e, self.bank(bs), [(KX[:, h * 2 + dh, mc * 128:(mc + 1) * 128], QX[:, h * 2 + dh, :]) for dh in range(2)]),
                         reads=[bKX, bQX], writes=[pb[bs]])
                    s.op("act", lambda e: e.activation(out=PT[pi][:], in_=self.bank(bs), func=AF.Exp, scale=1.0 / math.sqrt(MHD)),
                         reads=[pb[bs]], writes=[bPT[pi]])
            for h in range(MH):
                hp = h % 2
                s.op("pe", lambda e: self.mm_group(e, self.bank(5), [(ones_b[:], PT[h * 2 + mc][:]) for mc in range(2)]),
                     reads=[bPT[h * 2], bPT[h * 2 + 1], CON], writes=[pb[5]])
                s.op("act", lambda e: e.activation(out=RLs[:], in_=self.bank(5), func=AF.Ln), reads=[pb[5]], writes=[bRLs])
                s.op("act", lambda e: e.activation(out=RL[hp][:], in_=RLs[:], func=AF.Exp, scale=-1.0), reads=[bRLs], writes=[bRL[hp]])
                for dvh in range(2):
                    bo = 6 + dvh
                    s.op("pe", lambda e: self.mm_group(e, self.bank(bo), [(VX[:, mc, h * 256 + dvh * 128:h * 256 + (dvh + 1) * 128], PT[h * 2 + mc][:]) for mc in range(2)]),
                         reads=[bVX, bPT[h * 2], bPT[h * 2 + 1]], writes=[pb[bo]])
                    s.op("dve", lambda e: e.tensor_tensor(out=OT[:, h * 2 + dvh, :], in0=RL[hp][:], in1=self.bank(bo), op=ALU.mult),
                         reads=[bRL[hp], pb[bo]], writes=[bOT])
            if blk + 1 < NBLK:
                pro_T(blk + 1)
            for t in range(4):
                for dh in range(2):
                    bd = 1 + (t * 2 + dh) % 2
                    s.op("pe", lambda e, t=t, dh=dh, bd=bd: self.mm_group(e, self.bank(bd), [(OT[:, c, t * 128:(t + 1) * 128], WXO[:, c, dh * 512:(dh + 1) * 512]) for c in range(NCH)]),
                         reads=[bOT, CON, CONW], writes=[pb[bd]])
                    s.op("dve", lambda e, t=t, dh=dh, bd=bd: e.tensor_tensor(out=X[:, t, dh * 512:(dh + 1) * 512], in0=X[:, t, dh * 512:(dh + 1) * 512],
                                                                            in1=self.bank(bd), op=ALU.add),
                         reads=[bX, pb[bd]], writes=[bX])
            s.dma("sp", self.h2[tok0:tok0 + 512, :].rearrange("(t p) d -> p t d", p=128), X[:], store=bX)
            ffn_scale(blk)
        router_part(NBLK - 1)

    def prep_dense_scratch(self):
        s, W = self.s, self.w
        b = Buf("DSCR")
        v = lambda ap, c: ap.rearrange("p (c n) -> p c n", c=c)
        win_v = W["w_in"].rearrange("(c p) n -> p c n", p=128)
        for half in range(2):
            s.dma("pool", v(self.s_win2, NCH)[:, :, half * 1280:(half + 1) * 1280], win_v[:, :, C_POOL + half * 1280:C_POOL + (half + 1) * 1280], load=b)
        s.dma("pool", v(self.s_wab, 4), W["w_attn_branch"].rearrange("(c p) n -> p c n", p=128), load=b)
        s.dma("pool", v(self.s_pw, 4), W["pool_w"].rearrange("g c e -> c g e"), load=b)
        s.dma("pool", v(self.s_wpb, 4), W["w_pool_branch"].rearrange("(c p) n -> p c n", p=128), load=b)
        s.dma("pool", v(self.s_wmo, NCH), W["w_mix_out"].rearrange("(c p) n -> p c n", p=128), load=b)
        s.dma("pool", v(self.s_wxq, NCH), W["w_xq"].rearrange("(c p) n -> p c n", p=128), load=b)
        s.dma("pool", v(self.s_wxo, NCH), W["w_xo"].rearrange("(c p) n -> p c n", p=128), load=b)
        kv_v = W["w_xkv"].rearrange("(c p) n -> p c n", p=128)
        for q4 in range(4):
            s.dma("pool", v(self.s_wkv[q4], NCH), kv_v[:, :, q4 * 512:(q4 + 1) * 512], load=b)

    def prep_expert_scratch(self):
        s = self.s
        self.bWSCR = Buf("WSCR")
        for e in range(NE):
            rows = slice(e * 128, (e + 1) * 128)
            s.dma("pool", self.wgs[rows, :].rearrange("p (c f) -> p c f", c=NCH), self.w["w_exp_gate"][e].rearrange("(c p) f -> p c f", p=128), load=self.bWSCR)
            s.dma("pool", self.wus[rows, :].rearrange("p (c f) -> p c f", c=NCH), self.w["w_exp_up"][e].rearrange("(c p) f -> p c f", p=128), load=self.bWSCR)
            s.dma("pool", self.wds[rows, :].rearrange("p (c d) -> p c d", c=2), self.w["w_exp_down"][e].rearrange("(c p) d -> p c d", p=128), load=self.bWSCR)

    def phase3(self, ps):
        nc, s, pb = self.nc, self.s, self.pb
        sb = lambda n, sh, d=F32: ps.enter_context(nc.sbuf_tensor(self.uname(n), list(sh), d))
        NT = self.NTOK // 128
        NBK = self.NBK3
        TM = min(1024, self.NTOK)
        NMT = self.NTOK // TM
        NTT = TM // 128
        MAGIC = 12582912.0
        CON = Buf("con3")
        ident_b = sb("ident_b", [128, 128], BF16)
        ones_b = sb("ones_b", [128, 128], BF16)
        tri_b = sb("tri_b", [128, 128], BF16)
        kb = sb("kb", [128, NBK], F32)
        ep = sb("ep", [128, 8], F32)
        ones64 = sb("ones64", [128, max(NT, 32)], F32)
        g3b = sb("g3b", [128, D], F32)
        gfb = sb("gfb", [128, D], F32)
        wr = sb("wr", [128, NCH, 36], BF16)
        bb = sb("bb", [128, 36], F32)
        s.dma("pool", ident_b[:], self.c_ident, load=CON)
        s.dma("pool", ones_b[:], self.c_ones, load=CON)
        s.dma("pool", tri_b[:], self.c_tri, load=CON)
        s.dma("sp", kb[:], self.c_kb[:, 0:NBK], load=CON)
        s.dma("sp", ep[:], self.c_ep, load=CON)
        s.dma("sp", g3b[:], self.w["ffn_norm_g"].partition_broadcast(128), load=CON)
        s.dma("sp", gfb[:], self.w["final_norm_g"].partition_broadcast(128), load=CON)
        s.dma("pool", wr[:], self.w["w_router"].rearrange("(c p) n -> p c n", p=128), load=CON)
        s.dma("sp", bb[:], self.w["b_router"].partition_broadcast(128), load=CON)
        s.op("dve", lambda e: e.memset(ones64[:], 1.0), writes=[CON])
        OHb = [sb("OHb%d" % k, [128, NT, 32], BF16) for k in range(2)]; bOHb = [Buf("OHb%d" % k) for k in range(2)]
        W12 = sb("W12", [128, 2, NT], F32); bW12 = Buf("W12")
        DEST = sb("DEST", [128, 2, NT], I32); bDEST = Buf("DEST")
        IDXW = sb("IDXW", [128, NBK], I32); bIDXW = Buf("IDXW")
        nrm = self.mk_norm(sb, "3", xs=False)

        L = sb("L", [128, NT, 36], F32); bL = Buf("L")
        s.dma("sp", L[:], self.lscr.rearrange("(t p) n -> p t n", p=128), load=bL)
        with ExitStack() as p1b:
            sb1 = lambda n, sh, d=F32: p1b.enter_context(nc.sbuf_tensor(self.uname(n), list(sh), d))
            rr = [sb1("r%d" % i, [128, NT], F32) for i in range(6)]
            br = [Buf("r%d" % i) for i in range(6)]
            r1, r2, r3, r4, r5 = rr[1], rr[2], rr[3], rr[4], rr[5]
            dg = sb1("dg", [128, NT, 4], F32); bdg = Buf("dg")
            eg = sb1("eg", [128, NT, 4], F32); beg = Buf("eg")
            oh = sb1("oh", [128, NT, 4], F32); boh = Buf("oh")
            tmp = sb1("tmp", [128, NT, 4, 8], F32); btmp = Buf("tmp")
            sl = sb1("sl", [128, NT, 8], F32); bsl = Buf("sl")
            sl2 = sb1("sl2", [128, NT, 8], F32); bsl2 = Buf("sl2")
            eq1 = sb1("eq1", [128, NT, 8], F32); beq1 = Buf("eq1")
            eq2 = sb1("eq2", [128, NT, 8], F32); beq2 = Buf("eq2")
            Lg = L[:, :, 0:4]
            Le = L[:, :, 4:36].rearrange("p t (g e) -> p t g e", g=4)
            s.op("dve", lambda e: e.tensor_reduce(out=r1[:], in_=Lg, axis=AX.X, op=ALU.max), reads=[bL], writes=[br[1]])
            s.op("dve", lambda e: e.tensor_tensor(out=dg[:], in0=Lg, in1=_bc_last(r1[:], 4), op=ALU.subtract), reads=[bL, br[1]], writes=[bdg])
            s.op("act", lambda e: e.activation(out=eg[:], in_=dg[:], func=AF.Exp), reads=[bdg], writes=[beg])
            s.op("dve", lambda e: e.tensor_reduce(out=r2[:], in_=eg[:], axis=AX.X, op=ALU.add), reads=[beg], writes=[br[2]])
            s.op("dve", lambda e: e.reciprocal(out=r2[:], in_=r2[:]), reads=[br[2]], writes=[br[2]])
            s.op("dve", lambda e: e.tensor_single_scalar(out=oh[:], in_=dg[:], scalar=0.0, op=ALU.is_ge), reads=[bdg], writes=[boh])
            s.op("dve", lambda e: e.tensor_tensor(out=tmp[:], in0=Le, in1=_bc_last(oh[:], 8), op=ALU.mult), reads=[bL, boh], writes=[btmp])
            s.op("dve", lambda e: e.tensor_reduce(out=sl[:], in_=tmp[:].rearrange("p t g e -> p t e g"), axis=AX.X, op=ALU.add), reads=[btmp], writes=[bsl])
            s.op("dve", lambda e: e.tensor_reduce(out=r3[:], in_=sl[:], axis=AX.X, op=ALU.max), reads=[bsl], writes=[br[3]])
            s.op("dve", lambda e: e.tensor_tensor(out=eq1[:], in0=sl[:], in1=_bc_last(r3[:], 8), op=ALU.is_equal), reads=[bsl, br[3]], writes=[beq1])
            s.op("dve", lambda e: e.scalar_tensor_tensor(out=sl2[:], in0=eq1[:], scalar=-1e30, in1=sl[:], op0=ALU.mult, op1=ALU.add), reads=[beq1, bsl], writes=[bsl2])
            s.op("dve", lambda e: e.tensor_reduce(out=r4[:], in_=sl2[:], axis=AX.X, op=ALU.max), reads=[bsl2], writes=[br[4]])
            s.op("dve", lambda e: e.tensor_tensor(out=eq2[:], in0=sl2[:], in1=_bc_last(r4[:], 8), op=ALU.is_equal), reads=[bsl2, br[4]], writes=[beq2])
            for k, (eq, beq) in enumerate([(eq1, beq1), (eq2, beq2)]):
                s.op("dve", lambda e: e.tensor_tensor(out=OHb[k][:].rearrange("p t (g e) -> p t g e", g=4), in0=_bc_last(oh[:], 8),
                                                      in1=eq[:].unsqueeze(2).broadcast_to([128, NT, 4, 8]), op=ALU.mult), reads=[boh, beq], writes=[bOHb[k]])
            s.op("dve", lambda e: e.tensor_tensor(out=r5[:], in0=r4[:], in1=r3[:], op=ALU.subtract), reads=[br[3], br[4]], writes=[br[5]])
            s.op("act", lambda e: e.activation(out=r5[:], in_=r5[:], func=AF.Exp), reads=[br[5]], writes=[br[5]])
            s.op("dve", lambda e: e.tensor_scalar(out=r3[:], in0=r5[:], scalar1=1.0, scalar2=None, op0=ALU.add), reads=[br[5]], writes=[br[3]])
            s.op("dve", lambda e: e.reciprocal(out=r3[:], in_=r3[:]), reads=[br[3]], writes=[br[3]])
            s.op("dve", lambda e: e.tensor_tensor(out=W12[:, 0, :], in0=r3[:], in1=r2[:], op=ALU.mult), reads=[br[3], br[2]], writes=[bW12])
            s.op("dve", lambda e: e.tensor_tensor(out=W12[:, 1, :], in0=W12[:, 0, :], in1=r5[:], op=ALU.mult), reads=[bW12, br[5]], writes=[bW12])
            NC32 = NT * 32
            PW = [sb1("PW%d" % k, [128, NT, 32], F32) for k in range(2)]; bPW = [Buf("PW%d" % k) for k in range(2)]
            TOT = [sb1("TOT%d" % k, [128, NT, 32], F32) for k in range(2)]; bTOT = [Buf("TOT%d" % k) for k in range(2)]
            INC = [sb1("INC%d" % k, [128, 32, NT], F32) for k in range(2)]; bINC = [Buf("INC%d" % k) for k in range(2)]
            CN = sb1("CN", [128, 32], F32); bCN = Buf("CN")
            PC = sb1("PC", [128, 32], F32); bPC = Buf("PC")
            BINC = sb1("BINC", [128, 32], F32); bBINC = Buf("BINC")
            BASE = sb1("BASE", [128, 2, 32], F32); bBASE = Buf("BASE")
            DF = sb1("DF", [128, 2, NT], F32); bDF = Buf("DF")
            CMP = sb1("CMP", [128, NBK, 31], F32); bCMP = Buf("CMP")
            EK = sb1("EK", [128, NBK], F32); bEK = Buf("EK")
            for k in range(2):
                flat = OHb[k][:].rearrange("p t e -> p (t e)")
                for q in range(0, NC32, 512):
                    w_ = min(512, NC32 - q)
                    bk = 4 + (q // 512) % 2
                    s.op("pe", lambda e: e.matmul(out=self.bank(bk)[:, 0:w_], lhsT=tri_b[:], rhs=flat[:, q:q + w_], start=True, stop=True), reads=[bOHb[k], CON], writes=[pb[bk]])
                    s.op("act", lambda e: e.copy(out=PW[k][:].rearrange("p t e -> p (t e)")[:, q:q + w_], in_=self.bank(bk)[:, 0:w_]), reads=[pb[bk]], writes=[bPW[k]])
                    bk2 = 6 + (q // 512) % 2
                    s.op("pe", lambda e: e.matmul(out=self.bank(bk2)[:, 0:w_], lhsT=ones_b[:], rhs=flat[:, q:q + w_], start=True, stop=True), reads=[bOHb[k], CON], writes=[pb[bk2]])
                    s.op("dve", lambda e: e.tensor_copy(out=TOT[k][:].rearrange("p t e -> p (t e)")[:, q:q + w_], in_=self.bank(bk2)[:, 0:w_]), reads=[pb[bk2]], writes=[bTOT[k]])
                for ex in range(32):
                    s.op("dve", lambda e: e.tensor_tensor_scan(out=INC[k][:, ex, :], data0=ones64[:, 0:NT], data1=TOT[k][:, :, ex], initial=0.0, op0=ALU.mult, op1=ALU.add),
                         reads=[bTOT[k], CON], writes=[bINC[k]])
            s.op("dve", lambda e: e.tensor_tensor(out=CN[:], in0=INC[0][:, :, NT - 1], in1=INC[1][:, :, NT - 1], op=ALU.add), reads=[bINC[0], bINC[1]], writes=[bCN])
            s.op("dve", lambda e: e.tensor_scalar(out=PC[:], in0=CN[:], scalar1=511.0, scalar2=1.0 / 512, op0=ALU.add, op1=ALU.mult), reads=[bCN], writes=[bPC])
            s.op("dve", lambda e: e.tensor_scalar(out=PC[:], in0=PC[:], scalar1=-0.5 + 1.0 / 2048, scalar2=None, op0=ALU.add), reads=[bPC], writes=[bPC])
            s.op("dve", lambda e: e.tensor_scalar(out=PC[:], in0=PC[:], scalar1=MAGIC, scalar2=None, op0=ALU.add), reads=[bPC], writes=[bPC])
            s.op("dve", lambda e: e.tensor_scalar(out=PC[:], in0=PC[:], scalar1=-MAGIC, scalar2=512.0, op0=ALU.add, op1=ALU.mult), reads=[bPC], writes=[bPC])
            s.op("dve", lambda e: e.tensor_tensor_scan(out=BINC[:], data0=ones64[:, 0:32], data1=PC[:], initial=0.0, op0=ALU.mult, op1=ALU.add), reads=[bPC, CON], writes=[bBINC])
            s.op("dve", lambda e: e.tensor_tensor(out=BASE[:, 0, :], in0=BINC[:], in1=PC[:], op=ALU.subtract), reads=[bBINC, bPC], writes=[bBASE])
            s.op("dve", lambda e: e.tensor_tensor(out=BASE[:, 1, :], in0=BASE[:, 0, :], in1=INC[0][:, :, NT - 1], op=ALU.add), reads=[bBASE, bINC[0]], writes=[bBASE])
            for k in range(2):
                s.op("dve", lambda e: e.tensor_tensor(out=PW[k][:], in0=PW[k][:], in1=INC[k][:].rearrange("p e t -> p t e"), op=ALU.add), reads=[bPW[k], bINC[k]], writes=[bPW[k]])
                s.op("dve", lambda e: e.tensor_tensor(out=PW[k][:], in0=PW[k][:], in1=TOT[k][:], op=ALU.subtract), reads=[bPW[k], bTOT[k]], writes=[bPW[k]])
                s.op("dve", lambda e: e.tensor_tensor(out=PW[k][:], in0=PW[k][:], in1=BASE[:, k, :].unsqueeze(1).broadcast_to([128, NT, 32]), op=ALU.add), reads=[bPW[k], bBASE], writes=[bPW[k]])
                s.op("dve", lambda e: e.tensor_tensor(out=PW[k][:], in0=PW[k][:], in1=OHb[k][:], op=ALU.mult), reads=[bPW[k], bOHb[k]], writes=[bPW[k]])
                s.op("dve", lambda e: e.tensor_reduce(out=DF[:, k, :], in_=PW[k][:], axis=AX.X, op=ALU.add), reads=[bPW[k]], writes=[bDF])
            s.op("dve", lambda e: e.tensor_copy(out=DEST[:], in_=DF[:]), reads=[bDF], writes=[bDEST])
            s.op("dve", lambda e: e.tensor_tensor(out=CMP[:], in0=_bc_last(kb[:], 31), in1=BINC[:, 0:31].unsqueeze(1).broadcast_to([128, NBK, 31]), op=ALU.is_ge),
                 reads=[bBINC, CON], writes=[bCMP])
            s.op("dve", lambda e: e.tensor_reduce(out=EK[:], in_=CMP[:], axis=AX.X, op=ALU.add), reads=[bCMP], writes=[bEK])
            s.op("dve", lambda e: e.tensor_scalar(out=EK[:], in0=EK[:], scalar1=128.0, scalar2=ep[:, 0:1], op0=ALU.mult, op1=ALU.add), reads=[bEK, CON], writes=[bEK])
            s.op("dve", lambda e: e.tensor_copy(out=IDXW[:], in_=EK[:]), reads=[bEK], writes=[bIDXW])
            NXR = 8
            XR = [sb1("XR%d" % i, [128, D + 8], BF16) for i in range(NXR)]; bXR = [Buf("XR%d" % i) for i in range(NXR)]
            s.barrier(["sp", "pool"])
            for t in range(NT):
                ia, ib = (2 * t) % NXR, (2 * t + 1) % NXR
                s.dma("sp", XR[ia][:, 0:D], self.hn3[t * 128:(t + 1) * 128, :], load=bXR[ia])
                s.op("dve", lambda e: e.tensor_copy(out=XR[ib][:, 0:D], in_=XR[ia][:, 0:D]), reads=[bXR[ia]], writes=[bXR[ib]])
                for k, i in ((0, ia), (1, ib)):
                    s.op("dve", lambda e: e.tensor_copy(out=XR[i][:, D:D + 1], in_=W12[:, k, t:t + 1]), reads=[bW12], writes=[bXR[i]])
                    s.idma(self.sortx, XR[i][:], DEST[:, k, t:t + 1], scatter=True, buf=bXR[i], idxbuf=bDEST)
            s.barrier()
        with ExitStack() as p2:
            sb2 = lambda n, sh, d=F32: p2.enter_context(nc.sbuf_tensor(self.uname(n), list(sh), d))
            XSS = [sb2("XSS%d" % i, [128, 4, D + 8], BF16) for i in range(3)]; bXSS = [Buf("XSS%d" % i) for i in range(3)]
            XT = [sb2("XT%d" % i, [128, NCH, 512], BF16) for i in range(2)]; bXT = [Buf("XT%d" % i) for i in range(2)]
            CWT = [sb2("CWT%d" % i, [1, 512], BF16) for i in range(2)]; bCWT = [Buf("CWT%d" % i) for i in range(2)]
            WG = [sb2("WG%d" % i, [128, NCH * FF], BF16) for i in range(3)]; bWG = [Buf("WG%d" % i) for i in range(3)]
            WU = [sb2("WU%d" % i, [128, NCH * FF], BF16) for i in range(3)]; bWU = [Buf("WU%d" % i) for i in range(3)]
            WD = [sb2("WD%d" % i, [128, 2, D], BF16) for i in range(3)]; bWD = [Buf("WD%d" % i) for i in range(3)]
            HID = [sb2("HID%d" % i, [128, 2, 512], BF16) for i in range(2)]; bHID = [Buf("HID%d" % i) for i in range(2)]
            SG = [sb2("SG%d" % i, [128, 512], BF16) for i in range(2)]; bSG = [Buf("SG%d" % i) for i in range(2)]
            T1 = [sb2("T1%d" % i, [128, 512], BF16) for i in range(2)]; bT1 = [Buf("T1%d" % i) for i in range(2)]
            MO = [sb2("MO%d" % i, [128, 4, D], F32) for i in range(2)]; bMO = [Buf("MO%d" % i) for i in range(2)]

            def load_blk(k):
                i = k % 3
                s.dma("sp", XSS[i][:], self.sortx[k * 512:(k + 1) * 512, :].rearrange("(t p) c -> p t c", p=128), load=bXSS[i])
                s.idma(WG[i][:], self.wgs, IDXW[:, k:k + 1], scatter=False, buf=bWG[i], idxbuf=bIDXW)
                s.idma(WU[i][:], self.wus, IDXW[:, k:k + 1], scatter=False, buf=bWU[i], idxbuf=bIDXW)
                s.idma(WD[i][:].rearrange("p c d -> p (c d)"), self.wds, IDXW[:, k:k + 1], scatter=False, buf=bWD[i], idxbuf=bIDXW)

            def front(k):
                i = k % 2
                w3 = k % 3
                def trcw(e):
                    ins = None
                    for t in range(4):
                        ins = e.transpose(out=self.bank(1, BF16)[0:1, t * 128:(t + 1) * 128], in_=XSS[w3][:, t, D:D + 1], identity=ident_b[:])
                    return ins
                s.op("pe", trcw, reads=[bXSS[w3], CON], writes=[pb[1]])
                s.op("act", lambda e: e.copy(out=CWT[i][:], in_=self.bank(1, BF16)[0:1, 0:512]), reads=[pb[1]], writes=[bCWT[i]])
                for t in range(4):
                    tb = t % 2
                    def tr(e):
                        ins = None
                        for c in range(NCH):
                            ins = e.transpose(out=self.bank(tb, BF16)[:, c * 128:(c + 1) * 128], in_=XSS[w3][:, t, c * 128:(c + 1) * 128], identity=ident_b[:])
                        return ins
                    s.op("pe", tr, reads=[bXSS[w3], CON], writes=[pb[tb]])
                    if t % 2 == 0:
                        s.op("act", lambda e: e.copy(out=XT[i][:, :, t * 128:(t + 1) * 128], in_=self.bank(tb, BF16).rearrange("p (c t) -> p c t", c=NCH)), reads=[pb[tb]], writes=[bXT[i]])
                    else:
                        s.op("dve", lambda e: e.tensor_copy(out=XT[i][:, :, t * 128:(t + 1) * 128], in_=self.bank(tb, BF16).rearrange("p (c t) -> p c t", c=NCH)), reads=[pb[tb]], writes=[bXT[i]])
                for fc in range(2):
                    bg, bu = 4 + fc, 6 + fc
                    s.op("pe", lambda e: self.mm_group(e, self.bank(bg), [(WG[w3][:, c * FF + fc * 128:c * FF + (fc + 1) * 128], XT[i][:, c, :]) for c in range(NCH)]),
                         reads=[bWG[w3], bXT[i]], writes=[pb[bg]])
                    s.op("pe", lambda e: self.mm_group(e, self.bank(bu), [(WU[w3][:, c * FF + fc * 128:c * FF + (fc + 1) * 128], XT[i][:, c, :]) for c in range(NCH)]),
                         reads=[bWU[w3], bXT[i]], writes=[pb[bu]])
                    if fc == 0:
                        s.op("pe", lambda e: e.matmul(out=self.bank(1), lhsT=ones_b[0:1, :], rhs=CWT[i][:], start=True, stop=True), reads=[bCWT[i], CON], writes=[pb[1]])
                    s.op("act", lambda e: e.activation(out=SG[fc][:], in_=self.bank(bg), func=AF.Silu), reads=[pb[bg]], writes=[bSG[fc]])
                    s.op("dve", lambda e: e.tensor_tensor(out=T1[fc][:], in0=SG[fc][:], in1=self.bank(bu), op=ALU.mult), reads=[bSG[fc], pb[bu]], writes=[bT1[fc]])
                    s.op("dve", lambda e: e.tensor_tensor(out=HID[i][:, fc, :], in0=T1[fc][:], in1=self.bank(1), op=ALU.mult), reads=[bT1[fc], pb[1]], writes=[bHID[i]])

            def back(k):
                i = k % 2
                w3 = k % 3
                n = 0
                for t in range(4):
                    for dh in range(2):
                        bd = 2 + (n % 2)
                        s.op("pe", lambda e: self.mm_group(e, self.bank(bd), [(HID[i][:, fc, t * 128:(t + 1) * 128], WD[w3][:, fc, dh * 512:(dh + 1) * 512]) for fc in range(2)]),
                             reads=[bHID[i], bWD[w3]], writes=[pb[bd]])
                        dst = MO[i][:, t, dh * 512:(dh + 1) * 512]
                        if n % 2 == 0:
                            s.op("act", lambda e: e.copy(out=dst, in_=self.bank(bd)), reads=[pb[bd]], writes=[bMO[i]])
                        else:
                            s.op("dve", lambda e: e.tensor_copy(out=dst, in_=self.bank(bd)), reads=[pb[bd]], writes=[bMO[i]])
                        n += 1
                s.dma("sp", self.sortout[k * 512:(k + 1) * 512, :].rearrange("(t p) d -> p t d", p=128), MO[i][:], store=bMO[i])

            for k0 in range(min(3, NBK)):
                load_blk(k0)
            front(0)
            for k in range(NBK):
                if k + 1 < NBK:
                    front(k + 1)
                back(k)
                if k + 3 < NBK:
                    load_blk(k + 3)
            s.barrier()
        with ExitStack() as p3:
            sb3 = lambda n, sh, d=F32: p3.enter_context(nc.sbuf_tensor(self.uname(n), list(sh), d))
            NB4 = 6
            G1 = [sb3("G1%d" % i, [128, D], F32) for i in range(NB4)]; bG1 = [Buf("G1%d" % i) for i in range(NB4)]
            G2 = [sb3("G2%d" % i, [128, D], F32) for i in range(NB4)]; bG2 = [Buf("G2%d" % i) for i in range(NB4)]
            Hh = [sb3("Hh%d" % i, [128, D], F32) for i in range(NB4)]; bHh = [Buf("Hh%d" % i) for i in range(NB4)]
            sq = [sb3("sq%d" % i, [128, 1], F32) for i in range(NB4)]; bsq = [Buf("sq%d" % i) for i in range(NB4)]

            def fetch(t):
                k = t % NB4
                s.idma(G1[k][:], self.sortout, DEST[:, 0, t:t + 1], scatter=False, buf=bG1[k], idxbuf=bDEST)
                s.idma(G2[k][:], self.sortout, DEST[:, 1, t:t + 1], scatter=False, buf=bG2[k], idxbuf=bDEST)
                s.dma("sp", Hh[k][:], self.h2[t * 128:(t + 1) * 128, :], load=bHh[k])

            for t in range(min(NB4 - 1, NT)):
                fetch(t)
            for t in range(NT):
                k = t % NB4
                if t + NB4 - 1 < NT:
                    fetch(t + NB4 - 1)
                s.op("dve", lambda e: e.tensor_tensor(out=G1[k][:], in0=G1[k][:], in1=G2[k][:], op=ALU.add), reads=[bG1[k], bG2[k]], writes=[bG1[k]])
                s.op("dve", lambda e: e.tensor_tensor(out=Hh[k][:], in0=Hh[k][:], in1=G1[k][:], op=ALU.add), reads=[bHh[k], bG1[k]], writes=[bHh[k]])
                s.op("act", lambda e: e.activation(out=G2[k][:], in_=Hh[k][:], func=AF.Square, accum_out=sq[k][:]), reads=[bHh[k]], writes=[bG2[k], bsq[k]])
                s.op("act", lambda e: e.activation(out=sq[k][:], in_=sq[k][:], func=AF.Ln, scale=1.0 / D, bias=nrm["epsc"][:]), reads=[bsq[k]], writes=[bsq[k]])
                s.op("act", lambda e: e.activation(out=sq[k][:], in_=sq[k][:], func=AF.Exp, scale=-0.5), reads=[bsq[k]], writes=[bsq[k]])
                s.op("dve", lambda e: e.scalar_tensor_tensor(out=G1[k][:], in0=Hh[k][:], scalar=sq[k][:], in1=gfb[:], op0=ALU.mult, op1=ALU.mult),
                     reads=[bHh[k], bsq[k], CON], writes=[bG1[k]])
                s.dma("sp", self.out[t * 128:(t + 1) * 128, :], G1[k][:], store=bG1[k])

    def phase3_group(self, ps):
        nc, s, pb = self.nc, self.s, self.pb
        sb = lambda n, sh, d=F32: ps.enter_context(nc.sbuf_tensor(self.uname(n), list(sh), d))
        NT = self.NTOK // 128
        NBK = self.NTOK // 512 + 4
        TM = min(1024, self.NTOK)
        NMT = self.NTOK // TM
        NTT = TM // 128
        MAGIC = 12582912.0
        CON = Buf("con3")
        ident_b = sb("ident_b", [128, 128], BF16)
        ones_b = sb("ones_b", [128, 128], BF16)
        tri_b = sb("tri_b", [128, 128], BF16)
        sel = sb("sel", [8, 8 * 128], BF16)
        kb = sb("kb", [128, NBK], F32)
        ep = sb("ep", [128, 8], F32)
        ones64 = sb("ones64", [128, NT], F32)
        g3b = sb("g3b", [128, D], F32)
        gfb = sb("gfb", [128, D], F32)
        wr = sb("wr", [128, NCH, 36], BF16)
        bb = sb("bb", [128, NTT, 36], F32)
        s.dma("pool", ident_b[:], self.c_ident, load=CON)
        s.dma("pool", ones_b[:], self.c_ones, load=CON)
        s.dma("pool", tri_b[:], self.c_tri, load=CON)
        s.dma("pool", sel[:], self.c_sel[0:8, 0:1024], load=CON)
        s.dma("sp", kb[:], self.c_kb[:, 0:NBK], load=CON)
        s.dma("sp", ep[:], self.c_ep, load=CON)
        s.dma("sp", g3b[:], self.w["ffn_norm_g"].partition_broadcast(128), load=CON)
        s.dma("sp", gfb[:], self.w["final_norm_g"].partition_broadcast(128), load=CON)
        s.dma("pool", wr[:], self.w["w_router"].rearrange("(c p) n -> p c n", p=128), load=CON)
        for t in range(NTT):
            s.dma("sp", bb[:, t, :], self.w["b_router"].partition_broadcast(128), load=CON)
        s.op("dve", lambda e: e.memset(ones64[:], 1.0), writes=[CON])
        OHall = sb("OHall", [128, NT, 4], F32); bOH = Buf("OHall")
        CW8all = sb("CW8all", [128, NT, 8], BF16); bCW8 = Buf("CW8all")
        DEST = sb("DEST", [128, NT], I32); bDEST = Buf("DEST")
        IDXW = sb("IDXW", [128, NBK * 8], I32); bIDXW = Buf("IDXW")
        nrm = self.mk_norm(sb, "3")

        with ExitStack() as p1:
            sb1 = lambda n, sh, d=F32: p1.enter_context(nc.sbuf_tensor(self.uname(n), list(sh), d))
            H = sb1("H", [128, NTT, D], F32); bH = Buf("H")
            XT = sb1("XT", [128, NCH, TM], BF16); bXT = Buf("XT")
            L = sb1("L", [128, NTT, 36], F32); bL = Buf("L")
            rr = [sb1("r%d" % i, [128, NTT], F32) for i in range(6)]
            br = [Buf("r%d" % i) for i in range(6)]
            r1, r2, r3, r4, r5 = rr[1], rr[2], rr[3], rr[4], rr[5]
            dg = sb1("dg", [128, NTT, 4], F32); bdg = Buf("dg")
            eg = sb1("eg", [128, NTT, 4], F32); beg = Buf("eg")
            tmp = sb1("tmp", [128, NTT, 4, 8], F32); btmp = Buf("tmp")
            sl = sb1("sl", [128, NTT, 8], F32); bsl = Buf("sl")
            sl2 = sb1("sl2", [128, NTT, 8], F32); bsl2 = Buf("sl2")
            eq1 = sb1("eq1", [128, NTT, 8], F32); beq1 = Buf("eq1")
            eq2 = sb1("eq2", [128, NTT, 8], F32); beq2 = Buf("eq2")
            XS, bXS, rstd = nrm["XS"], nrm["bXS"], nrm["rstd"]
            for mt in range(NMT):
                tok0 = mt * TM
                oh = OHall[:, mt * NTT:(mt + 1) * NTT, :]
                s.dma("sp", H[:], self.h2[tok0:tok0 + TM, :].rearrange("(t p) d -> p t d", p=128), load=bH)
                self.rms_stats(nrm, lambda t: H[:, t, :], bH, NTT)
                for t in range(NTT):
                    k = t % 2
                    s.op("dve", lambda e, t=t, k=k: e.scalar_tensor_tensor(out=XS[k][:], in0=H[:, t, :], scalar=rstd[:, t:t + 1],
                                                                           in1=g3b[:], op0=ALU.mult, op1=ALU.mult),
                         reads=[bH, nrm["brstd"], CON], writes=[bXS[k]])
                    s.dma("sp", self.hn3[tok0 + t * 128:tok0 + (t + 1) * 128, :], XS[k][:], store=bXS[k])
                    def tr(e, k=k):
                        ins = None
                        for c in range(NCH):
                            ins = e.transpose(out=self.bank(0, BF16)[:, c * 128:(c + 1) * 128], in_=XS[k][:, c * 128:(c + 1) * 128], identity=ident_b[:])
                        return ins
                    s.op("pe", tr, reads=[bXS[k], CON], writes=[pb[0]])
                    s.op("act", lambda e, t=t: e.copy(out=XT[:, :, t * 128:(t + 1) * 128], in_=self.bank(0, BF16).rearrange("p (c t) -> p c t", c=NCH)),
                         reads=[pb[0]], writes=[bXT])
                def rl(e):
                    ins = None
                    for t in range(NTT):
                        for c in range(NCH):
                            ins = e.matmul(out=self.bank(1)[:, t * 36:(t + 1) * 36], lhsT=XT[:, c, t * 128:(t + 1) * 128], rhs=wr[:, c, :],
                                           start=(c == 0), stop=(c == NCH - 1))
                    return ins
                s.op("pe", rl, reads=[bXT, CON], writes=[pb[1]])
                s.op("dve", lambda e: e.tensor_tensor(out=L[:], in0=self.bank(1)[:, :NTT * 36].rearrange("p (t n) -> p t n", t=NTT), in1=bb[:], op=ALU.add),
                     reads=[pb[1], CON], writes=[bL])
                Lg = L[:, :, 0:4]
                Le = L[:, :, 4:36].rearrange("p t (g e) -> p t g e", g=4)
                s.op("dve", lambda e: e.tensor_reduce(out=r1[:], in_=Lg, axis=AX.X, op=ALU.max), reads=[bL], writes=[br[1]])
                s.op("dve", lambda e: e.tensor_tensor(out=dg[:], in0=Lg, in1=_bc_last(r1[:], 4), op=ALU.subtract), reads=[bL, br[1]], writes=[bdg])
                s.op("act", lambda e: e.activation(out=eg[:], in_=dg[:], func=AF.Exp), reads=[bdg], writes=[beg])
                s.op("dve", lambda e: e.tensor_reduce(out=r2[:], in_=eg[:], axis=AX.X, op=ALU.add), reads=[beg], writes=[br[2]])
                s.op("dve", lambda e: e.reciprocal(out=r2[:], in_=r2[:]), reads=[br[2]], writes=[br[2]])
                s.op("dve", lambda e, oh=oh: e.tensor_single_scalar(out=oh, in_=dg[:], scalar=0.0, op=ALU.is_ge), reads=[bdg], writes=[bOH])
                s.op("dve", lambda e, oh=oh: e.tensor_tensor(out=tmp[:], in0=Le, in1=_bc_last(oh, 8), op=ALU.mult), reads=[bL, bOH], writes=[btmp])
                s.op("dve", lambda e: e.tensor_reduce(out=sl[:], in_=tmp[:].rearrange("p t g e -> p t e g"), axis=AX.X, op=ALU.add), reads=[btmp], writes=[bsl])
                s.op("dve", lambda e: e.tensor_reduce(out=r3[:], in_=sl[:], axis=AX.X, op=ALU.max), reads=[bsl], writes=[br[3]])
                s.op("dve", lambda e: e.tensor_tensor(out=eq1[:], in0=sl[:], in1=_bc_last(r3[:], 8), op=ALU.is_equal), reads=[bsl, br[3]], writes=[beq1])
                s.op("dve", lambda e: e.scalar_tensor_tensor(out=sl2[:], in0=eq1[:], scalar=-1e30, in1=sl[:], op0=ALU.mult, op1=ALU.add),
                     reads=[beq1, bsl], writes=[bsl2])
                s.op("dve", lambda e: e.tensor_reduce(out=r4[:], in_=sl2[:], axis=AX.X, op=ALU.max), reads=[bsl2], writes=[br[4]])
                s.op("dve", lambda e: e.tensor_tensor(out=eq2[:], in0=sl2[:], in1=_bc_last(r4[:], 8), op=ALU.is_equal), reads=[bsl2, br[4]], writes=[beq2])
                s.op("dve", lambda e: e.tensor_tensor(out=r5[:], in0=r4[:], in1=r3[:], op=ALU.subtract), reads=[br[3], br[4]], writes=[br[5]])
                s.op("act", lambda e: e.activation(out=r5[:], in_=r5[:], func=AF.Exp), reads=[br[5]], writes=[br[5]])
                s.op("dve", lambda e: e.tensor_scalar(out=r3[:], in0=r5[:], scalar1=1.0, scalar2=None, op0=ALU.add), reads=[br[5]], writes=[br[3]])
                s.op("dve", lambda e: e.reciprocal(out=r3[:], in_=r3[:]), reads=[br[3]], writes=[br[3]])
                s.op("dve", lambda e: e.tensor_tensor(out=r3[:], in0=r3[:], in1=r2[:], op=ALU.mult), reads=[br[3], br[2]], writes=[br[3]])
                s.op("dve", lambda e: e.tensor_tensor(out=r4[:], in0=r3[:], in1=r5[:], op=ALU.mult), reads=[br[3], br[5]], writes=[br[4]])
                s.op("dve", lambda e: e.tensor_tensor(out=eq1[:], in0=eq1[:], in1=_bc_last(r3[:], 8), op=ALU.mult), reads=[beq1, br[3]], writes=[beq1])
                s.op("dve", lambda e: e.tensor_tensor(out=eq2[:], in0=eq2[:], in1=_bc_last(r4[:], 8), op=ALU.mult), reads=[beq2, br[4]], writes=[beq2])
                s.op("dve", lambda e, mt=mt: e.tensor_tensor(out=CW8all[:, mt * NTT:(mt + 1) * NTT, :], in0=eq1[:], in1=eq2[:], op=ALU.add),
                     reads=[beq1, beq2], writes=[bCW8])
            OHb = sb1("OHb", [128, NT * 4], BF16); bOHb = Buf("OHb")
            TOTs = sb1("TOTs", [128, NT, 4], F32); bTOT = Buf("TOTs")
            INC = sb1("INC", [128, 4, NT], F32); bINC = Buf("INC")
            EXC = sb1("EXC", [128, 4, NT], F32); bEXC = Buf("EXC")
            PC = sb1("PC", [128, 4], F32); bPC = Buf("PC")
            BASE = sb1("BASE", [128, 4], F32); bBASE = Buf("BASE")
            END = sb1("END", [128, 4], F32); bEND = Buf("END")
            V = sb1("V", [128, NT, 4], F32); bV = Buf("V")
            DF = sb1("DF", [128, NT], F32); bDF = Buf("DF")
            GK = sb1("GK", [128, NBK], F32); bGK = Buf("GK")
            GT = sb1("GT", [128, NBK], F32); bGT = Buf("GT")
            IXF = sb1("IXF", [128, NBK, 8], F32); bIXF = Buf("IXF")
            NC4 = NT * 4
            s.op("dve", lambda e: e.tensor_copy(out=OHb[:], in_=OHall[:].rearrange("p t g -> p (t g)")), reads=[bOH], writes=[bOHb])
            s.op("pe", lambda e: e.matmul(out=self.bank(2)[:, 0:NC4], lhsT=tri_b[:], rhs=OHb[:], start=True, stop=True), reads=[bOHb, CON], writes=[pb[2]])
            s.op("pe", lambda e: e.matmul(out=self.bank(3)[:, 0:NC4], lhsT=ones_b[:], rhs=OHb[:], start=True, stop=True), reads=[bOHb, CON], writes=[pb[3]])
            s.op("act", lambda e: e.copy(out=TOTs[:].rearrange("p t g -> p (t g)"), in_=self.bank(3)[:, 0:NC4]), reads=[pb[3]], writes=[bTOT])
            for g in range(4):
                s.op("dve", lambda e, g=g: e.tensor_tensor_scan(out=INC[:, g, :], data0=ones64[:], data1=TOTs[:, :, g], initial=0.0, op0=ALU.mult, op1=ALU.add),
                     reads=[bTOT, CON], writes=[bINC])
            s.op("dve", lambda e: e.tensor_tensor(out=EXC[:], in0=INC[:], in1=TOTs[:].rearrange("p t g -> p g t"), op=ALU.subtract), reads=[bINC, bTOT], writes=[bEXC])
            s.op("dve", lambda e: e.tensor_scalar(out=PC[:], in0=INC[:, :, NT - 1], scalar1=511.0, scalar2=1.0 / 512, op0=ALU.add, op1=ALU.mult), reads=[bINC], writes=[bPC])
            s.op("dve", lambda e: e.tensor_scalar(out=PC[:], in0=PC[:], scalar1=-0.5 + 1.0 / 2048, scalar2=None, op0=ALU.add), reads=[bPC], writes=[bPC])
            s.op("dve", lambda e: e.tensor_scalar(out=PC[:], in0=PC[:], scalar1=MAGIC, scalar2=None, op0=ALU.add), reads=[bPC], writes=[bPC])
            s.op("dve", lambda e: e.tensor_scalar(out=PC[:], in0=PC[:], scalar1=-MAGIC, scalar2=512.0, op0=ALU.add, op1=ALU.mult), reads=[bPC], writes=[bPC])
            s.op("dve", lambda e: e.memset(BASE[:], 0.0), writes=[bBASE])
            for g in range(1, 4):
                s.op("dve", lambda e, g=g: e.tensor_tensor(out=BASE[:, g:g + 1], in0=BASE[:, g - 1:g], in1=PC[:, g - 1:g], op=ALU.add), reads=[bBASE, bPC], writes=[bBASE])
            s.op("dve", lambda e: e.tensor_tensor(out=END[:], in0=BASE[:], in1=PC[:], op=ALU.add), reads=[bBASE, bPC], writes=[bEND])
            s.op("dve", lambda e: e.tensor_tensor(out=V[:], in0=self.bank(2)[:, 0:NC4].rearrange("p (t g) -> p t g", g=4), in1=EXC[:].rearrange("p g t -> p t g"), op=ALU.add),
                 reads=[pb[2], bEXC], writes=[bV])
            s.op("dve", lambda e: e.tensor_tensor(out=V[:], in0=V[:], in1=BASE[:].unsqueeze(1).broadcast_to([128, NT, 4]), op=ALU.add), reads=[bV, bBASE], writes=[bV])
            s.op("dve", lambda e: e.tensor_tensor(out=V[:], in0=V[:], in1=OHall[:], op=ALU.mult), reads=[bV, bOH], writes=[bV])
            s.op("dve", lambda e: e.tensor_reduce(out=DF[:], in_=V[:], axis=AX.X, op=ALU.add), reads=[bV], writes=[bDF])
            s.op("dve", lambda e: e.tensor_copy(out=DEST[:], in_=DF[:]), reads=[bDF], writes=[bDEST])
            s.op("dve", lambda e: e.tensor_scalar(out=GK[:], in0=kb[:], scalar1=END[:, 0:1], scalar2=None, op0=ALU.is_ge), reads=[bEND, CON], writes=[bGK])
            for g in range(1, 3):
                s.op("dve", lambda e, g=g: e.tensor_scalar(out=GT[:], in0=kb[:], scalar1=END[:, g:g + 1], scalar2=None, op0=ALU.is_ge), reads=[bEND, CON], writes=[bGT])
                s.op("dve", lambda e: e.tensor_tensor(out=GK[:], in0=GK[:], in1=GT[:], op=ALU.add), reads=[bGK, bGT], writes=[bGK])
            s.op("dve", lambda e: e.scalar_tensor_tensor(out=IXF[:], in0=_bc_last(GK[:], 8), scalar=1024.0, in1=ep[:].unsqueeze(1).broadcast_to([128, NBK, 8]),
                                                         op0=ALU.mult, op1=ALU.add), reads=[bGK, CON], writes=[bIXF])
            s.op("dve", lambda e: e.tensor_copy(out=IDXW[:], in_=IXF[:].rearrange("p k e -> p (k e)")), reads=[bIXF], writes=[bIDXW])
            XR = [sb1("XR%d" % i, [128, D + 8], BF16) for i in range(3)]; bXR = [Buf("XR%d" % i) for i in range(3)]
            s.barrier(["sp", "pool"])
            for t in range(NT):
                k = t % 3
                s.dma("sp", XR[k][:, 0:D], self.hn3[t * 128:(t + 1) * 128, :], load=bXR[k])
                s.op("dve", lambda e, t=t, k=k: e.tensor_copy(out=XR[k][:, D:D + 8], in_=CW8all[:, t, :]), reads=[bCW8], writes=[bXR[k]])
                s.idma(self.sortx, XR[k][:], DEST[:, t:t + 1], scatter=True, buf=bXR[k], idxbuf=bDEST)
            s.barrier()
        with ExitStack() as p2:
            sb2 = lambda n, sh, d=F32: p2.enter_context(nc.sbuf_tensor(self.uname(n), list(sh), d))
            XSS = sb2("XSS", [128, 4, D + 8], BF16); bXSS = Buf("XSS")
            XT = sb2("XT", [128, NCH, 512], BF16); bXT = Buf("XT")
            CWT = sb2("CWT", [8, 512], BF16); bCWT = Buf("CWT")
            WG = [sb2("WG%d" % i, [128, NCH * FF], BF16) for i in range(2)]; bWG = [Buf("WG%d" % i) for i in range(2)]
            WU = [sb2("WU%d" % i, [128, NCH * FF], BF16) for i in range(2)]; bWU = [Buf("WU%d" % i) for i in range(2)]
            WD = [sb2("WD%d" % i, [128, 8, D], BF16) for i in range(2)]; bWD = [Buf("WD%d" % i) for i in range(2)]
            HID = sb2("HID", [128, 8, 512], BF16); bHID = [Buf("HID%d" % i) for i in range(8)]
            SG = [sb2("SG%d" % i, [128, 512], BF16) for i in range(2)]; bSG = [Buf("SG%d" % i) for i in range(2)]
            T1 = [sb2("T1%d" % i, [128, 512], BF16) for i in range(2)]; bT1 = [Buf("T1%d" % i) for i in range(2)]
            MO = [sb2("MO%d" % i, [128, 4, D], F32) for i in range(2)]; bMO = [Buf("MO%d" % i) for i in range(2)]

            def load_expert(j):
                sl_ = j % 2
                s.idma(WG[sl_][:], self.wgs, IDXW[:, j:j + 1], scatter=False, buf=bWG[sl_], idxbuf=bIDXW)
                s.idma(WU[sl_][:], self.wus, IDXW[:, j:j + 1], scatter=False, buf=bWU[sl_], idxbuf=bIDXW)

            def load_down(q):
                sl_ = q % 2
                for el in range(4):
                    j = q * 4 + el
                    s.idma(WD[sl_][:, el * 2:el * 2 + 2, :].rearrange("p c d -> p (c d)"), self.wds, IDXW[:, j:j + 1], scatter=False, buf=bWD[sl_], idxbuf=bIDXW)

            NJ = NBK * 8
            load_expert(0); load_expert(1); load_down(0)
            it = 0
            for k in range(NBK):
                s.dma("sp", XSS[:], self.sortx[k * 512:(k + 1) * 512, :].rearrange("(t p) c -> p t c", p=128), load=bXSS)
                for t in range(4):
                    def tr(e, t=t):
                        ins = None
                        for c in range(NCH):
                            ins = e.transpose(out=self.bank(0, BF16)[:, c * 128:(c + 1) * 128], in_=XSS[:, t, c * 128:(c + 1) * 128], identity=ident_b[:])
                        return ins
                    s.op("pe", tr, reads=[bXSS, CON], writes=[pb[0]])
                    s.op("act", lambda e, t=t: e.copy(out=XT[:, :, t * 128:(t + 1) * 128], in_=self.bank(0, BF16).rearrange("p (c t) -> p c t", c=NCH)),
                         reads=[pb[0]], writes=[bXT])
                def trcw(e):
                    ins = None
                    for t in range(4):
                        ins = e.transpose(out=self.bank(1, BF16)[0:8, t * 128:(t + 1) * 128], in_=XSS[:, t, D:D + 8], identity=ident_b[:])
                    return ins
                s.op("pe", trcw, reads=[bXSS, CON], writes=[pb[1]])
                s.op("act", lambda e: e.copy(out=CWT[:], in_=self.bank(1, BF16)[0:8, 0:512]), reads=[pb[1]], writes=[bCWT])
                mo = k % 2
                for sg in range(2):
                    for el4 in range(4):
                        el = sg * 4 + el4
                        j = k * 8 + el
                        kk = j % 2
                        bc = 2 + (j % 2)
                        s.op("pe", lambda e, bc=bc, el=el: e.matmul(out=self.bank(bc), lhsT=sel[:, el * 128:(el + 1) * 128], rhs=CWT[:], start=True, stop=True),
                             reads=[bCWT, CON], writes=[pb[bc]])
                        for fc in range(2):
                            jj = it % 2
                            bg, bu = 4 + jj, 6 + jj
                            s.op("pe", lambda e, kk=kk, fc=fc, bg=bg: self.mm_group(e, self.bank(bg), [(WG[kk][:, c * FF + fc * 128:c * FF + (fc + 1) * 128], XT[:, c, :]) for c in range(NCH)]),
                                 reads=[bWG[kk], bXT], writes=[pb[bg]])
                            s.op("pe", lambda e, kk=kk, fc=fc, bu=bu: self.mm_group(e, self.bank(bu), [(WU[kk][:, c * FF + fc * 128:c * FF + (fc + 1) * 128], XT[:, c, :]) for c in range(NCH)]),
                                 reads=[bWU[kk], bXT], writes=[pb[bu]])
                            s.op("act", lambda e, jj=jj, bg=bg: e.activation(out=SG[jj][:], in_=self.bank(bg), func=AF.Silu), reads=[pb[bg]], writes=[bSG[jj]])
                            s.op("dve", lambda e, jj=jj, bu=bu: e.tensor_tensor(out=T1[jj][:], in0=SG[jj][:], in1=self.bank(bu), op=ALU.mult),
                                 reads=[bSG[jj], pb[bu]], writes=[bT1[jj]])
                            hj = el4 * 2 + fc
                            s.op("dve", lambda e, jj=jj, bc=bc, hj=hj: e.tensor_tensor(out=HID[:, hj, :], in0=T1[jj][:], in1=self.bank(bc), op=ALU.mult),
                                 reads=[bT1[jj], pb[bc]], writes=[bHID[hj]])
                            it += 1
                        if j + 2 < NJ:
                            load_expert(j + 2)
                    q = k * 2 + sg
                    ws = q % 2
                    for t in range(4):
                        for dh in range(2):
                            bd = (t * 2 + dh) % 2
                            s.op("pe", lambda e, t=t, dh=dh, bd=bd, ws=ws: self.mm_group(e, self.bank(bd), [(HID[:, jx, t * 128:(t + 1) * 128], WD[ws][:, jx, dh * 512:(dh + 1) * 512]) for jx in range(8)]),
                                 reads=bHID + [bWD[ws]], writes=[pb[bd]])
                            dst = MO[mo][:, t, dh * 512:(dh + 1) * 512]
                            if sg == 0:
                                s.op("act", lambda e, dst=dst, bd=bd: e.copy(out=dst, in_=self.bank(bd)), reads=[pb[bd]], writes=[bMO[mo]])
                            else:
                                s.op("dve", lambda e, dst=dst, bd=bd: e.tensor_tensor(out=dst, in0=dst, in1=self.bank(bd), op=ALU.add), reads=[bMO[mo], pb[bd]], writes=[bMO[mo]])
                    if q + 1 < NBK * 2:
                        load_down(q + 1)
                s.dma("sp", self.sortout[k * 512:(k + 1) * 512, :].rearrange("(t p) d -> p t d", p=128), MO[mo][:], store=bMO[mo])
            s.barrier()
        with ExitStack() as p3:
            sb3 = lambda n, sh, d=F32: p3.enter_context(nc.sbuf_tensor(self.uname(n), list(sh), d))
            G = [sb3("G%d" % i, [128, D], F32) for i in range(4)]; bG = [Buf("G%d" % i) for i in range(4)]
            Hh = [sb3("Hh%d" % i, [128, D], F32) for i in range(4)]; bHh = [Buf("Hh%d" % i) for i in range(4)]
            ssq, rstd = nrm["ssq"], nrm["rstd"]
            sq = [sb3("sq%d" % i, [128, 1], F32) for i in range(4)]; bsq = [Buf("sq%d" % i) for i in range(4)]
            for t in range(NT):
                k = t % 4
                s.idma(G[k][:], self.sortout, DEST[:, t:t + 1], scatter=False, buf=bG[k], idxbuf=bDEST)
                s.dma("sp", Hh[k][:], self.h2[t * 128:(t + 1) * 128, :], load=bHh[k])
                s.op("dve", lambda e, k=k: e.tensor_tensor(out=Hh[k][:], in0=Hh[k][:], in1=G[k][:], op=ALU.add), reads=[bHh[k], bG[k]], writes=[bHh[k]])
                s.op("act", lambda e, k=k: e.activation(out=nrm["JUNK"][:], in_=Hh[k][:], func=AF.Square, accum_out=sq[k][:]), reads=[bHh[k]], writes=[nrm["bJ"], bsq[k]])
                s.op("act", lambda e, k=k: e.activation(out=sq[k][:], in_=sq[k][:], func=AF.Ln, scale=1.0 / D, bias=nrm["epsc"][:]), reads=[bsq[k]], writes=[bsq[k]])
                s.op("act", lambda e, k=k: e.activation(out=sq[k][:], in_=sq[k][:], func=AF.Exp, scale=-0.5), reads=[bsq[k]], writes=[bsq[k]])
                s.op("dve", lambda e, k=k: e.scalar_tensor_tensor(out=G[k][:], in0=Hh[k][:], scalar=sq[k][:], in1=gfb[:], op0=ALU.mult, op1=ALU.mult),
                     reads=[bHh[k], bsq[k], CON], writes=[bG[k]])
                s.dma("sp", self.out[t * 128:(t + 1) * 128, :], G[k][:], store=bG[k])

    def phase1(self, ps):
        nc, s, pb = self.nc, self.s, self.pb
        sb = lambda n, sh, d=F32: ps.enter_context(nc.sbuf_tensor(self.uname(n), list(sh), d))
        S = self.S
        BPS = S // 512
        NKC = S // 128
        W = self.w
        SCALE = 1.0 / math.sqrt(NOPE + ROPE)
        TWO_PI = 2.0 * math.pi
        MAGIC = 12582912.0
        PI_LO = 3.1415925
        CON = Buf("con1")
        ident_b = sb("ident_b", [128, 128], BF16)
        g1b = sb("g1b", [128, D], F32)
        gqkvb = sb("gqkvb", [128, 640], F32)
        sel2 = sb("sel2", [64, 128], BF16)
        ropec = sb("ropec", [128, 4], F32)
        WIN1 = sb("WIN1", [128, NCH, 704], BF16)
        WQ = sb("WQ", [128, 3, NH, 128], BF16)
        WKN = sb("WKN", [128, 2, NH, 128], BF16)
        WV = sb("WV", [128, 2, 512], BF16)
        ident_f = sb("ident_f", [65, 65], F32)
        s.dma("sp", ident_f[:], self.c_ident[0:65, 0:65], load=CON)
        s.dma("pool", ident_b[:], self.c_ident, load=CON)
        s.dma("pool", sel2[:], self.c_sel2, load=CON)
        s.dma("sp", ropec[:], self.c_rope, load=CON)
        s.dma("sp", g1b[:], W["mix_norm_g"].partition_broadcast(128), load=CON)
        s.dma("sp", gqkvb[:, 0:384], W["q_norm_g"].partition_broadcast(128), load=CON)
        s.dma("sp", gqkvb[:, 384:640], W["kv_norm_g"].partition_broadcast(128), load=CON)
        CONW = Buf("conw1")
        s.op("dve", lambda e: e.memset(WKN[:], 0.0), writes=[CONW])
        win_v = W["w_in"].rearrange("(c p) n -> p c n", p=128)
        s.dma("pool", WIN1[:, :, 0:672], win_v[:, :, 0:672], load=CON)
        s.dma("pool", WIN1[:, :, 672:688], win_v[:, :, 656:672], load=CON)
        s.dma("pool", WIN1[:, :, 688:704], win_v[:, :, 640:656], load=CON)
        for c in range(3):
            src = W["w_q_up"][c * 128:(c + 1) * 128, :].rearrange("p (h n) -> p h n", h=NH)
            s.dma("pool", WQ[:, c, :, 0:96], src, load=CONW)
            s.dma("pool", WQ[:, c, :, 96:112], src[:, :, 80:96], load=CONW)
            s.dma("pool", WQ[:, c, :, 112:128], src[:, :, 64:80], load=CONW)
        for c in range(2):
            src = W["w_kv_up"][c * 128:(c + 1) * 128, :].rearrange("p (h n) -> p h n", h=NH)
            s.dma("pool", WKN[:, c, :, 0:64], src[:, :, 0:64], load=CONW)
            s.dma("pool", WV[:, c, :].rearrange("p (h n) -> p h n", h=NH), src[:, :, 64:128], load=CONW)
        self.prep_dense_scratch()
        self.prep_expert_scratch()
        nrm = self.mk_norm(sb, "1", xs=False, junk=False)
        X = sb("X", [128, 4, D], F32); bX = Buf("X")
        HT = sb("HT", [128, NCH, 512], BF16); bHT = Buf("HT")
        LAT = [sb("LAT%d" % i, [128, 640], F32) for i in range(2)]; bLAT = [Buf("LAT%d" % i) for i in range(2)]
        LS = [sb("LS%d" % i, [128, 640], BF16) for i in range(2)]; bLS = [Buf("LS%d" % i) for i in range(2)]
        LT = sb("LT", [128, 5, 512], BF16); bLT = Buf("LT")
        lsq_ = [sb("lsq%d" % i, [128, 2], F32) for i in range(2)]; blsq_ = [Buf("lsq%d" % i) for i in range(2)]
        lrs_ = [sb("lrs%d" % i, [128, 2], F32) for i in range(2)]; blrs_ = [Buf("lrs%d" % i) for i in range(2)]
        bJ2 = [Buf("J0"), Buf("J1")]
        PKS = sb("PKS", [64, 512], BF16); bPKS = Buf("PKS")
        QT = sb("QT", [128, NH, 512], BF16); bQT = Buf("QT")
        PT = [sb("PT%d" % i, [128, 512], BF16) for i in range(4)]; bPT = [Buf("PT%d" % i) for i in range(4)]
        ATM = sb("ATM", [128, 4, 512], BF16); bATM = Buf("ATM")
        ATT = sb("ATT", [128, 4, 512], BF16); bATT = Buf("ATT")
        rec = [sb("rec%d" % i, [128, 4], F32) for i in range(2)]; brec = [Buf("rec%d" % i) for i in range(2)]
        KT = sb("KT", [128, NH, S], BF16); bKT = [Buf("KT%d" % i) for i in range(BPS)]
        VA = sb("VA", [128, NKC, NH, 65], BF16); bVA = [Buf("VA%d" % i) for i in range(BPS)]
        TQ = sb("TQ", [128, 512], BF16); bTQ = Buf("TQ")
        TK = sb("TK", [64, 512], BF16); bTK = Buf("TK")
        posI = sb("posI", [128, 512], I32); bposI = Buf("posI")
        posF = sb("posF", [128, 512], F32); bposF = Buf("posF")
        ARG = sb("ARG", [128, 512], F32); bARG = Buf("ARG")
        TT_ = sb("TT", [128, 512], F32); bTT = Buf("TT")
        nrm["JUNK"] = TT_[:].bitcast(BF16)
        nrm["bJ"] = bTT
        s.op("dve", lambda e: e.memset(VA[:, :, :, 64:65], 1.0), writes=bVA)
        QTs = [QT, sb("QT2", [128, NH, 512], BF16)]; bQTs = [bQT, Buf("QT2")]
        XS4 = sb("XS4", [128, 2, D], BF16); bXS4 = Buf("XS4"); bXS4b = Buf("XS4b")

        def stageA(b, i):
            tok0 = b * S + i * 512
            cols = slice(i * 512, (i + 1) * 512)
            QTc, bQTc = QTs[i % 2], bQTs[i % 2]
            s.dma("sp", X[:], self.x[tok0:tok0 + 512, :].rearrange("(t p) d -> p t d", p=128), load=bX); yield
            s.dma("sp", posI[:], self.pos[b, i * 512:(i + 1) * 512].partition_broadcast(128), load=bposI); yield
            s.op("dve", lambda e: e.tensor_copy(out=posF[:], in_=posI[:]), reads=[bposI], writes=[bposF]); yield
            for tbl, btbl, ic, nr in [(TQ, bTQ, 0, 128), (TK, bTK, 2, 64)]:
                s.op("dve", lambda e: e.tensor_scalar(out=ARG[:nr], in0=posF[:nr], scalar1=ropec[:nr, ic:ic + 1],
                                                      scalar2=ropec[:nr, ic + 1:ic + 2], op0=ALU.mult, op1=ALU.add),
                     reads=[bposF, CON], writes=[bARG]); yield
                s.op("dve", lambda e: e.tensor_scalar(out=TT_[:nr], in0=ARG[:nr], scalar1=1.0 / TWO_PI, scalar2=MAGIC, op0=ALU.mult, op1=ALU.add),
                     reads=[bARG], writes=[bTT]); yield
                s.op("dve", lambda e: e.tensor_scalar(out=TT_[:nr], in0=TT_[:nr], scalar1=-MAGIC, scalar2=None, op0=ALU.add),
                     reads=[bTT], writes=[bTT]); yield
                s.op("dve", lambda e: e.scalar_tensor_tensor(out=ARG[:nr], in0=TT_[:nr], scalar=-TWO_PI, in1=ARG[:nr], op0=ALU.mult, op1=ALU.add),
                     reads=[bTT, bARG], writes=[bARG]); yield
                s.op("dve", lambda e: e.tensor_scalar(out=ARG[:nr], in0=ARG[:nr], scalar1=-PI_LO, scalar2=PI_LO, op0=ALU.max, op1=ALU.min),
                     reads=[bARG], writes=[bARG]); yield
                s.op("act", lambda e: e.activation(out=tbl[:nr], in_=ARG[:nr], func=AF.Sin), reads=[bARG], writes=[btbl]); yield
            for t in range(4):
                s.op("act", lambda e: e.activation(out=HT[:].rearrange("p c t -> p (c t)")[:, t * D:(t + 1) * D], in_=X[:, t, :], func=AF.Square,
                                                   accum_out=nrm["ssq"][:, t:t + 1]),
                     reads=[bX], writes=[bHT, nrm["bssq"]]); yield
            s.op("act", lambda e: e.activation(out=nrm["rstd"][:, 0:4], in_=nrm["ssq"][:, 0:4], func=AF.Ln, scale=1.0 / D, bias=nrm["epsc"][:]),
                 reads=[nrm["bssq"]], writes=[nrm["brstd"]]); yield
            s.op("act", lambda e: e.activation(out=nrm["rstd"][:, 0:4], in_=nrm["rstd"][:, 0:4], func=AF.Exp, scale=-0.5), reads=[nrm["brstd"]], writes=[nrm["brstd"]]); yield
            bXS2 = [bXS4, bXS4b]
            for t in range(4):
                s.op("dve", lambda e: e.scalar_tensor_tensor(out=XS4[:, t % 2, :], in0=X[:, t, :], scalar=nrm["rstd"][:, t:t + 1], in1=g1b[:], op0=ALU.mult, op1=ALU.mult),
                     reads=[bX, nrm["brstd"], CON], writes=[bXS2[t % 2]]); yield
                def tr0(e):
                    ins = None
                    for c in range(NCH):
                        ins = e.transpose(out=self.bank(0, BF16)[:, c * 128:(c + 1) * 128], in_=XS4[:, t % 2, c * 128:(c + 1) * 128], identity=ident_b[:])
                    return ins
                s.op("pe", tr0, reads=[bXS2[t % 2], CON], writes=[pb[0]]); yield
                s.op("dve", lambda e: e.tensor_copy(out=HT[:, :, t * 128:(t + 1) * 128], in_=self.bank(0, BF16).rearrange("p (c t) -> p c t", c=NCH)),
                     reads=[pb[0]], writes=[bHT]); yield
            for t in range(4):
                k = t % 2
                lsq, blsq, lrs, blrs = lsq_[k], blsq_[k], lrs_[k], blrs_[k]
                ts_ = slice(t * 128, (t + 1) * 128)
                s.op("pe", lambda e: self.mm_group(e, self.bank(1), [(HT[:, c, ts_], WIN1[:, c, 0:512]) for c in range(NCH)]),
                     reads=[bHT, CON], writes=[pb[1]]); yield
                s.op("pe", lambda e: self.mm_group(e, self.bank(2)[:, 0:128], [(HT[:, c, ts_], WIN1[:, c, 512:640]) for c in range(NCH)]),
                     reads=[bHT, CON], writes=[pb[2]]); yield
                s.op("dve", lambda e: e.tensor_copy(out=LAT[k][:, 0:512], in_=self.bank(1)), reads=[pb[1]], writes=[bLAT[k]]); yield
                s.op("dve", lambda e: e.tensor_copy(out=LAT[k][:, 512:640], in_=self.bank(2)[:, 0:128]), reads=[pb[2]], writes=[bLAT[k]]); yield
                s.op("act", lambda e: e.activation(out=LS[k][:, 0:384], in_=LAT[k][:, 0:384], func=AF.Square, accum_out=lsq[:, 0:1]),
                     reads=[bLAT[k]], writes=[bLS[k], blsq]); yield
                s.op("act", lambda e: e.activation(out=LS[k][:, 384:640], in_=LAT[k][:, 384:640], func=AF.Square, accum_out=lsq[:, 1:2]),
                     reads=[bLAT[k]], writes=[bLS[k], blsq]); yield
                s.op("act", lambda e: e.activation(out=lrs[:, 0:1], in_=lsq[:, 0:1], func=AF.Ln, scale=1.0 / Q_LORA, bias=nrm["epsc"][:]), reads=[blsq], writes=[blrs]); yield
                s.op("act", lambda e: e.activation(out=lrs[:, 1:2], in_=lsq[:, 1:2], func=AF.Ln, scale=1.0 / KV_LORA, bias=nrm["epsc"][:]), reads=[blsq], writes=[blrs]); yield
                s.op("act", lambda e: e.activation(out=lrs[:], in_=lrs[:], func=AF.Exp, scale=-0.5), reads=[blrs], writes=[blrs]); yield
                s.op("dve", lambda e: e.scalar_tensor_tensor(out=LS[k][:, 0:384], in0=LAT[k][:, 0:384], scalar=lrs[:, 0:1], in1=gqkvb[:, 0:384],
                                                             op0=ALU.mult, op1=ALU.mult), reads=[bLAT[k], blrs, CON], writes=[bLS[k]]); yield
                s.op("dve", lambda e: e.scalar_tensor_tensor(out=LS[k][:, 384:640], in0=LAT[k][:, 384:640], scalar=lrs[:, 1:2], in1=gqkvb[:, 384:640],
                                                             op0=ALU.mult, op1=ALU.mult), reads=[bLAT[k], blrs, CON], writes=[bLS[k]]); yield
                if t >= 1:
                    kp = (t - 1) % 2
                    tsp = slice((t - 1) * 128, t * 128)
                    def trp(e):
                        ins = None
                        for c in range(5):
                            ins = e.transpose(out=self.bank(0, BF16)[:, c * 128:(c + 1) * 128], in_=LS[kp][:, c * 128:(c + 1) * 128], identity=ident_b[:])
                        return ins
                    s.op("pe", trp, reads=[bLS[kp], CON], writes=[pb[0]]); yield
                    s.op("dve", lambda e: e.tensor_copy(out=LT[:, :, tsp], in_=self.bank(0, BF16)[:, 0:640].rearrange("p (c t) -> p c t", c=5)),
                         reads=[pb[0]], writes=[bLT]); yield
            def tr3(e):
                ins = None
                for c in range(5):
                    ins = e.transpose(out=self.bank(0, BF16)[:, c * 128:(c + 1) * 128], in_=LS[1][:, c * 128:(c + 1) * 128], identity=ident_b[:])
                return ins
            s.op("pe", tr3, reads=[bLS[1], CON], writes=[pb[0]]); yield
            s.op("dve", lambda e: e.tensor_copy(out=LT[:, :, 384:512], in_=self.bank(0, BF16)[:, 0:640].rearrange("p (c t) -> p c t", c=5)),
                 reads=[pb[0]], writes=[bLT]); yield
            for h in range(NH):
                bk = 2 - (h % 2)
                s.op("pe", lambda e: self.mm_group(e, self.bank(bk), [(WQ[:, c, h, :], LT[:, c, :]) for c in range(3)]),
                     reads=[bLT, CON, CONW], writes=[pb[bk]]); yield
                s.op("dve", lambda e: e.tensor_tensor(out=QTc[:, h, :], in0=self.bank(bk), in1=TQ[:], op=ALU.mult),
                     reads=[pb[bk], bTQ], writes=[bQTc]); yield

            yield "kv"
            s.op("pe", lambda e: self.mm_group(e, self.bank(1)[0:64, :], [(WIN1[:, c, 640:704], HT[:, c, :]) for c in range(NCH)]),
                 reads=[bHT, CON], writes=[pb[1]]); yield
            s.op("dve", lambda e: e.tensor_tensor(out=PKS[:], in0=self.bank(1)[0:64, :], in1=TK[:], op=ALU.mult), reads=[pb[1], bTK], writes=[bPKS]); yield
            for h in range(NH):
                bk = 2 - (h % 2)
                s.op("pe", lambda e: self.mm_group(e, self.bank(bk), [(sel2[:], PKS[:])] + [(WKN[:, c, h, :], LT[:, 3 + c, :]) for c in range(2)]),
                     reads=[bPKS, bLT, CON, CONW], writes=[pb[bk]]); yield
                s.op("dve", lambda e: e.tensor_copy(out=KT[:, h, cols], in_=self.bank(bk)), reads=[pb[bk]], writes=[bKT[i]]); yield
            for t in range(4):
                bk = 2 - (t % 2)
                ts_ = slice(t * 128, (t + 1) * 128)
                s.op("pe", lambda e: self.mm_group(e, self.bank(bk), [(LT[:, 3 + c, ts_], WV[:, c, :]) for c in range(2)]),
                     reads=[bLT, CON, CONW], writes=[pb[bk]]); yield
                s.op("dve", lambda e: e.tensor_copy(out=VA[:, i * 4 + t, :, 0:64], in_=self.bank(bk).rearrange("p (h n) -> p h n", h=NH)),
                     reads=[pb[bk]], writes=[bVA[i]]); yield
        hold_state = {"hold": False}

        def drain(g, n=None):
            if g is None:
                return
            if hold_state["hold"] and hold_state.get("at_kv") is g:
                return
            k = 0
            for v in g:
                if v == "kv" and hold_state["hold"]:
                    hold_state["at_kv"] = g
                    return
                k += 1
                if n is not None and k >= n:
                    return

        ATMs = [ATM, ATM]; bATMs = [bATM, bATM]

        def stageB(b, i, nxt, prevC):
            QTc, bQTc = QTs[i % 2], bQTs[i % 2]
            ATMc, bATMc = ATMs[i % 2], bATMs[i % 2]
            nkc = 4 * i + 4
            per = max(1, -(-150 // (NH * nkc)))
            tb3 = self.bank(3)[:, 0:260].rearrange("p (q n) -> p q n", n=65)

            def epilogue(h):
                accb = 6 + (h % 2)
                hp = h % 2
                s.op("dve", lambda e: e.tensor_copy(out=TT_[0:65, :], in_=self.bank(accb)[0:65, :]), reads=[pb[accb]], writes=[bTT])
                def trb(e):
                    ins = None
                    for qt in range(4):
                        ins = e.transpose(out=self.bank(3)[:, qt * 65:(qt + 1) * 65], in_=TT_[0:65, qt * 128:(qt + 1) * 128], identity=ident_f[:])
                    return ins
                s.op("pe", trb, reads=[bTT, CON], writes=[pb[3]])
                s.op("dve", lambda e: e.reciprocal(out=rec[hp][:], in_=tb3[:, :, 64]), reads=[pb[3]], writes=[brec[hp]])
                s.op("dve", lambda e: e.tensor_tensor(out=ATMc[:, :, h * 64:(h + 1) * 64], in0=tb3[:, :, 0:64],
                                                      in1=_bc_last(rec[hp][:], 64), op=ALU.mult),
                     reads=[pb[3], brec[hp]], writes=[bATMc])

            def emit_pv(h, kc, slot, j0):
                accb = 6 + (h % 2)
                qs = j0 * 128
                s.op("pe", lambda e: e.matmul(out=self.bank(accb)[0:65, qs:512], lhsT=VA[:, kc, h, :], rhs=PT[slot][:, qs:512],
                                              start=(kc == 0), stop=(kc == nkc - 1)),
                     reads=[bPT[slot], bVA[kc // 4]], writes=[pb[accb]])

            steps = [(h, kc) for h in range(NH) for kc in range(nkc)]
            pend = []
            epi_q = []
            for n, (h, kc) in enumerate(steps):
                j = kc - 4 * i
                j0 = max(0, j)
                qs = j0 * 128
                sbk = 4 + (n % 2)
                slot = n % 4
                s.op("pe", lambda e: e.matmul(out=self.bank(sbk)[:, qs:512], lhsT=KT[:, h, kc * 128:(kc + 1) * 128],
                                              rhs=QTc[:, h, qs:512], start=True, stop=True),
                     reads=[bKT[kc // 4], bQTc], writes=[pb[sbk]])
                s.op("act", lambda e: e.activation(out=PT[slot][:, qs:512], in_=self.bank(sbk)[:, qs:512], func=AF.Exp, scale=SCALE),
                     reads=[pb[sbk]], writes=[bPT[slot]])
                if j >= 0:
                    s.op("dve", lambda e: e.memset(PT[slot][64:128, qs:qs + 64], 0.0), writes=[bPT[slot]])
                pend.append((h, kc, slot, j0))
                if len(pend) > 2:
                    p_ = pend.pop(0)
                    emit_pv(*p_)
                    if p_[1] == nkc - 1:
                        epi_q.append((p_[0], n + 1))
                while epi_q and epi_q[0][1] <= n:
                    epilogue(epi_q.pop(0)[0])
                if n == 2 and prevC is not None:
                    prevC()
                    prevC = None
                drain(nxt, per)
            for p_ in pend:
                emit_pv(*p_)
                if p_[1] == nkc - 1:
                    epi_q.append((p_[0], 0))
            for h_, _ in epi_q:
                epilogue(h_)
            if prevC is not None:
                prevC()

        def stageC(b, i):
            tok0 = b * S + i * 512
            ATMc, bATMc = ATMs[i % 2], bATMs[i % 2]
            for t in range(4):
                def tr2(e):
                    ins = None
                    for c in range(4):
                        ins = e.transpose(out=self.bank(0, BF16)[:, c * 128:(c + 1) * 128], in_=ATMc[:, t, c * 128:(c + 1) * 128], identity=ident_b[:])
                    return ins
                s.op("pe", tr2, reads=[bATMc, CON], writes=[pb[0]])
                s.op("dve", lambda e: e.tensor_copy(out=ATT[:, :, t * 128:(t + 1) * 128], in_=self.bank(0, BF16)[:, 0:512].rearrange("p (c t) -> p c t", c=4)),
                     reads=[pb[0]], writes=[bATT])
            s.dma("sp", self.attnT[:, tok0:tok0 + 512].rearrange("(c p) t -> p c t", p=128), ATT[:], store=bATT)

        carry = None
        for b in range(self.NB):
            if carry is None:
                drain(stageA(b, 0))
            else:
                hold_state["hold"] = False
                hold_state["at_kv"] = None
                drain(carry)
                carry = None
            prevC = None
            for i in range(BPS):
                if i + 1 < BPS:
                    nxt = stageA(b, i + 1)
                elif b + 1 < self.NB:
                    nxt = stageA(b + 1, 0)
                    hold_state["hold"] = True
                    carry = nxt
                else:
                    nxt = None
                stageB(b, i, nxt, prevC)
                drain(nxt)
                prevC = (lambda b=b, i=i: stageC(b, i))
            prevC()
    def phase3_dense(self, ps):
        nc, s = self.nc, self.s
        sb = lambda n, sh, d=F32: ps.enter_context(nc.sbuf_tensor(self.uname(n), list(sh), d))
        TM = 1024
        NMT = self.NTOK // TM
        NTT = TM // 128
        CON = Buf("con3")
        ident_f = sb("ident_f", [128, 128], F32)
        ident_b = sb("ident_b", [128, 128], BF16)
        sel = sb("sel", [32, 32 * 128], BF16)
        g3b = sb("g3b", [128, D], F32)
        gfb = sb("gfb", [128, D], F32)
        wr = sb("wr", [128, NCH, 36], BF16)
        bb = sb("bb", [128, NTT, 36], F32)
        s.dma("sp", ident_f[:], self.c_ident, load=CON)
        s.dma("pool", ident_b[:], self.c_ident, load=CON)
        s.dma("pool", sel[:], self.c_sel, load=CON)
        s.dma("sp", g3b[:], self.w["ffn_norm_g"].partition_broadcast(128), load=CON)
        s.dma("sp", gfb[:], self.w["final_norm_g"].partition_broadcast(128), load=CON)
        s.dma("pool", wr[:], self.w["w_router"].rearrange("(c p) n -> p c n", p=128), load=CON)
        for t in range(NTT):
            s.dma("sp", bb[:, t, :], self.w["b_router"].partition_broadcast(128), load=CON)
        H = sb("H", [128, NTT, D], F32); bH = Buf("H")
        XS = [sb("XS%d" % i, [128, D], BF16) for i in range(2)]; bXS = [Buf("XS%d" % i) for i in range(2)]
        XT = sb("XT", [128, NCH, TM], BF16); bXT = Buf("XT")
        JUNK = sb("JUNK", [128, D], BF16); bJ = Buf("JUNK")
        ssq = sb("ssq", [128, NTT], F32); bssq = Buf("ssq")
        rstd = sb("rstd", [128, NTT], F32); brstd = Buf("rstd")
        L = sb("L", [128, NTT, 36], F32); bL = Buf("L")
        r1 = sb("r1", [128, NTT], F32); r2 = sb("r2", [128, NTT], F32); r3 = sb("r3", [128, NTT], F32)
        r4 = sb("r4", [128, NTT], F32); r5 = sb("r5", [128, NTT], F32)
        br = [Buf("r%d" % i) for i in range(6)]
        dg = sb("dg", [128, NTT, 4], F32); bdg = Buf("dg")
        eg = sb("eg", [128, NTT, 4], F32); beg = Buf("eg")
        oh = sb("oh", [128, NTT, 4], F32); boh = Buf("oh")
        tmp = sb("tmp", [128, NTT, 4, 8], F32); btmp = Buf("tmp")
        sl = sb("sl", [128, NTT, 8], F32); bsl = Buf("sl")
        sl2 = sb("sl2", [128, NTT, 8], F32); bsl2 = Buf("sl2")
        eq1 = sb("eq1", [128, NTT, 8], F32); beq1 = Buf("eq1")
        eq2 = sb("eq2", [128, NTT, 8], F32); beq2 = Buf("eq2")
        cw8 = sb("cw8", [128, NTT, 8], F32); bcw8 = Buf("cw8")
        CW = sb("CW", [128, NTT, 32], BF16); bCW = Buf("CW")
        CWT = sb("CWT", [32, TM], BF16); bCWT = Buf("CWT")
        WG = [sb("WG%d" % i, [128, NCH, FF], BF16) for i in range(2)]; bWG = [Buf("WG%d" % i) for i in range(2)]
        WU = [sb("WU%d" % i, [128, NCH, FF], BF16) for i in range(2)]; bWU = [Buf("WU%d" % i) for i in range(2)]
        WD = [sb("WD%d" % i, [128, 8, D], BF16) for i in range(2)]; bWD = [Buf("WD%d" % i) for i in range(2)]
        HID = sb("HID", [128, 8, TM], BF16); bHID = [Buf("HID%d" % i) for i in range(8)]
        SG = [sb("SG%d" % i, [128, 512], BF16) for i in range(2)]; bSG = [Buf("SG%d" % i) for i in range(2)]
        T1 = [sb("T1%d" % i, [128, 512], BF16) for i in range(2)]; bT1 = [Buf("T1%d" % i) for i in range(2)]
        OUT = [sb("OUT%d" % i, [128, D], F32) for i in range(2)]; bOUT = [Buf("OUT%d" % i) for i in range(2)]
        pb = self.pb
        wg, wu, wd = self.w["w_exp_gate"], self.w["w_exp_up"], self.w["w_exp_down"]

        def load_expert(e):
            sl_ = e % 2
            s.dma("pool", WG[sl_][:], wg[e].rearrange("(c p) f -> p c f", p=128), load=bWG[sl_])
            s.dma("pool", WU[sl_][:], wu[e].rearrange("(c p) f -> p c f", p=128), load=bWU[sl_])

        def load_down(sg):
            sl_ = sg % 2
            for el in range(4):
                e = sg * 4 + el
                s.dma("pool", WD[sl_][:, el * 2:el * 2 + 2, :], wd[e].rearrange("(c p) d -> p c d", p=128),
                      load=bWD[sl_])

        def rms_stats(src_fn, n):
            for t in range(n):
                s.op("act", lambda e, t=t: e.activation(out=JUNK[:], in_=src_fn(t), func=AF.Square,
                                                        accum_out=ssq[:, t:t + 1]),
                     reads=[bH], writes=[bJ, bssq])
            s.op("act", lambda e: e.activation(out=rstd[:, :n], in_=ssq[:, :n], func=AF.Sqrt, scale=1.0 / D, bias=EPS),
                 reads=[bssq], writes=[brstd])
            s.op("dve", lambda e: e.reciprocal(out=rstd[:, :n], in_=rstd[:, :n]), reads=[brstd], writes=[brstd])

        for mt in range(NMT):
            tok0 = mt * TM
            load_expert(0)
            load_expert(1)
            load_down(0)
            s.dma("sp", H[:], self.h2[tok0:tok0 + TM, :].rearrange("(t p) d -> p t d", p=128), load=bH)
            rms_stats(lambda t: H[:, t, :], NTT)
            for t in range(NTT):
                k = t % 2
                s.op("dve", lambda e, t=t, k=k: e.scalar_tensor_tensor(out=XS[k][:], in0=H[:, t, :], scalar=rstd[:, t:t + 1],
                                                                       in1=g3b[:], op0=ALU.mult, op1=ALU.mult),
                     reads=[bH, brstd, CON], writes=[bXS[k]])
                def tr(e, k=k):
                    ins = None
                    for c in range(NCH):
                        ins = e.transpose(out=self.bank(0, BF16)[:, c * 128:(c + 1) * 128], in_=XS[k][:, c * 128:(c + 1) * 128],
                                          identity=ident_b[:])
                    return ins
                s.op("pe", tr, reads=[bXS[k], CON], writes=[pb[0]])
                s.op("act", lambda e, t=t: e.copy(out=XT[:, :, t * 128:(t + 1) * 128],
                                                  in_=self.bank(0, BF16).rearrange("p (c t) -> p c t", c=NCH)),
                     reads=[pb[0]], writes=[bXT])
            def rl(e):
                ins = None
                for t in range(NTT):
                    for c in range(NCH):
                        ins = e.matmul(out=self.bank(1)[:, t * 36:(t + 1) * 36], lhsT=XT[:, c, t * 128:(t + 1) * 128],
                                       rhs=wr[:, c, :], start=(c == 0), stop=(c == NCH - 1))
                return ins
            s.op("pe", rl, reads=[bXT, CON], writes=[pb[1]])
            s.op("dve", lambda e: e.tensor_tensor(out=L[:], in0=self.bank(1)[:, :NTT * 36].rearrange("p (t n) -> p t n", t=NTT),
                                                  in1=bb[:], op=ALU.add), reads=[pb[1], CON], writes=[bL])
            Lg = L[:, :, 0:4]
            Le = L[:, :, 4:36].rearrange("p t (g e) -> p t g e", g=4)
            s.op("dve", lambda e: e.tensor_reduce(out=r1[:], in_=Lg, axis=AX.X, op=ALU.max), reads=[bL], writes=[br[1]])
            s.op("dve", lambda e: e.tensor_tensor(out=dg[:], in0=Lg, in1=_bc_last(r1[:], 4), op=ALU.subtract),
                 reads=[bL, br[1]], writes=[bdg])
            s.op("act", lambda e: e.activation(out=eg[:], in_=dg[:], func=AF.Exp), reads=[bdg], writes=[beg])
            s.op("dve", lambda e: e.tensor_reduce(out=r2[:], in_=eg[:], axis=AX.X, op=ALU.add), reads=[beg], writes=[br[2]])
            s.op("dve", lambda e: e.reciprocal(out=r2[:], in_=r2[:]), reads=[br[2]], writes=[br[2]])
            s.op("dve", lambda e: e.tensor_single_scalar(out=oh[:], in_=dg[:], scalar=0.0, op=ALU.is_ge), reads=[bdg], writes=[boh])
            s.op("dve", lambda e: e.tensor_tensor(out=tmp[:], in0=Le, in1=_bc_last(oh[:], 8), op=ALU.mult),
                 reads=[bL, boh], writes=[btmp])
            s.op("dve", lambda e: e.tensor_reduce(out=sl[:], in_=tmp[:].rearrange("p t g e -> p t e g"), axis=AX.X, op=ALU.add),
                 reads=[btmp], writes=[bsl])
            s.op("dve", lambda e: e.tensor_reduce(out=r3[:], in_=sl[:], axis=AX.X, op=ALU.max), reads=[bsl], writes=[br[3]])
            s.op("dve", lambda e: e.tensor_tensor(out=eq1[:], in0=sl[:], in1=_bc_last(r3[:], 8), op=ALU.is_equal),
                 reads=[bsl, br[3]], writes=[beq1])
            s.op("dve", lambda e: e.scalar_tensor_tensor(out=sl2[:], in0=eq1[:], scalar=-1e30, in1=sl[:], op0=ALU.mult, op1=ALU.add),
                 reads=[beq1, bsl], writes=[bsl2])
            s.op("dve", lambda e: e.tensor_reduce(out=r4[:], in_=sl2[:], axis=AX.X, op=ALU.max), reads=[bsl2], writes=[br[4]])
            s.op("dve", lambda e: e.tensor_tensor(out=eq2[:], in0=sl2[:], in1=_bc_last(r4[:], 8), op=ALU.is_equal),
                 reads=[bsl2, br[4]], writes=[beq2])
            s.op("dve", lambda e: e.tensor_tensor(out=r5[:], in0=r4[:], in1=r3[:], op=ALU.subtract), reads=[br[3], br[4]], writes=[br[5]])
            s.op("act", lambda e: e.activation(out=r5[:], in_=r5[:], func=AF.Exp), reads=[br[5]], writes=[br[5]])
            s.op("dve", lambda e: e.tensor_scalar(out=r3[:], in0=r5[:], scalar1=1.0, scalar2=None, op0=ALU.add), reads=[br[5]], writes=[br[3]])
            s.op("dve", lambda e: e.reciprocal(out=r3[:], in_=r3[:]), reads=[br[3]], writes=[br[3]])
            s.op("dve", lambda e: e.tensor_tensor(out=r3[:], in0=r3[:], in1=r2[:], op=ALU.mult), reads=[br[3], br[2]], writes=[br[3]])
            s.op("dve", lambda e: e.tensor_tensor(out=r4[:], in0=r3[:], in1=r5[:], op=ALU.mult), reads=[br[3], br[5]], writes=[br[4]])
            s.op("dve", lambda e: e.tensor_tensor(out=eq1[:], in0=eq1[:], in1=_bc_last(r3[:], 8), op=ALU.mult),
                 reads=[beq1, br[3]], writes=[beq1])
            s.op("dve", lambda e: e.tensor_tensor(out=eq2[:], in0=eq2[:], in1=_bc_last(r4[:], 8), op=ALU.mult),
                 reads=[beq2, br[4]], writes=[beq2])
            s.op("dve", lambda e: e.tensor_tensor(out=cw8[:], in0=eq1[:], in1=eq2[:], op=ALU.add), reads=[beq1, beq2], writes=[bcw8])
            s.op("dve", lambda e: e.tensor_tensor(out=CW[:].rearrange("p t (g e) -> p t g e", g=4), in0=_bc_last(oh[:], 8),
                                                  in1=cw8[:].unsqueeze(2).broadcast_to([128, NTT, 4, 8]), op=ALU.mult),
                 reads=[boh, bcw8], writes=[bCW])
            def trcw(e):
                ins = None
                for t in range(NTT):
                    ins = e.transpose(out=self.bank(1, BF16)[0:32, t * 128:(t + 1) * 128], in_=CW[:, t, :], identity=ident_b[:])
                return ins
            s.op("pe", trcw, reads=[bCW, CON], writes=[pb[1]])
            s.op("act", lambda e: e.copy(out=CWT[:], in_=self.bank(1, BF16)[0:32, :]), reads=[pb[1]], writes=[bCWT])
            it = 0
            for sg in range(8):
                for el in range(4):
                    e_ = sg * 4 + el
                    k = e_ % 2
                    for half in range(2):
                        tsl = slice(half * 512, (half + 1) * 512)
                        bc = 2 + (it % 2)
                        s.op("pe", lambda e, bc=bc, e_=e_, tsl=tsl: e.matmul(out=self.bank(bc), lhsT=sel[:, e_ * 128:(e_ + 1) * 128],
                                                                          rhs=CWT[:, tsl], start=True, stop=True),
                             reads=[bCWT, CON], writes=[pb[bc]])
                        for fc in range(2):
                            j = it % 2
                            bg, bu = 4 + j, 6 + j
                            def mm(e, W, bnk, fc=fc, tsl=tsl):
                                ins = None
                                for c in range(NCH):
                                    ins = e.matmul(out=self.bank(bnk), lhsT=W[:, c, fc * 128:(fc + 1) * 128], rhs=XT[:, c, tsl],
                                                   start=(c == 0), stop=(c == NCH - 1))
                                return ins
                            s.op("pe", lambda e, mm=mm, k=k, bg=bg: mm(e, WG[k], bg), reads=[bWG[k], bXT], writes=[pb[bg]])
                            s.op("pe", lambda e, mm=mm, k=k, bu=bu: mm(e, WU[k], bu), reads=[bWU[k], bXT], writes=[pb[bu]])
                            s.op("act", lambda e, j=j, bg=bg: e.activation(out=SG[j][:], in_=self.bank(bg), func=AF.Silu),
                                 reads=[pb[bg]], writes=[bSG[j]])
                            s.op("dve", lambda e, j=j, bu=bu: e.tensor_tensor(out=T1[j][:], in0=SG[j][:], in1=self.bank(bu), op=ALU.mult),
                                 reads=[bSG[j], pb[bu]], writes=[bT1[j]])
                            hj = el * 2 + fc
                            s.op("dve", lambda e, j=j, bc=bc, hj=hj, tsl=tsl: e.tensor_tensor(out=HID[:, hj, tsl], in0=T1[j][:],
                                                                                            in1=self.bank(bc), op=ALU.mult),
                                 reads=[bT1[j], pb[bc]], writes=[bHID[hj]])
                            it += 1
                    if e_ + 2 < NE:
                        load_expert(e_ + 2)
                ws = sg % 2
                for t in range(NTT):
                    for dh in range(2):
                        bd = (t * 2 + dh) % 2
                        def dmm(e, t=t, dh=dh, bd=bd, ws=ws):
                            ins = None
                            for j in range(8):
                                ins = e.matmul(out=self.bank(bd), lhsT=HID[:, j, t * 128:(t + 1) * 128],
                                               rhs=WD[ws][:, j, dh * 512:(dh + 1) * 512], start=(j == 0), stop=(j == 7))
                            return ins
                        s.op("pe", dmm, reads=bHID + [bWD[ws]], writes=[pb[bd]])
                        s.op("dve", lambda e, t=t, dh=dh, bd=bd: e.tensor_tensor(out=H[:, t, dh * 512:(dh + 1) * 512],
                                                                                in0=H[:, t, dh * 512:(dh + 1) * 512],
                                                                                in1=self.bank(bd), op=ALU.add),
                             reads=[bH, pb[bd]], writes=[bH])
                if sg + 1 < 8:
                    load_down(sg + 1)
            rms_stats(lambda t: H[:, t, :], NTT)
            for t in range(NTT):
                k = t % 2
                s.op("dve", lambda e, t=t, k=k: e.scalar_tensor_tensor(out=OUT[k][:], in0=H[:, t, :], scalar=rstd[:, t:t + 1],
                                                                       in1=gfb[:], op0=ALU.mult, op1=ALU.mult),
                     reads=[bH, brstd, CON], writes=[bOUT[k]])
                s.dma("sp", self.out[tok0 + t * 128: tok0 + (t + 1) * 128, :], OUT[k][:], store=bOUT[k])


def _consts():
    ident = np.eye(128, dtype=np.float32)
    sel = np.zeros((32, 32, 128), np.float32)
    for e in range(32):
        sel[e, e, :] = 1.0
    inv16 = np.tile((1.0 / np.arange(1, 17, dtype=np.float32))[None, :], (128, 1)).astype(np.float32)
    ones = np.ones((128, 128), np.float32)
    sel2 = np.zeros((64, 128), np.float32)
    for i in range(64):
        sel2[i, 64 + (i % 32)] = 1.0
        sel2[i, 96 + (i % 32)] = 1.0
    invf = (1.0 / (np.float32(10000.0) ** (np.arange(0, 32, 2, dtype=np.float32) / np.float32(32)))).astype(np.float32)
    rope = np.zeros((128, 4), np.float32)
    hp, pi = np.float32(np.pi / 2), np.float32(np.pi)
    for p in range(128):
        if p < 64:
            rope[p, 0], rope[p, 1] = 0.0, hp
        elif p < 96:
            rope[p, 0], rope[p, 1] = invf[p % 16], hp
        elif p < 112:
            rope[p, 0], rope[p, 1] = invf[p % 16], pi
        else:
            rope[p, 0], rope[p, 1] = invf[p % 16], 0.0
        if p < 32:
            rope[p, 2], rope[p, 3] = invf[p % 16], hp
        elif p < 48:
            rope[p, 2], rope[p, 3] = invf[p % 16], pi
        elif p < 64:
            rope[p, 2], rope[p, 3] = invf[p % 16], 0.0
    tri = np.triu(np.ones((128, 128), np.float32), 1)
    kbv = np.tile((512.0 * np.arange(96, dtype=np.float32))[None, :], (128, 1)).astype(np.float32)
    epv = (np.arange(8, dtype=np.float32)[None, :] * 128 + np.arange(128, dtype=np.float32)[:, None]).astype(np.float32)
    return {"c_tri": tri, "c_kb": kbv, "c_ep": epv, "c_ident": ident, "c_sel": sel.reshape(32, 32 * 128), "c_inv16": inv16, "c_ones": ones, "c_sel2": sel2, "c_rope": rope}


def _weights(inp):
    w = {}
    for n in ["mix_norm_g", "w_in", "q_norm_g", "w_q_up", "kv_norm_g", "w_kv_up", "w_attn_branch", "pool_w", "pool_scale",
              "w_pool_branch", "w_mix_out", "xattn_norm_g", "mem_norm_g", "w_xq", "w_xkv", "w_xo", "ffn_norm_g",
              "w_exp_gate", "w_exp_up", "w_exp_down"]:
        w[n] = np.ascontiguousarray(np.asarray(inp[n], np.float32)[0])
    w["final_norm_g"] = np.ascontiguousarray(np.asarray(inp["final_norm_g"], np.float32))
    w["w_router"] = np.ascontiguousarray(np.concatenate([np.asarray(inp["w_router_group"])[0], np.asarray(inp["w_router_expert"])[0]], axis=1))
    w["b_router"] = np.ascontiguousarray(np.concatenate([np.asarray(inp["b_router_group"])[0], np.asarray(inp["b_router_expert"])[0]], axis=0))
    return w


def kernel(**inputs):
    ncores = 8
    x = np.asarray(inputs["x"], np.float32)
    B, S, _ = x.shape
    NB = B // ncores
    prog = Prog(NB, S)
    nc = prog.build()
    w = _weights(inputs)
    w.update(_consts())
    mem = np.asarray(inputs["mem"], np.float32)
    pos = np.asarray(inputs["positions"], np.int32)
    in_maps = []
    for c in range(ncores):
        m = dict(w)
        m["x"] = np.ascontiguousarray(x[c * NB:(c + 1) * NB].reshape(NB * S, D))
        m["mem"] = np.ascontiguousarray(mem[c * NB:(c + 1) * NB].reshape(NB * MEM, D))
        m["positions"] = np.ascontiguousarray(pos[c * NB:(c + 1) * NB])
        in_maps.append(m)
    res = run_bass_kernel_spmd(nc, in_maps, core_ids=list(range(ncores)))
    outs = [np.asarray(r["out"]).reshape(NB, S, D) for r in res.results]
    return np.concatenate(outs, axis=0).astype(np.float32)
```
